# Optimizing a Trainium2 kernel written in Bass

```python
import jax, jax.numpy as jnp
from jax import lax
import numpy as np

D_MODEL = 1024
BATCH = 8
SEQ = 8192
DEPTH = 4

EPS = 1e-6
FFN_DIM = 1408
Q_BLOCK = 128

SB_HEADS = 4
SB_HEAD_DIM = 64
SB_WIDTH = SB_HEADS * SB_HEAD_DIM

ML_HEADS = 4
ML_HEAD_DIM = 128
ML_WIDTH = ML_HEADS * ML_HEAD_DIM
ML_CHUNK = 128
ML_CONV = 4
ML_FORGET_BIAS_LO = 3.0
ML_FORGET_BIAS_HI = 6.0

MLA_HEADS = 4
MLA_NOPE = 64
MLA_ROPE = 32
MLA_QK = MLA_NOPE + MLA_ROPE
MLA_V = 64
MLA_WIDTH = MLA_HEADS * MLA_V
MLA_Q_RANK = 256
MLA_KV_RANK = 128
ROPE_THETA = 10000.0

N_BRANCH = 3
SEGMENTS = (
    ('sb_q', SB_WIDTH), ('sb_k', SB_WIDTH), ('sb_v', SB_WIDTH),
    ('ml_q', ML_WIDTH), ('ml_k', ML_WIDTH), ('ml_v', ML_WIDTH), ('ml_o', ML_WIDTH),
    ('ml_i', ML_HEADS), ('ml_f', ML_HEADS),
    ('mla_cq', MLA_Q_RANK), ('mla_ckv', MLA_KV_RANK), ('mla_kr', MLA_ROPE),
    ('gates', N_BRANCH * D_MODEL),
)
N_IN = sum(w for _, w in SEGMENTS)

kernel_name = 'hybrid_sb_mlstm_mla_macaron'


def _seg_range(name):
    off = 0
    for n, w in SEGMENTS:
        if n == name:
            return off, off + w
        off += w
    raise KeyError(name)


def _split_cols(z):
    out = {}
    off = 0
    for n, w in SEGMENTS:
        out[n] = z[..., off:off + w]
        off += w
    return out


def _rms_norm(x, gain):
    xf = x.astype(jnp.float32)
    y = xf * lax.rsqrt(jnp.mean(xf * xf, axis=-1, keepdims=True) + EPS)
    return (y * gain.astype(jnp.float32)).astype(x.dtype)


def _swiglu(u, wi, wo):
    a, g = jnp.split(u @ wi, 2, axis=-1)
    return (jax.nn.silu(a) * g) @ wo


def _to_heads(z, n_heads):
    b, s, w = z.shape
    return z.reshape(b, s, n_heads, w // n_heads).transpose(0, 2, 1, 3)


def _from_heads(z):
    b, h, s, d = z.shape
    return z.transpose(0, 2, 1, 3).reshape(b, s, h * d)


def _strict_lower(n):
    return jnp.asarray(np.tril(np.ones((n, n), np.float32), -1))


def _causal_mask(i, n, strict):
    qpos = i * Q_BLOCK + np.arange(Q_BLOCK)
    kpos = np.arange(n)
    m = kpos[None, :] < qpos[:, None] if strict else kpos[None, :] <= qpos[:, None]
    return jnp.asarray(m)


def _stick_breaking_attention(q, k, v):
    b, h, s, _ = q.shape
    scale = SB_HEAD_DIM ** -0.5
    tri_in = _strict_lower(Q_BLOCK)
    outs = []
    for i in range(s // Q_BLOCK):
        nk = i + 1
        n = nk * Q_BLOCK
        qb = q[:, :, i * Q_BLOCK:n]
        z = jnp.einsum('bhqd,bhkd->bhqk', qb, k[:, :, :n], preferred_element_type=jnp.float32) * scale
        mask = _causal_mask(i, n, strict=True)
        log_keep = jnp.where(mask, jax.nn.log_sigmoid(-z), 0.0)
        lk = log_keep.reshape(b, h, Q_BLOCK, nk, Q_BLOCK)
        within = jnp.einsum('bhqnj,js->bhqns', lk, tri_in)
        after = jnp.einsum('bhqm,mn->bhqn', jnp.sum(lk, axis=-1), _strict_lower(nk))
        later = (within + after[..., None]).reshape(b, h, Q_BLOCK, n)
        w = jnp.where(mask, jnp.exp(jax.nn.log_sigmoid(z) + later), 0.0)
        outs.append(jnp.einsum('bhqk,bhkd->bhqd', w.astype(v.dtype), v[:, :, :n]))
    return jnp.concatenate(outs, axis=2)


def _causal_softmax_attention(q, k, v, scale):
    s = q.shape[2]
    outs = []
    for i in range(s // Q_BLOCK):
        n = (i + 1) * Q_BLOCK
        qb = q[:, :, i * Q_BLOCK:n]
        z = jnp.einsum('bhqd,bhkd->bhqk', qb, k[:, :, :n], preferred_element_type=jnp.float32) * scale
        z = jnp.where(_causal_mask(i, n, strict=False), z, -jnp.inf)
        p = jnp.exp(z - jnp.max(z, axis=-1, keepdims=True))
        denom = jnp.sum(p, axis=-1, keepdims=True)
        o = jnp.einsum('bhqk,bhkd->bhqd', p.astype(v.dtype), v[:, :, :n], preferred_element_type=jnp.float32)
        outs.append((o / denom).astype(v.dtype))
    return jnp.concatenate(outs, axis=2)


def _rope(x, positions):
    half = MLA_ROPE // 2
    inv_freq = jnp.power(ROPE_THETA, -jnp.arange(half, dtype=jnp.float32) / half)
    ang = positions.astype(jnp.float32)[:, None, :, None] * inv_freq
    cos, sin = jnp.cos(ang), jnp.sin(ang)
    xf = x.astype(jnp.float32)
    x1, x2 = xf[..., :half], xf[..., half:]
    return jnp.concatenate([x1 * cos - x2 * sin, x2 * cos + x1 * sin], axis=-1).astype(x.dtype)


def _mla(c_q, c_kv, k_rope, positions, q_norm, kv_norm, wq_up, wkv_up, q_gain, k_gain):
    b, s, _ = c_q.shape
    q = (_rms_norm(c_q, q_norm) @ wq_up).reshape(b, s, MLA_HEADS, MLA_QK)
    kv = (_rms_norm(c_kv, kv_norm) @ wkv_up).reshape(b, s, MLA_HEADS, MLA_NOPE + MLA_V)
    k_nope, v = kv[..., :MLA_NOPE], kv[..., MLA_NOPE:]
    k_pe = jnp.broadcast_to(k_rope[:, :, None, :], (b, s, MLA_HEADS, MLA_ROPE))
    k = jnp.concatenate([k_nope, k_pe], axis=-1)
    q = _rms_norm(q, q_gain).transpose(0, 2, 1, 3)
    k = _rms_norm(k, k_gain).transpose(0, 2, 1, 3)
    v = v.transpose(0, 2, 1, 3)
    q = jnp.concatenate([q[..., :MLA_NOPE], _rope(q[..., MLA_NOPE:], positions)], axis=-1)
    k = jnp.concatenate([k[..., :MLA_NOPE], _rope(k[..., MLA_NOPE:], positions)], axis=-1)
    return _from_heads(_causal_softmax_attention(q, k, v, MLA_QK ** -0.5))


def _causal_dwconv(x, w, bias):
    y = lax.conv_general_dilated(
        x, w[:, None, :], window_strides=(1,), padding=[(ML_CONV - 1, 0)],
        dimension_numbers=('NWC', 'WIO', 'NWC'), feature_group_count=x.shape[-1])
    return y + bias


def _mlstm_chunkwise(q, k, v, ig, lf):
    b, h, s, dk = q.shape
    dv = v.shape[-1]
    nc = s // ML_CHUNK

    def chunks(z):
        return jnp.moveaxis(z.reshape((b, h, nc, ML_CHUNK) + z.shape[3:]), 2, 0)

    causal = jnp.asarray(np.tril(np.ones((ML_CHUNK, ML_CHUNK), dtype=bool)))
    lower_incl = jnp.asarray(np.tril(np.ones((ML_CHUNK, ML_CHUNK), np.float32)))

    def step(carry, xs):
        c_prev, n_prev, m_prev = carry
        qc, kc, vc, igc, lfc = xs
        cum_f = jnp.einsum('bhs,ts->bht', lfc, lower_incl)
        log_intra = jnp.where(causal, cum_f[..., :, None] - cum_f[..., None, :] + igc[..., None, :], -jnp.inf)
        log_inter = cum_f + m_prev[..., None]
        m = jnp.maximum(log_inter, jnp.max(log_intra, axis=-1))
        w_intra = jnp.exp(log_intra - m[..., None])
        w_inter = jnp.exp(log_inter - m)
        scores = jnp.einsum('bhtd,bhsd->bhts', qc, kc) * w_intra
        num = scores @ vc + w_inter[..., None] * (qc @ c_prev)
        den = jnp.sum(scores, axis=-1) + w_inter * jnp.einsum('bhtd,bhd->bht', qc, n_prev)
        h_out = num / jnp.maximum(jnp.abs(den), jnp.exp(-m))[..., None]
        f_total = cum_f[..., -1]
        log_to_end = f_total[..., None] - cum_f + igc
        m_new = jnp.maximum(f_total + m_prev, jnp.max(log_to_end, axis=-1))
        decay = jnp.exp(f_total + m_prev - m_new)
        w_end = jnp.exp(log_to_end - m_new[..., None])
        c_new = decay[..., None, None] * c_prev + jnp.einsum('bhsd,bhsv->bhdv', kc * w_end[..., None], vc)
        n_new = decay[..., None] * n_prev + jnp.einsum('bhs,bhsd->bhd', w_end, kc)
        return (c_new, n_new, m_new), h_out

    f32 = jnp.float32
    init = (jnp.zeros((b, h, dk, dv), f32), jnp.zeros((b, h, dk), f32), jnp.zeros((b, h), f32))
    _, hs = lax.scan(step, init, (chunks(q), chunks(k), chunks(v), chunks(ig), chunks(lf)))
    return jnp.moveaxis(hs, 0, 2).reshape(b, h, s, dv)


def _mlstm(q, k, v, o_pre, i_pre, f_pre, conv_w, conv_b, out_gain):
    b, s, _ = q.shape
    f32 = jnp.float32
    qk = jax.nn.silu(_causal_dwconv(jnp.concatenate([q, k], axis=-1), conv_w, conv_b))
    q, k = jnp.split(qk, 2, axis=-1)
    qh = _to_heads(q, ML_HEADS).astype(f32)
    kh = _to_heads(k, ML_HEADS).astype(f32) * ML_HEAD_DIM ** -0.5
    vh = _to_heads(v, ML_HEADS).astype(f32)
    ig = i_pre.astype(f32).transpose(0, 2, 1)
    lf = jax.nn.log_sigmoid(f_pre.astype(f32)).transpose(0, 2, 1)
    hh = _mlstm_chunkwise(qh, kh, vh, ig, lf).transpose(0, 2, 1, 3)
    hh = _rms_norm(hh, out_gain.reshape(ML_HEADS, ML_HEAD_DIM)).reshape(b, s, ML_WIDTH)
    return (jax.nn.sigmoid(o_pre.astype(f32)) * hh).astype(v.dtype)


def setup_inputs(seed: int = 0) -> dict:
    key = jax.random.key(seed)
    ks = jax.random.split(key, 32)
    f32 = jnp.float32

    def normal(k, shape, fan_in):
        return jax.random.normal(k, shape, f32) * fan_in ** -0.5

    def gain(k, shape):
        return 1.0 + 0.02 * jax.random.normal(k, shape, f32)

    L, D = DEPTH, D_MODEL
    x = jax.random.normal(ks[0], (BATCH, SEQ, D), f32)
    offsets = jax.random.randint(ks[1], (BATCH, 1), 0, 4096, dtype=jnp.int32)
    positions = offsets + jnp.arange(SEQ, dtype=jnp.int32)[None, :]

    b_in = 0.01 * jax.random.normal(ks[2], (L, N_IN), f32)
    f0, f1 = _seg_range('ml_f')
    b_in = b_in.at[:, f0:f1].add(jnp.linspace(ML_FORGET_BIAS_LO, ML_FORGET_BIAS_HI, ML_HEADS, dtype=f32))

    return {
        'x': x,
        'positions': positions,
        'ffn1_norm': gain(ks[3], (L, D)),
        'ffn1_wi': normal(ks[4], (L, D, 2 * FFN_DIM), D),
        'ffn1_wo': normal(ks[5], (L, FFN_DIM, D), FFN_DIM),
        'mix_norm': gain(ks[6], (L, D)),
        'w_in': normal(ks[7], (L, D, N_IN), D),
        'b_in': b_in,
        'ml_conv_w': normal(ks[8], (L, ML_CONV, 2 * ML_WIDTH), ML_CONV),
        'ml_conv_b': 0.01 * jax.random.normal(ks[9], (L, 2 * ML_WIDTH), f32),
        'ml_out_norm': gain(ks[10], (L, ML_WIDTH)),
        'mla_q_norm': gain(ks[11], (L, MLA_Q_RANK)),
        'mla_kv_norm': gain(ks[12], (L, MLA_KV_RANK)),
        'mla_wq_up': normal(ks[13], (L, MLA_Q_RANK, MLA_HEADS * MLA_QK), MLA_Q_RANK),
        'mla_wkv_up': normal(ks[14], (L, MLA_KV_RANK, MLA_HEADS * (MLA_NOPE + MLA_V)), MLA_KV_RANK),
        'mla_q_gain': gain(ks[15], (L, MLA_QK)),
        'mla_k_gain': gain(ks[16], (L, MLA_QK)),
        'w_up_sb': normal(ks[17], (L, SB_WIDTH, D), SB_WIDTH),
        'w_up_ml': normal(ks[18], (L, ML_WIDTH, D), ML_WIDTH),
        'w_up_mla': normal(ks[19], (L, MLA_WIDTH, D), MLA_WIDTH),
        'w_out': normal(ks[20], (L, D, D), D),
        'ffn2_norm': gain(ks[21], (L, D)),
        'ffn2_wi': normal(ks[22], (L, D, 2 * FFN_DIM), D),
        'ffn2_wo': normal(ks[23], (L, FFN_DIM, D), FFN_DIM),
    }


def reference(x, positions, ffn1_norm, ffn1_wi, ffn1_wo, mix_norm, w_in, b_in, ml_conv_w, ml_conv_b,
              ml_out_norm, mla_q_norm, mla_kv_norm, mla_wq_up, mla_wkv_up, mla_q_gain, mla_k_gain,
              w_up_sb, w_up_ml, w_up_mla, w_out, ffn2_norm, ffn2_wi, ffn2_wo):
    b, s, d = x.shape
    for l in range(DEPTH):
        x = x + 0.5 * _swiglu(_rms_norm(x, ffn1_norm[l]), ffn1_wi[l], ffn1_wo[l])

        u = _rms_norm(x, mix_norm[l])
        c = _split_cols(u @ w_in[l] + b_in[l])

        y_sb = _from_heads(_stick_breaking_attention(
            _to_heads(c['sb_q'], SB_HEADS), _to_heads(c['sb_k'], SB_HEADS), _to_heads(c['sb_v'], SB_HEADS)))
        y_ml = _mlstm(c['ml_q'], c['ml_k'], c['ml_v'], c['ml_o'], c['ml_i'], c['ml_f'],
                      ml_conv_w[l], ml_conv_b[l], ml_out_norm[l])
        y_mla = _mla(c['mla_cq'], c['mla_ckv'], c['mla_kr'], positions, mla_q_norm[l], mla_kv_norm[l],
                     mla_wq_up[l], mla_wkv_up[l], mla_q_gain[l], mla_k_gain[l])

        g = jax.nn.sigmoid(c['gates'].astype(jnp.float32)).astype(x.dtype).reshape(b, s, N_BRANCH, d)
        merged = (g[:, :, 0] * (y_sb @ w_up_sb[l])
                  + g[:, :, 1] * (y_ml @ w_up_ml[l])
                  + g[:, :, 2] * (y_mla @ w_up_mla[l]))
        x = x + merged @ w_out[l]

        x = x + 0.5 * _swiglu(_rms_norm(x, ffn2_norm[l]), ffn2_wi[l], ffn2_wo[l])
    return x
```

```python
from contextlib import ExitStack
import numpy as np
import concourse.bass as bass
import concourse.mybir as mybir
from concourse.bass_utils import run_bass_kernel_spmd

F32 = mybir.dt.float32
BF16 = mybir.dt.bfloat16
I32 = mybir.dt.int32
AF = mybir.ActivationFunctionType
ALU = mybir.AluOpType

D = 1024
FF = 1408
NIN = 6312
O_SBQ, O_SBK, O_SBV = 0, 256, 512
O_MLQ, O_MLK, O_MLV, O_MLO, O_MLI, O_MLF = 768, 1280, 1792, 2304, 2816, 2820
O_CQ, O_CKV, O_KR, O_G = 2824, 3080, 3208, 3240
EPS = 1e-6
TT = 512
NV_L = 120
CAST_ELEMS = 1 << 16
TWO_PI = 6.283185307179586
C1 = 6.28125
C2 = TWO_PI - C1


class Chan:
    def __init__(self, sem):
        self.sem = sem
        self.count = 0


class Buf:
    def __init__(self, ctx, t, name):
        self.ctx = ctx
        self.t = t
        self.name = name
        self.lw = None
        self.rd = []
        self.lchan = None
        self.schan = None

    def __getitem__(self, k):
        return self.t[k]


class Op:
    __slots__ = ("eng", "fn", "deps", "signal", "idx", "epoch")

    def __init__(self, eng, fn, deps):
        self.eng = eng
        self.fn = fn
        self.deps = deps
        self.signal = False
        self.idx = 0
        self.epoch = 0


class Ctx:
    ENG = ("pe", "act", "dve", "pool", "sp")

    def __init__(self, nc, es):
        self.nc = nc
        self.es = es
        self.e = {"pe": nc.tensor, "act": nc.scalar, "dve": nc.vector, "pool": nc.gpsimd, "sp": nc.sync}
        self.ops = []
        self.last = {k: None for k in self.ENG}
        self.bufs = []
        self.chans = []
        self.free_chans = []
        self.stacks = [es]
        self.scope_bufs = [[]]
        self.nsem = 0
        self.ninst = 0
        self.limit = None
        self.cnt = {k: 0 for k in self.ENG}
        self.epoch = {k: 0 for k in self.ENG}
        self.sems = None
        self.seen_op = {k: {} for k in self.ENG}
        self.seen_ch = {k: {} for k in self.ENG}
        self.misc = self.new_chan("misc")
        self.out_chan = self.new_chan("outc")

    def sem(self, name):
        self.nsem += 1
        return self.es.enter_context(self.nc.semaphore(name))

    def new_chan(self, name):
        if self.free_chans:
            return self.free_chans.pop()
        c = Chan(self.sem("c_" + name))
        self.chans.append(c)
        return c

    def push(self):
        st = ExitStack()
        self.stacks.append(st)
        self.scope_bufs.append([])

    def pop(self):
        st = self.stacks.pop()
        for b in self.scope_bufs.pop():
            self.bufs.remove(b)
            for c in (b.lchan, b.schan):
                if c is not None:
                    self.free_chans.append(c)
        st.close()

    def sb(self, name, shape, dt):
        self.uid = getattr(self, "uid", 0) + 1
        b = Buf(self, self.stacks[-1].enter_context(self.nc.sbuf_tensor("%s_s%d" % (name, self.uid), list(shape), dt)), name)
        self.bufs.append(b)
        self.scope_bufs[-1].append(b)
        return b

    def ps(self, name, shape, dt):
        b = Buf(self, self.es.enter_context(self.nc.psum_tensor(name + "_p", list(shape), dt)), name)
        self.bufs.append(b)
        return b

    def _deps(self, reads, writes):
        deps = []
        for b in reads:
            if b.lw is not None:
                deps.append(b.lw)
        for b in writes:
            if b.lw is not None:
                deps.append(b.lw)
            deps.extend(b.rd)
        return deps

    def op(self, eng, fn, reads=(), writes=()):
        if self.limit is not None:
            self.limit -= 1
            if self.limit < 0:
                return None
        deps = self._deps(reads, writes)
        o = Op(eng, fn, deps)
        tok = ("op", o)
        for b in writes:
            b.lw = tok
            b.rd = []
        for b in reads:
            if b not in writes:
                b.rd.append(tok)
                if len(b.rd) > 64:
                    b.rd = b.rd[-48:]
        self.ops.append(o)
        self.last[eng] = o
        return o

    def dma(self, q, out_ap, in_ap, reads=(), writes=(), chan=None):
        deps = self._deps(reads, writes)
        chans = []
        for b in writes:
            if b.lchan is None:
                b.lchan = self.new_chan("l_" + b.name)
            chans.append(b.lchan)
        for b in reads:
            if b.schan is None:
                b.schan = self.new_chan("s_" + b.name)
            chans.append(b.schan)
        if chan is not None:
            chans.append(chan)
        if not chans:
            chans = [self.misc]
        ch = chans[0]
        assert len(chans) == 1, "dma must touch exactly one tracked buf"
        ch.count += 16
        tok = ("dma", ch, ch.count)

        def fn(e, out_ap=out_ap, in_ap=in_ap, ch=ch):
            return e.dma_start(out=out_ap, in_=in_ap).then_inc(ch.sem, 16)

        o = Op(q, fn, deps)
        o.signal = None
        for b in writes:
            b.lw = tok
            b.rd = []
        for b in reads:
            b.rd.append(tok)
        self.ops.append(o)
        return o

    def barrier(self):
        toks = []
        for k in self.ENG:
            if self.last[k] is not None:
                toks.append(("op", self.last[k]))
        for c in self.chans:
            if c.count:
                toks.append(("dma", c, c.count))
        for k in self.ENG:
            o = Op(k, None, list(toks))
            o.signal = None
            self.ops.append(o)
            o.fn = "barrier"
        for b in self.bufs:
            b.lw = None
            b.rd = []
        self.ops.append("epoch")
        self.emit()
        self.ops = []
        self.last = {k: None for k in self.ENG}

    def emit(self):
        for o in self.ops:
            if o == "epoch":
                continue
            for d in o.deps:
                if d[0] == "op" and (d[1].eng != o.eng or o.eng != "pe") and d[1].signal is not None:
                    d[1].signal = True
        cnt = self.cnt
        epoch = self.epoch
        if self.sems is None:
            self.sems = {k: [self.sem("e_%s_0" % k)] for k in self.ENG}
        sems = self.sems
        for o in self.ops:
            if o == "epoch":
                for k in self.ENG:
                    if cnt[k] > 20000:
                        epoch[k] += 1
                        cnt[k] = 0
                        sems[k].append(self.sem("e_%s_%d" % (k, epoch[k])))
                continue
            if o.signal is True:
                cnt[o.eng] += 1
                o.idx = cnt[o.eng]
                o.epoch = epoch[o.eng]
        seen_op = self.seen_op
        seen_ch = self.seen_ch
        ninst = 0
        for o in self.ops:
            if o == "epoch":
                continue
            e = self.e[o.eng]
            need_op = {}
            need_ch = {}
            for d in o.deps:
                if d[0] == "op":
                    s = d[1]
                    if s.eng == o.eng and o.eng == "pe":
                        continue
                    if s.signal is not True:
                        continue
                    key = (s.eng, s.epoch)
                    if seen_op[o.eng].get(key, 0) >= s.idx:
                        continue
                    need_op[key] = max(need_op.get(key, 0), s.idx)
                else:
                    _, ch, c = d
                    if seen_ch[o.eng].get(ch, 0) >= c:
                        continue
                    need_ch[ch] = max(need_ch.get(ch, 0), c)
            for key, v in need_op.items():
                e.wait_ge(sems[key[0]][key[1]], v)
                seen_op[o.eng][key] = v
                ninst += 1
            for ch, v in need_ch.items():
                e.wait_ge(ch.sem, v)
                seen_ch[o.eng][ch] = v
                ninst += 1
            if o.fn == "barrier":
                continue
            ins = o.fn(e)
            ninst += 1
            if o.signal is True:
                ins.then_inc(sems[o.eng][o.epoch], 1)
        self.ninst += ninst
        return ninst


class Ring:
    def __init__(self, bufs):
        self.bufs = bufs
        self.i = 0

    def next(self):
        b = self.bufs[self.i % len(self.bufs)]
        self.i += 1
        return b


CONST_COLS = {}


def build_consts():
    cols = []
    off = 0

    def add(name, arr):
        nonlocal off
        a = np.zeros((128, arr.shape[1]), np.float32)
        a[: arr.shape[0]] = arr
        CONST_COLS[name] = (off, arr.shape[1])
        off += arr.shape[1]
        cols.append(a)

    j = np.arange(128)[:, None]
    s = np.arange(128)[None, :]
    add("tri", (j > s).astype(np.float32))
    add("upper", (j <= s).astype(np.float32))
    add("ident", (j == s).astype(np.float32))
    negm = np.where(j > s, -30000.0, 0.0).astype(np.float32)
    add("negm4", np.tile(negm, (1, 4)))
    masks = []
    for jj in range(4):
        ms = np.zeros((128, 512), np.float32)
        mi = np.zeros((128, 512), np.float32)
        for jq in range(4):
            if jq > jj:
                ms[:, jq * 128:(jq + 1) * 128] = 1.0
                mi[:, jq * 128:(jq + 1) * 128] = 1.0
            elif jq == jj:
                ms[:, jq * 128:(jq + 1) * 128] = (j < s)
                mi[:, jq * 128:(jq + 1) * 128] = (j <= s)
        masks.append((ms, mi))
        add("sbn%d" % jj, (1.0 - ms) * -1e9)
    rot = np.zeros((96, 96), np.float32)
    for i in range(16):
        rot[80 + i, 64 + i] = -1.0
        rot[64 + i, 80 + i] = 1.0
    add("rot", rot)
    invf = np.power(np.float32(10000.0), -np.arange(16, dtype=np.float32) / np.float32(16)).astype(np.float32)
    add("invf", np.concatenate([invf, invf])[:, None])
    mk = np.concatenate([m[0] for m in masks] + [m[1] for m in masks], axis=1)
    return np.concatenate(cols, axis=1), mk


def col_layout(v):
    n = v.shape[0] // 128
    return v.reshape(n, 128).T


def build_vecs(inp, L):
    out = np.zeros((128, L * NV_L), np.float32)
    for l in range(L):
        o = l * NV_L
        b = inp["b_in"][l]

        def put(off, arr):
            out[: arr.shape[0], o + off: o + off + arr.shape[1]] = arr

        put(0, col_layout(inp["ffn1_norm"][l]))
        put(8, col_layout(inp["mix_norm"][l]))
        put(16, col_layout(inp["ffn2_norm"][l]))
        put(24, col_layout(b[O_SBQ:O_SBQ + 256]))
        put(26, col_layout(b[O_SBK:O_SBK + 256]))
        put(28, col_layout(b[O_MLQ:O_MLQ + 512]))
        put(32, col_layout(b[O_MLK:O_MLK + 512]))
        put(36, col_layout(b[O_MLO:O_MLO + 512]))
        put(40, col_layout(b[O_CQ:O_CQ + 256]))
        put(42, col_layout(b[O_CKV:O_CKV + 128]))
        put(43, col_layout(b[O_G:O_G + 3072]))
        kr = np.zeros((96, 1), np.float32)
        kr[64:96, 0] = b[O_KR:O_KR + 32]
        put(67, kr)
        cw = inp["ml_conv_w"][l]
        for c in range(8):
            put(68 + c * 4, cw[:, c * 128:(c + 1) * 128].T)
        put(100, col_layout(inp["ml_conv_b"][l]))
        put(108, col_layout(inp["ml_out_norm"][l]))
        put(112, col_layout(inp["mla_q_norm"][l]))
        put(114, col_layout(inp["mla_kv_norm"][l]))
        put(115, inp["mla_q_gain"][l][:, None])
        put(116, inp["mla_k_gain"][l][:, None])
    return out


def build_program(S, L, dbg=None, phases=("p1", "p23", "p4", "p5")):
    NT = S // TT
    NB = S // 128
    nc = bass.Bass("TRN2", target_bir_lowering=False)
    es = ExitStack()
    cx = Ctx(nc, es)

    def din(name, shape, dt=F32):
        return nc.dram_tensor(name, list(shape), dt, kind="ExternalInput").ap()

    def dscr(name, shape, dt):
        return nc.dram_tensor(name, list(shape), dt, kind="Internal").ap()

    xT_in = din("xT", [D, S])
    pos_in = din("pos", [1, S], I32)
    consts_in = din("consts", [128, NCONST])
    masks_in = din("masks", [128, 4096])
    vecs_in = din("vecs", [128, L * NV_L])
    W = {}
    wshapes = {"ffn1_wi": (D, 2 * FF), "ffn1_wo": (FF, D), "w_in": (D, NIN), "mla_wq_up": (256, 384),
               "mla_wkv_up": (128, 512), "w_up_sb": (256, D), "w_up_ml": (512, D), "w_up_mla": (256, D),
               "w_out": (D, D), "ffn2_wi": (D, 2 * FF), "ffn2_wo": (FF, D)}
    Wb = {}
    for k, (a, b) in wshapes.items():
        W[k] = din(k, [L, a, b])
        Wb[k] = dscr(k + "_b", [L, a, b], BF16)
    b_in_d = din("b_in", [L, NIN])
    yT_out = nc.dram_tensor("yT", [D, S], F32, kind="ExternalOutput").ap()
    dbg_out = None
    if dbg is not None:
        dbg_out = nc.dram_tensor("dbg", list(dbg[1]), dbg[2], kind="ExternalOutput").ap()

    xres = dscr("xres", [D, S], F32)
    sbq = dscr("sbq", [256, S], BF16)
    sbk = dscr("sbk", [256, S], BF16)
    sbv = dscr("sbv", [S, 256], BF16)
    mlq = dscr("mlq", [512, S], BF16)
    mlk = dscr("mlk", [512, S], BF16)
    mlv = dscr("mlv", [S, 512], BF16)
    mlo = dscr("mlo", [512, S], BF16)
    mlif = dscr("mlif", [S, 8], F32)
    mlaq = dscr("mlaq", [4, 96, S], BF16)
    mlak = dscr("mlak", [4, 96, S], BF16)
    mlav = dscr("mlav", [S, 256], BF16)
    gat = dscr("gat", [3072, S], BF16)
    ysb = dscr("ysb", [256, S], BF16)
    yml = dscr("yml", [512, S], BF16)
    ymla = dscr("ymla", [256, S], BF16)
    cosd = dscr("cosd", [32, S], F32)
    sind = dscr("sind", [32, S], F32)

    consts = cx.sb("consts", [128, NCONST], F32)
    vecs = cx.sb("vecs", [128, L * NV_L], F32)
    cb16 = cx.sb("cb16", [128, 128 * 3 + 512 * 8], BF16)
    ones32 = cx.sb("ones32", [128, 128], F32)
    pbanks = [cx.ps("ps%d" % i, [128, 512], F32) for i in range(7)]
    pring = Ring(pbanks)
    psb = cx.ps("psb", [128, 1024], BF16)

    def cc(name):
        o, n = CONST_COLS[name]
        return consts[:, o:o + n]

    ONESB = cb16[:, 0:128]
    TRIB = cb16[:, 128:256]
    IDB = cb16[:, 256:384]

    def SBMB(j):
        return cb16[:, 384 + j * 512: 384 + (j + 1) * 512]

    def MLAMB(j):
        return cb16[:, 384 + 2048 + j * 512: 384 + 2048 + (j + 1) * 512]

    cx.dma("sp", consts[:], consts_in, writes=[consts])
    cx.dma("sp", vecs[:], vecs_in, writes=[vecs])
    cx.op("dve", lambda e: e.memset(ones32[:], 1.0), writes=[ones32])
    cx.op("dve", lambda e: e.memset(cb16[:, 0:128], 1.0), writes=[cb16])
    cx.op("dve", lambda e: e.tensor_copy(out=cb16[:, 128:256], in_=cc("tri")), reads=[consts], writes=[cb16])
    cx.op("dve", lambda e: e.tensor_copy(out=cb16[:, 256:384], in_=cc("ident")), reads=[consts], writes=[cb16])
    cx.push()
    mk32 = cx.sb("mk32", [128, 4096], F32)
    cx.dma("sp", mk32[:], masks_in, writes=[mk32])
    cx.op("dve", lambda e: e.tensor_copy(out=cb16[:, 384:384 + 4096], in_=mk32[:]), reads=[mk32], writes=[cb16])
    import os
    for k, (a, b) in wshapes.items():
        if os.environ.get("SKIP_CAST"):
            break
        for l in range(L):
            rows = a
            step = max(1, min(rows, CAST_ELEMS // b))
            r0 = 0
            while r0 < rows:
                r1 = min(rows, r0 + step)
                cx.dma("pool", Wb[k][l, r0:r1, :], W[k][l, r0:r1, :])
                r0 = r1
    RC = 512
    posi = cx.sb("posi", [32, RC], I32)
    posf = cx.sb("posf", [32, RC], F32)
    rk = cx.sb("rk", [32, RC], F32)
    rt = cx.sb("rt", [32, RC], F32)
    rs = cx.sb("rs", [32, RC], F32)
    rc_ = cx.sb("rc", [32, RC], F32)
    invf = cc("invf")[0:32, :]
    MAGIC = 12582912.0
    for r0 in range(0, 0 if os.environ.get("SKIP_ROPE") else S, RC):
        cx.dma("sp", posi[:], pos_in[:, r0:r0 + RC].partition_broadcast(32), writes=[posi])
        cx.op("dve", lambda e: e.tensor_copy(out=posf[:], in_=posi[:]), reads=[posi], writes=[posf])
        cx.op("dve", lambda e: e.tensor_scalar(out=posf[:], in0=posf[:], scalar1=invf, scalar2=None, op0=ALU.mult),
              reads=[posf, consts], writes=[posf])
        for which, dst, shift, dd in (("s", rs, 0.0, sind), ("c", rc_, np.pi / 2, cosd)):
            def f1(e, shift=shift):
                return e.tensor_scalar(out=rk[:], in0=posf[:], scalar1=shift, scalar2=1.0 / TWO_PI, op0=ALU.add, op1=ALU.mult)
            cx.op("dve", f1, reads=[posf], writes=[rk])
            cx.op("dve", lambda e: e.tensor_scalar(out=rk[:], in0=rk[:], scalar1=MAGIC, scalar2=None, op0=ALU.add), reads=[rk], writes=[rk])
            cx.op("dve", lambda e: e.tensor_scalar(out=rk[:], in0=rk[:], scalar1=-MAGIC, scalar2=None, op0=ALU.add), reads=[rk], writes=[rk])
            cx.op("dve", lambda e: e.scalar_tensor_tensor(out=rt[:], in0=rk[:], scalar=-C1, in1=posf[:], op0=ALU.mult, op1=ALU.add),
                  reads=[rk, posf], writes=[rt])
            cx.op("dve", lambda e: e.scalar_tensor_tensor(out=rt[:], in0=rk[:], scalar=-C2, in1=rt[:], op0=ALU.mult, op1=ALU.add),
                  reads=[rk, rt], writes=[rt])
            if shift != 0.0:
                cx.op("dve", lambda e, shift=shift: e.tensor_scalar(out=rt[:], in0=rt[:], scalar1=shift, scalar2=None, op0=ALU.add),
                      reads=[rt], writes=[rt])
            cx.op("dve", lambda e: e.tensor_scalar(out=rt[:], in0=rt[:], scalar1=3.1415925, scalar2=-3.1415925, op0=ALU.min, op1=ALU.max),
                  reads=[rt], writes=[rt])
            cx.op("act", lambda e, dst=dst: e.activation(out=dst[:], in_=rt[:], func=AF.Sin), reads=[rt], writes=[dst])
            cx.dma("pool", dd[:, r0:r0 + RC], dst[:], reads=[dst])
    cx.barrier()
    cx.pop()

    cm = {}

    def alloc_common():
        cm["xt"] = cx.sb("xt", [128, 8, TT], F32)
        cm["sq"] = cx.sb("sq", [128, 8, TT], BF16)
        cm["rstd"] = cx.sb("rstd", [128, TT], F32)
        cm["u"] = cx.sb("u", [128, 8, TT], BF16)
        cm["hh"] = cx.sb("hh", [128, 11, TT], BF16)
        cm["tmpr"] = Ring([cx.sb("tmpf%d" % i, [128, TT], F32) for i in range(2)])
        cm["wring"] = Ring([cx.sb("wsl%d" % i, [128, 11 * 512], BF16) for i in range(3)])

    def V(l, off, n=1):
        return vecs[:, l * NV_L + off: l * NV_L + off + n]

    def wload(wname, l, r0, r1, c0, c1):
        sl = cm["wring"].next()
        kc = (r1 - r0 + 127) // 128
        ncol = c1 - c0
        rows = r1 - r0
        if rows % 128 == 0:
            dst = sl.t[:, 0:kc * ncol].rearrange("p (k c) -> p k c", k=kc)
            cx.dma("sp", dst, Wb[wname][l, r0:r1, c0:c1].rearrange("(k p) c -> p k c", p=128), writes=[sl])
        else:
            assert kc == 1
            dst = sl.t[0:rows, 0:ncol]
            cx.dma("sp", dst, Wb[wname][l, r0:r1, c0:c1], writes=[sl])

        def view(k, a, b):
            return sl.t[:, k * ncol + a: k * ncol + b]
        return sl, view

    def rmsnorm_fm(src, l, voff, dst, nch=8, dim=D, c_lo=0):
        sq, rstd = cm["sq"], cm["rstd"]
        cx.op("act", lambda e: e.activation(out=sq[:, 0:nch, :], in_=src[:, c_lo:c_lo + nch, :], func=AF.Square), reads=[src], writes=[sq])
        p = pring.next()

        def mm(e):
            ins = None
            for c in range(nch):
                ins = e.matmul(p[:], ONESB, sq[:, c, :], start=(c == 0), stop=(c == nch - 1))
            return ins
        cx.op("pe", mm, reads=[sq, cb16], writes=[p])
        cx.op("act", lambda e: e.activation(out=rstd[:], in_=p[:], func=AF.Sqrt, scale=1.0 / dim, bias=EPSB[:, 0:1]), reads=[p, epsb], writes=[rstd])
        cx.op("dve", lambda e: e.reciprocal(out=rstd[:], in_=rstd[:]), reads=[rstd], writes=[rstd])
        for c in range(nch):
            cx.op("dve", lambda e, c=c: e.scalar_tensor_tensor(out=dst[:, c_lo + c, :], in0=src[:, c_lo + c, :], scalar=V(l, voff + c), in1=rstd[:],
                                                               op0=ALU.mult, op1=ALU.mult), reads=[src, rstd, vecs], writes=[dst])

    epsb = cx.sb("epsb", [128, 1], F32)
    EPSB = epsb
    cx.op("dve", lambda e: e.memset(epsb[:], EPS), writes=[epsb])

    def linear_fm(wname, l, K, c0, c1, src, epi, group=512, wrows=None):
        kc = K // 128
        g0 = c0
        m = 0
        while g0 < c1:
            g1 = min(c1, g0 + group)
            sl, view = wload(wname, l, 0, K, g0, g1)
            a = 0
            while a < g1 - g0:
                mw = min(128, g1 - g0 - a)
                p = pring.next()

                def mm(e, a=a, mw=mw, p=p, view=view):
                    ins = None
                    for k in range(kc):
                        ins = e.matmul(p[0:mw, :], view(k, a, a + mw), src[:, k, :], start=(k == 0), stop=(k == kc - 1))
                    return ins
                cx.op("pe", mm, reads=[sl, src], writes=[p])
                epi(p, m, mw)
                m += 1
                a += mw
            g0 = g1

    def ffn(l, wi, wo, normoff):
        xt, u, hh, tmpr = cm["xt"], cm["u"], cm["hh"], cm["tmpr"]
        rmsnorm_fm(xt, l, normoff, u)
        for g0 in range(0, FF, 512):
            g1 = min(FF, g0 + 512)
            sla, va = wload(wi, l, 0, D, g0, g1)
            slg, vg = wload(wi, l, 0, D, FF + g0, FF + g1)
            for a in range(0, g1 - g0, 128):
                j = (g0 + a) // 128
                pa = pring.next()
                pg = pring.next()

                def mm(e, a=a, pa=pa, va=va):
                    ins = None
                    for k in range(8):
                        ins = e.matmul(pa[:], va(k, a, a + 128), u[:, k, :], start=(k == 0), stop=(k == 7))
                    return ins
                cx.op("pe", mm, reads=[sla, u], writes=[pa])

                def mm2(e, a=a, pg=pg, vg=vg):
                    ins = None
                    for k in range(8):
                        ins = e.matmul(pg[:], vg(k, a, a + 128), u[:, k, :], start=(k == 0), stop=(k == 7))
                    return ins
                cx.op("pe", mm2, reads=[slg, u], writes=[pg])
                t = tmpr.next()
                cx.op("act", lambda e, t=t, pa=pa: e.activation(out=t[:], in_=pa[:], func=AF.Silu), reads=[pa], writes=[t])
                cx.op("dve", lambda e, t=t, pg=pg, j=j: e.tensor_tensor(out=hh[:, j, :], in0=t[:], in1=pg[:], op=ALU.mult), reads=[t, pg], writes=[hh])
        for g0 in range(0, D, 512):
            sl, view = wload(wo, l, 0, FF, g0, g0 + 512)
            for a in range(0, 512, 128):
                m = (g0 + a) // 128
                p = pring.next()

                def mm(e, a=a, p=p, view=view):
                    ins = None
                    for k in range(11):
                        ins = e.matmul(p[:], view(k, a, a + 128), hh[:, k, :], start=(k == 0), stop=(k == 10))
                    return ins
                cx.op("pe", mm, reads=[sl, hh], writes=[p])
                cx.op("dve", lambda e, p=p, m=m: e.scalar_tensor_tensor(out=xt[:, m, :], in0=p[:], scalar=0.5, in1=xt[:, m, :], op0=ALU.mult, op1=ALU.add),
                      reads=[p, xt], writes=[xt])

    def p1(l, first):
        cx.push()
        alloc_common()
        xt, tmpr = cm["xt"], cm["tmpr"]
        u2 = cx.sb("u2", [128, 8, TT], BF16)
        st_qk = cx.sb("st_qk", [128, 4, TT], BF16)
        st_tok = cx.sb("st_tok", [128, 4, 1024], BF16)
        st_if = cx.sb("st_if", [128, 4, 8], F32)
        brow = cx.sb("brow", [128, 776], F32)
        xc = cx.sb("xc", [128, 8, 3 + TT], F32)
        cacc = cx.sb("cacc", [128, TT], F32)
        st_mqk = cx.sb("st_mqk", [128, 8, TT], BF16)
        st_o = cx.sb("st_o", [128, 4, TT], BF16)
        st_g = cx.sb("st_g", [128, 8, TT], BF16)
        cqf = cx.sb("cqf", [128, 3, TT], F32)
        cqn = cx.sb("cqn", [128, 3, TT], BF16)
        qn = cx.sb("qn", [96, TT], F32)
        qsq = cx.sb("qsq", [96, TT], F32)
        rtmp = cx.sb("rtmp", [96, TT], F32)
        qb = cx.sb("qb", [96, 8, TT], BF16)
        cst = cx.sb("cst", [96, TT], F32)
        snt = cx.sb("snt", [96, TT], F32)
        wkpad = cx.sb("wkpad", [128, 4, 96], BF16)
        wkr = cx.sb("wkr", [128, 8, 96], BF16)
        wqu = cx.sb("wqu", [128, 2, 384], BF16)
        wkvv = cx.sb("wkvv", [128, 256], BF16)
        et = cx.sb("et", [128, 4, 4], F32)

        src_x = xT_in if first else xres
        cx.op("pool", lambda e: e.memset(wkpad[:], 0.0), writes=[wkpad])
        cx.op("pool", lambda e: e.memset(wkr[:], 0.0), writes=[wkr])
        for h in range(4):
            cx.dma("sp", wkpad[:, h, 0:64], Wb["mla_wkv_up"][l, :, h * 128:h * 128 + 64], writes=[wkpad])
            cx.dma("sp", wkvv[:, h * 64:(h + 1) * 64], Wb["mla_wkv_up"][l, :, h * 128 + 64:h * 128 + 128], writes=[wkvv])
        cx.dma("sp", wkr[:, :, 64:96], Wb["w_in"][l, :, O_KR:O_KR + 32].rearrange("(k p) c -> p k c", p=128), writes=[wkr])
        cx.dma("sp", wqu[:], Wb["mla_wq_up"][l].rearrange("(k p) c -> p k c", p=128), writes=[wqu])
        cx.dma("sp", brow[:, 0:256], b_in_d[l:l + 1, O_SBV:O_SBV + 256].partition_broadcast(128), writes=[brow])
        cx.dma("sp", brow[:, 256:768], b_in_d[l:l + 1, O_MLV:O_MLV + 512].partition_broadcast(128), writes=[brow])
        cx.dma("sp", brow[:, 768:776], b_in_d[l:l + 1, O_MLI:O_MLI + 8].partition_broadcast(128), writes=[brow])
        cx.op("dve", lambda e: e.memset(xc[:, :, 0:3], 0.0), writes=[xc])
        for t in range(NT):
            t0 = t * TT
            cx.dma("sp", xt[:], src_x[:, t0:t0 + TT].rearrange("(c p) t -> p c t", p=128), writes=[xt])
            ffn(l, "ffn1_wi", "ffn1_wo", 0)
            cx.dma("pool", xres[:, t0:t0 + TT].rearrange("(c p) t -> p c t", p=128), xt[:], reads=[xt])
            rmsnorm_fm(xt, l, 8, u2)
            def epi_qk(p, m, mw, base=0, boff=24):
                cx.op("act", lambda e: e.activation(out=st_qk[:, base + m, :], in_=p[:], func=AF.Identity, bias=V(l, boff + m)),
                      reads=[p, vecs], writes=[st_qk])
            linear_fm("w_in", l, D, O_SBQ, O_SBQ + 256, u2, lambda p, m, mw: epi_qk(p, m, mw, 0, 24))
            linear_fm("w_in", l, D, O_SBK, O_SBK + 256, u2, lambda p, m, mw: epi_qk(p, m, mw, 2, 26))
            cx.dma("pool", sbq[:, t0:t0 + TT].rearrange("(c p) t -> p c t", p=128), st_qk[:, 0:2, :], reads=[st_qk])
            cx.dma("pool", sbk[:, t0:t0 + TT].rearrange("(c p) t -> p c t", p=128), st_qk[:, 2:4, :], reads=[st_qk])
            def epi_c(p, m, mw, base, boff):
                cx.op("act", lambda e: e.activation(out=xc[:, base + m, 3:3 + TT], in_=p[:], func=AF.Identity, bias=V(l, boff + m)),
                      reads=[p, vecs], writes=[xc])
            linear_fm("w_in", l, D, O_MLQ, O_MLQ + 512, u2, lambda p, m, mw: epi_c(p, m, mw, 0, 28))
            linear_fm("w_in", l, D, O_MLK, O_MLK + 512, u2, lambda p, m, mw: epi_c(p, m, mw, 4, 32))
            for c in range(8):
                cx.op("dve", lambda e, c=c: e.tensor_scalar(out=cacc[:], in0=xc[:, c, 0:TT], scalar1=V(l, 68 + c * 4 + 0), scalar2=None, op0=ALU.mult),
                      reads=[xc, vecs], writes=[cacc])
                for j in range(1, 4):
                    cx.op("dve", lambda e, c=c, j=j: e.scalar_tensor_tensor(out=cacc[:], in0=xc[:, c, j:j + TT], scalar=V(l, 68 + c * 4 + j), in1=cacc[:],
                                                                        op0=ALU.mult, op1=ALU.add), reads=[xc, vecs, cacc], writes=[cacc])
                if c < 4:
                    cx.op("act", lambda e, c=c: e.activation(out=st_mqk[:, c, :], in_=cacc[:], func=AF.Silu, bias=V(l, 100 + c)),
                          reads=[cacc, vecs], writes=[st_mqk])
                else:
                    tq = tmpr.next()
                    cx.op("act", lambda e, c=c, tq=tq: e.activation(out=tq[:], in_=cacc[:], func=AF.Silu, bias=V(l, 100 + c)),
                          reads=[cacc, vecs], writes=[tq])
                    cx.op("dve", lambda e, c=c, tq=tq: e.tensor_scalar(out=st_mqk[:, c, :], in0=tq[:], scalar1=128.0 ** -0.5, scalar2=None, op0=ALU.mult),
                          reads=[tq], writes=[st_mqk])
            cx.op("dve", lambda e: e.tensor_copy(out=xc[:, :, 0:3], in_=xc[:, :, TT:TT + 3]), reads=[xc], writes=[xc])
            cx.dma("pool", mlq[:, t0:t0 + TT].rearrange("(c p) t -> p c t", p=128), st_mqk[:, 0:4, :], reads=[st_mqk])
            cx.dma("pool", mlk[:, t0:t0 + TT].rearrange("(c p) t -> p c t", p=128), st_mqk[:, 4:8, :], reads=[st_mqk])
            def epi_o(p, m, mw):
                cx.op("act", lambda e: e.activation(out=st_o[:, m, :], in_=p[:], func=AF.Sigmoid, bias=V(l, 36 + m)), reads=[p, vecs], writes=[st_o])
            linear_fm("w_in", l, D, O_MLO, O_MLO + 512, u2, epi_o)
            cx.dma("pool", mlo[:, t0:t0 + TT].rearrange("(c p) t -> p c t", p=128), st_o[:], reads=[st_o])
            for gg in range(3):
                def epi_g(p, m, mw, gg=gg):
                    cx.op("act", lambda e: e.activation(out=st_g[:, m, :], in_=p[:], func=AF.Sigmoid, bias=V(l, 43 + gg * 8 + m)), reads=[p, vecs], writes=[st_g])
                linear_fm("w_in", l, D, O_G + gg * 1024, O_G + (gg + 1) * 1024, u2, epi_g)
                cx.dma("pool", gat[gg * 1024:(gg + 1) * 1024, t0:t0 + TT].rearrange("(c p) t -> p c t", p=128), st_g[:], reads=[st_g])
            slv, vv = wload("w_in", l, 0, D, O_SBV, O_SBV + 256)
            slm, vm = wload("w_in", l, 0, D, O_MLV, O_MLV + 512)
            slg, vgt = wload("w_in", l, 0, D, O_MLI, O_MLI + 8)
            for s4 in range(4):
                ts_ = slice(s4 * 128, (s4 + 1) * 128)
                for (view, sl, ncol, so, bo) in ((vv, slv, 256, 0, 0), (vm, slm, 512, 256, 256)):
                    p = pring.next()

                    def mm(e, p=p, view=view, ncol=ncol, ts_=ts_):
                        ins = None
                        for k in range(8):
                            ins = e.matmul(p[:, 0:ncol], u2[:, k, ts_], view(k, 0, ncol), start=(k == 0), stop=(k == 7))
                        return ins
                    cx.op("pe", mm, reads=[sl, u2], writes=[p])
                    cx.op("dve", lambda e, p=p, ncol=ncol, so=so, bo=bo, s4=s4: e.tensor_tensor(out=st_tok[:, s4, so:so + ncol], in0=p[:, 0:ncol], in1=brow[:, bo:bo + ncol], op=ALU.add),
                          reads=[p, brow], writes=[st_tok])
                p = pring.next()

                def mm(e, p=p, ts_=ts_):
                    ins = None
                    for k in range(8):
                        ins = e.matmul(p[:, 0:8], u2[:, k, ts_], vgt(k, 0, 8), start=(k == 0), stop=(k == 7))
                    return ins
                cx.op("pe", mm, reads=[slg, u2], writes=[p])
                cx.op("dve", lambda e, p=p, s4=s4: e.tensor_tensor(out=st_if[:, s4, :], in0=p[:, 0:8], in1=brow[:, 768:776], op=ALU.add),
                      reads=[p, brow], writes=[st_if])
            cx.op("act", lambda e: e.activation(out=et[:], in_=st_if[:, :, 4:8], func=AF.Exp, scale=-1.0), reads=[st_if], writes=[et])
            cx.op("act", lambda e: e.activation(out=et[:], in_=et[:], func=AF.Ln, bias=1.0), reads=[et], writes=[et])
            cx.op("dve", lambda e: e.tensor_scalar(out=st_if[:, :, 4:8], in0=et[:], scalar1=-1.0, scalar2=None, op0=ALU.mult), reads=[et], writes=[st_if])
            def epi_cq(p, m, mw, base, boff):
                cx.op("act", lambda e: e.activation(out=cqf[:, base + m, :], in_=p[:], func=AF.Identity, bias=V(l, boff + m)), reads=[p, vecs], writes=[cqf])
            linear_fm("w_in", l, D, O_CQ, O_CQ + 256, u2, lambda p, m, mw: epi_cq(p, m, mw, 0, 40))
            linear_fm("w_in", l, D, O_CKV, O_CKV + 128, u2, lambda p, m, mw: epi_cq(p, m, mw, 2, 42))
            rmsnorm_fm(cqf, l, 112, cqn, nch=2, dim=256, c_lo=0)
            rmsnorm_fm(cqf, l, 114, cqn, nch=1, dim=128, c_lo=2)
            cx.dma("sp", cst[64:96, :], cosd[:, t0:t0 + TT], writes=[cst])
            cx.dma("sp", snt[64:96, :], sind[:, t0:t0 + TT], writes=[snt])

            def norm_rope(p, gcol, dst_i, bias):
                if bias is None:
                    cx.op("act", lambda e: e.activation(out=qn[:], in_=p[0:96, :], func=AF.Copy), reads=[p], writes=[qn])
                else:
                    cx.op("act", lambda e: e.activation(out=qn[:], in_=p[0:96, :], func=AF.Identity, bias=bias), reads=[p, vecs], writes=[qn])
                cx.op("act", lambda e: e.activation(out=qsq[:], in_=qn[:], func=AF.Square), reads=[qn], writes=[qsq])
                p2 = pring.next()
                cx.op("pe", lambda e: e.matmul(p2[0:96, :], ones32[0:96, 0:96], qsq[:], start=True, stop=True), reads=[qsq, ones32], writes=[p2])
                cx.op("act", lambda e: e.activation(out=rtmp[:], in_=p2[0:96, :], func=AF.Sqrt, scale=1.0 / 96, bias=EPSB[0:96, 0:1]), reads=[p2, epsb], writes=[rtmp])
                cx.op("dve", lambda e: e.reciprocal(out=rtmp[:], in_=rtmp[:]), reads=[rtmp], writes=[rtmp])
                cx.op("dve", lambda e: e.scalar_tensor_tensor(out=qn[:], in0=qn[:], scalar=gcol, in1=rtmp[:], op0=ALU.mult, op1=ALU.mult),
                      reads=[qn, rtmp, vecs], writes=[qn])
                p3 = pring.next()
                cx.op("pe", lambda e: e.matmul(p3[0:96, :], cc("rot")[0:96, :], qn[:], start=True, stop=True), reads=[qn, consts], writes=[p3])
                cx.op("dve", lambda e: e.tensor_tensor(out=rtmp[64:96, :], in0=p3[64:96, :], in1=snt[64:96, :], op=ALU.mult), reads=[p3, snt], writes=[rtmp])
                cx.op("dve", lambda e: e.tensor_tensor(out=qn[64:96, :], in0=qn[64:96, :], in1=cst[64:96, :], op=ALU.mult), reads=[qn, cst], writes=[qn])
                cx.op("dve", lambda e: e.tensor_tensor(out=qn[64:96, :], in0=qn[64:96, :], in1=rtmp[64:96, :], op=ALU.add), reads=[qn, rtmp], writes=[qn])
                cx.op("act", lambda e: e.activation(out=qb[:, dst_i, :], in_=qn[:], func=AF.Copy), reads=[qn], writes=[qb])

            for h in range(4):
                p = pring.next()

                def mmq(e, p=p, h=h):
                    e.matmul(p[0:96, :], wqu[:, 0, h * 96:(h + 1) * 96], cqn[:, 0, :], start=True, stop=False)
                    return e.matmul(p[0:96, :], wqu[:, 1, h * 96:(h + 1) * 96], cqn[:, 1, :], start=False, stop=True)
                cx.op("pe", mmq, reads=[wqu, cqn], writes=[p])
                norm_rope(p, V(l, 115)[0:96, :], h, None)
                p = pring.next()

                def mmk(e, p=p, h=h):
                    e.matmul(p[0:96, :], wkpad[:, h, :], cqn[:, 2, :], start=True, stop=False)
                    ins = None
                    for k in range(8):
                        ins = e.matmul(p[0:96, :], wkr[:, k, :], u2[:, k, :], start=False, stop=(k == 7))
                    return ins
                cx.op("pe", mmk, reads=[wkpad, wkr, cqn, u2], writes=[p])
                norm_rope(p, V(l, 116)[0:96, :], 4 + h, V(l, 67)[0:96, :])
            cx.dma("pool", mlaq[:, :, t0:t0 + TT].rearrange("h p t -> p h t"), qb[:, 0:4, :], reads=[qb])
            cx.dma("pool", mlak[:, :, t0:t0 + TT].rearrange("h p t -> p h t"), qb[:, 4:8, :], reads=[qb])
            for s4 in range(4):
                ts_ = slice(s4 * 128, (s4 + 1) * 128)
                p = pring.next()
                cx.op("pe", lambda e, p=p, ts_=ts_: e.matmul(p[:, 0:256], cqn[:, 2, ts_], wkvv[:], start=True, stop=True), reads=[cqn, wkvv], writes=[p])
                cx.op("act", lambda e, p=p, s4=s4: e.activation(out=st_tok[:, s4, 768:1024], in_=p[:, 0:256], func=AF.Copy), reads=[p], writes=[st_tok])
            cx.dma("pool", sbv[t0:t0 + TT, :].rearrange("(s p) c -> p s c", p=128), st_tok[:, :, 0:256], reads=[st_tok])
            cx.dma("pool", mlv[t0:t0 + TT, :].rearrange("(s p) c -> p s c", p=128), st_tok[:, :, 256:768], reads=[st_tok])
            cx.dma("pool", mlav[t0:t0 + TT, :].rearrange("(s p) c -> p s c", p=128), st_tok[:, :, 768:1024], reads=[st_tok])
            cx.dma("pool", mlif[t0:t0 + TT, :].rearrange("(s p) c -> p s c", p=128), st_if[:], reads=[st_if])
        cx.barrier()
        cx.pop()

    def p23(l):
        cx.push()
        KT = cx.sb("KT", [96, S], BF16)
        VV = cx.sb("VV", [128, NB, 64], BF16)
        QTr = Ring([cx.sb("QT%d" % i, [96, TT], BF16) for i in range(2)])
        er = Ring([cx.sb("e%d" % i, [128, TT], F32) for i in range(3)])
        spr = Ring([cx.sb("sp%d" % i, [128, TT], F32) for i in range(4)])
        lkr = Ring([cx.sb("lk%d" % i, [128, TT], BF16) for i in range(4)])
        ar = Ring([cx.sb("a%d" % i, [128, TT], F32) for i in range(3)])
        wr = Ring([cx.sb("w%d" % i, [128, TT], BF16) for i in range(4)])
        CS = cx.sb("CS", [128, TT], F32)
        obr = Ring([cx.sb("ob%d" % i, [64, TT], BF16) for i in range(2)])
        recb = cx.sb("recb", [64, TT], F32)
        ring5 = Ring(pbanks[0:5])
        pacc = [pbanks[5], pbanks[6]]

        def run_pipe_(units, nst):
            n = len(units)
            for i in range(n + nst - 1):
                for s in range(nst):
                    k = i - s
                    if 0 <= k < n:
                        units[k][s]()

        def p2_sb(l):
            for h in range(4):
                cx.dma("sp", KT[0:64, :], sbk[h * 64:(h + 1) * 64, :], writes=[KT])
                cx.dma("sp", VV[:], sbv[:, h * 64:(h + 1) * 64].rearrange("(n p) c -> p n c", p=128), writes=[VV])
                units = []
                for T in range(NT):
                    QT = QTr.next()
                    po = pacc[T % 2]
                    nlist = list(range(4 * T + 3, -1, -1))
                    for ui, n in enumerate(nlist):
                        st = {}
                        first = ui == 0
                        last = ui == len(nlist) - 1
                        jj = n - 4 * T

                        def A(st=st, n=n, first=first, QT=QT, T=T, jj=jj):
                            if first:
                                cx.dma("sp", QT[0:64, :], sbq[h * 64:(h + 1) * 64, T * TT:(T + 1) * TT], writes=[QT])
                            pz = ring5.next()
                            cx.op("pe", lambda e: e.matmul(pz[:], KT[0:64, n * 128:(n + 1) * 128], QT[0:64, :], start=True, stop=True), reads=[KT, QT], writes=[pz])
                            et_ = er.next()
                            spt = spr.next()
                            lkb = lkr.next()
                            cx.op("act", lambda e: e.activation(out=et_[:], in_=pz[:], func=AF.Exp, scale=-0.125), reads=[pz], writes=[et_])
                            cx.op("act", lambda e: e.activation(out=spt[:], in_=et_[:], func=AF.Ln, bias=1.0), reads=[et_], writes=[spt])
                            cx.op("dve", lambda e: e.scalar_tensor_tensor(out=lkb[:], in0=pz[:], scalar=-0.125, in1=spt[:], op0=ALU.mult, op1=ALU.subtract),
                                  reads=[pz, spt], writes=[lkb])
                            if jj >= 0:
                                cx.op("dve", lambda e: e.tensor_tensor(out=lkb[:], in0=lkb[:], in1=SBMB(jj), op=ALU.mult), reads=[lkb, cb16], writes=[lkb])
                            st["sp"] = spt
                            st["lk"] = lkb

                        def B(st=st, jj=jj, first=first):
                            if first:
                                cx.op("dve", lambda e: e.memset(CS[:], 0.0), writes=[CS])
                            pl = ring5.next()
                            pc = ring5.next()
                            lkb = st["lk"]
                            spt = st["sp"]
                            cx.op("pe", lambda e: e.matmul(pl[:], TRIB, lkb[:], start=True, stop=True), reads=[lkb, cb16], writes=[pl])
                            cx.op("pe", lambda e: e.matmul(pc[:], ONESB, lkb[:], start=True, stop=True), reads=[lkb, cb16], writes=[pc])
                            at = ar.next()
                            cx.op("dve", lambda e: e.tensor_tensor(out=at[:], in0=pl[:], in1=spt[:], op=ALU.subtract), reads=[pl, spt], writes=[at])
                            cx.op("dve", lambda e: e.tensor_tensor(out=at[:], in0=at[:], in1=CS[:], op=ALU.add), reads=[at, CS], writes=[at])
                            if jj >= 0:
                                cx.op("dve", lambda e: e.tensor_tensor(out=at[:], in0=at[:], in1=cc("sbn%d" % jj), op=ALU.add), reads=[at, consts], writes=[at])
                            cx.op("dve", lambda e: e.tensor_tensor(out=CS[:], in0=CS[:], in1=pc[:], op=ALU.add), reads=[CS, pc], writes=[CS])
                            wt = wr.next()
                            cx.op("act", lambda e: e.activation(out=wt[:], in_=at[:], func=AF.Exp), reads=[at], writes=[wt])
                            st["w"] = wt

                        def C(st=st, n=n, first=first, last=last, po=po, T=T):
                            wt = st["w"]
                            cx.op("pe", lambda e: e.matmul(po[0:64, :], VV[:, n, :], wt[:], start=first, stop=last), reads=[VV, wt], writes=[po])
                            if last:
                                ob = obr.next()
                                cx.op("act", lambda e: e.activation(out=ob[:], in_=po[0:64, :], func=AF.Copy), reads=[po], writes=[ob])
                                cx.dma("pool", ysb[h * 64:(h + 1) * 64, T * TT:(T + 1) * TT], ob[:], reads=[ob])
                        units.append((A, B, C))
                run_pipe_(units, 3)
            cx.barrier()

        def p3_mla(l):
            sc = 96.0 ** -0.5
            for h in range(4):
                cx.dma("sp", KT[:], mlak[h], writes=[KT])
                cx.dma("sp", VV[:], mlav[:, h * 64:(h + 1) * 64].rearrange("(n p) c -> p n c", p=128), writes=[VV])
                units = []
                for T in range(NT):
                    QT = QTr.next()
                    nlist = list(range(0, 4 * T + 4))
                    for ui, n in enumerate(nlist):
                        st = {}
                        first = ui == 0
                        last = ui == len(nlist) - 1
                        jj = n - 4 * T

                        def A(st=st, n=n, first=first, QT=QT, T=T, jj=jj):
                            if first:
                                cx.dma("sp", QT[:], mlaq[h, :, T * TT:(T + 1) * TT], writes=[QT])
                            pz = ring5.next()
                            cx.op("pe", lambda e: e.matmul(pz[:], KT[:, n * 128:(n + 1) * 128], QT[:], start=True, stop=True), reads=[KT, QT], writes=[pz])
                            wt = wr.next()
                            cx.op("act", lambda e: e.activation(out=wt[:], in_=pz[:], func=AF.Exp, scale=sc), reads=[pz], writes=[wt])
                            if jj >= 0:
                                cx.op("dve", lambda e: e.tensor_tensor(out=wt[:], in0=wt[:], in1=MLAMB(jj), op=ALU.mult), reads=[wt, cb16], writes=[wt])
                            st["w"] = wt

                        def B(st=st, n=n, first=first, last=last, T=T):
                            wt = st["w"]
                            cx.op("pe", lambda e: e.matmul(pacc[0][0:64, :], VV[:, n, :], wt[:], start=first, stop=last), reads=[VV, wt], writes=[pacc[0]])
                            cx.op("pe", lambda e: e.matmul(pacc[1][0:64, :], ONESB[:, 0:64], wt[:], start=first, stop=last), reads=[cb16, wt], writes=[pacc[1]])
                            if last:
                                ob = obr.next()
                                cx.op("dve", lambda e: e.reciprocal(out=recb[:], in_=pacc[1][0:64, :]), reads=[pacc[1]], writes=[recb])
                                cx.op("dve", lambda e: e.tensor_tensor(out=ob[:], in0=pacc[0][0:64, :], in1=recb[:], op=ALU.mult), reads=[pacc[0], recb], writes=[ob])
                                cx.dma("pool", ymla[h * 64:(h + 1) * 64, T * TT:(T + 1) * TT], ob[:], reads=[ob])
                        units.append((A, B))
                run_pipe_(units, 2)
            cx.barrier()

        p2_sb(l)
        p3_mla(l)
        cx.pop()

    def p4_ml(l):
        cx.push()
        if os.environ.get("P4_LIMIT"):
            cx.limit = int(os.environ["P4_LIMIT"])
        mQ = cx.sb("mQ", [128, 4, TT], BF16)
        mK = cx.sb("mK", [128, 4, TT], BF16)
        mV = cx.sb("mV", [128, 4, 512], BF16)
        mO = cx.sb("mO", [128, 4, TT], BF16)
        mIF = cx.sb("mIF", [128, 4, 8], F32)
        bcol = cx.sb("bcol", [128, 4], F32)
        wend = cx.sb("wend", [128, 4], F32)
        decay = cx.sb("decay", [128, 4], F32)
        Bm = cx.sb("Bm", [128, 4, 128], F32)
        Am = cx.sb("Am", [128, 4, 128], F32)
        Gt = cx.sb("Gt", [128, 4, 128], F32)
        Pm = cx.sb("Pm", [128, 4, 128], BF16)
        qa = cx.sb("qa", [128, 4, 128], BF16)
        kw = cx.sb("kw", [128, 4, 128], BF16)
        C32 = cx.sb("C32", [128, 4, 128], F32)
        Cb = cx.sb("Cb", [128, 4, 128], BF16)
        n32 = cx.sb("n32", [128, 4], F32)
        Nb = cx.sb("Nb", [128, 4, 128], BF16)
        dn = cx.sb("dn", [128, 4, 128], F32)
        HT = cx.sb("HT", [128, 4, 128], F32)
        hsq = cx.sb("hsq", [128, 4, 128], BF16)
        hr = cx.sb("hr", [128, 4, 128], F32)
        yst = cx.sb("yst", [128, 4, TT], BF16)
        onecol = cx.sb("onecol", [128, 1], BF16)
        psm = pbanks[4]

        cx.op("dve", lambda e: e.memset(C32[:], 0.0), writes=[C32])
        cx.op("dve", lambda e: e.memset(Cb[:], 0.0), writes=[Cb])
        cx.op("dve", lambda e: e.memset(n32[:], 0.0), writes=[n32])
        cx.op("dve", lambda e: e.memset(Nb[:], 0.0), writes=[Nb])
        cx.op("dve", lambda e: e.memset(onecol[:], 1.0), writes=[onecol])
        r4 = Ring(pbanks[0:4])
        for t in range(NT):
            t0 = t * TT
            cx.dma("sp", mQ[:], mlq[:, t0:t0 + TT].rearrange("(h p) t -> p h t", p=128), writes=[mQ])
            cx.dma("sp", mK[:], mlk[:, t0:t0 + TT].rearrange("(h p) t -> p h t", p=128), writes=[mK])
            cx.dma("sp", mV[:], mlv[t0:t0 + TT, :].rearrange("(s p) c -> p s c", p=128), writes=[mV])
            cx.dma("sp", mO[:], mlo[:, t0:t0 + TT].rearrange("(h p) t -> p h t", p=128), writes=[mO])
            cx.dma("sp", mIF[:], mlif[t0:t0 + TT, :].rearrange("(s p) c -> p s c", p=128), writes=[mIF])
            for c4 in range(4):
                cs = slice(c4 * 128, (c4 + 1) * 128)
                cx.op("pe", lambda e, c4=c4: e.matmul(psm[:, 0:4], cc("upper"), mIF[:, c4, 4:8], start=True, stop=True), reads=[consts, mIF], writes=[psm])
                cx.op("dve", lambda e, c4=c4: e.tensor_tensor(out=bcol[:], in0=mIF[:, c4, 0:4], in1=psm[:, 0:4], op=ALU.subtract), reads=[mIF, psm], writes=[bcol])
                cx.op("dve", lambda e, c4=c4: e.tensor_tensor(out=Bm[:], in0=cc("upper").unsqueeze(1).to_broadcast([128, 4, 128]),
                                                              in1=mIF[:, c4, 4:8].unsqueeze(2).to_broadcast([128, 4, 128]), op=ALU.mult),
                      reads=[consts, mIF], writes=[Bm])
                pR = r4.next()
                pR2 = r4.next()
                Bf = Bm[:].rearrange("p h t -> p (h t)")
                cx.op("pe", lambda e, pR=pR: e.matmul(pR[:], ones32[:], Bf, start=True, stop=True), reads=[ones32, Bm], writes=[pR])

                def mmR2(e, pR2=pR2):
                    e.matmul(pR2[:], ones32[:], Bf, start=True, stop=False)
                    return e.matmul(pR2[:], cc("ident"), cc("negm4"), start=False, stop=True)
                cx.op("pe", mmR2, reads=[ones32, Bm, consts], writes=[pR2])
                cx.op("act", lambda e, pR=pR: e.activation(out=Am[:].rearrange("p h t -> p (h t)"), in_=pR[:], func=AF.Exp), reads=[pR], writes=[Am])
                for h in range(4):
                    cx.op("act", lambda e, h=h, pR2=pR2: e.activation(out=Gt[:, h, :], in_=pR2[:, h * 128:(h + 1) * 128], func=AF.Exp, bias=bcol[:, h:h + 1]),
                          reads=[pR2, bcol], writes=[Gt])
                for h in range(4):
                    cx.op("act", lambda e, h=h, pR=pR: e.activation(out=decay[:, h:h + 1], in_=pR[:, h * 128 + 127:h * 128 + 128], func=AF.Exp), reads=[pR], writes=[decay])
                cx.op("act", lambda e: e.activation(out=wend[:], in_=bcol[:], func=AF.Exp), reads=[bcol], writes=[wend])
                cx.op("dve", lambda e: e.tensor_tensor(out=wend[:], in0=wend[:], in1=decay[:], op=ALU.mult), reads=[wend, decay], writes=[wend])
                pS = r4.next()

                def mmS(e, pS=pS, cs=cs):
                    ins = None
                    for h in range(4):
                        ins = e.matmul(pS[:, h * 128:(h + 1) * 128], mK[:, h, cs], mQ[:, h, cs], start=True, stop=True)
                    return ins
                cx.op("pe", mmS, reads=[mK, mQ], writes=[pS])
                cx.op("dve", lambda e, pS=pS: e.tensor_tensor(out=Pm[:].rearrange("p h t -> p (h t)"), in0=pS[:], in1=Gt[:].rearrange("p h t -> p (h t)"), op=ALU.mult),
                      reads=[pS, Gt], writes=[Pm])
                cx.op("dve", lambda e, cs=cs: e.tensor_tensor(out=qa[:], in0=mQ[:, :, cs], in1=Am[:], op=ALU.mult), reads=[mQ, Am], writes=[qa])
                pN = r4.next()
                pD = pbanks[5]

                def mmN(e, pN=pN, c4=c4):
                    ins = None
                    for h in range(4):
                        e.matmul(pN[:, h * 128:(h + 1) * 128], mV[:, c4, h * 128:(h + 1) * 128], Pm[:, h, :], start=True, stop=False)
                        ins = e.matmul(pN[:, h * 128:(h + 1) * 128], Cb[:, h, :], qa[:, h, :], start=False, stop=True)
                    return ins
                cx.op("pe", mmN, reads=[mV, Pm, Cb, qa], writes=[pN])

                def mmD(e):
                    ins = None
                    for h in range(4):
                        e.matmul(pD[:, h * 128:(h + 1) * 128], ONESB, Pm[:, h, :], start=True, stop=False)
                        ins = e.matmul(pD[:, h * 128:(h + 1) * 128], Nb[:, h, :], qa[:, h, :], start=False, stop=True)
                    return ins
                cx.op("pe", mmD, reads=[cb16, Pm, Nb, qa], writes=[pD])
                dnf = dn[:].rearrange("p h t -> p (h t)")
                cx.op("act", lambda e: e.activation(out=dnf, in_=pD[:], func=AF.Abs), reads=[pD], writes=[dn])
                cx.op("dve", lambda e: e.tensor_scalar(out=dnf, in0=dnf, scalar1=1.0, scalar2=None, op0=ALU.max), reads=[dn], writes=[dn])
                cx.op("dve", lambda e: e.reciprocal(out=dnf, in_=dnf), reads=[dn], writes=[dn])
                HTf = HT[:].rearrange("p h t -> p (h t)")
                cx.op("dve", lambda e, pN=pN: e.tensor_tensor(out=HTf, in0=pN[:], in1=dnf, op=ALU.mult), reads=[pN, dn], writes=[HT])
                cx.op("pe", lambda e, cs=cs: [e.transpose(psb[:, h * 128:(h + 1) * 128], mK[:, h, cs], IDB) for h in range(4)][-1], reads=[mK, cb16], writes=[psb])
                cx.op("dve", lambda e: e.tensor_tensor(out=kw[:], in0=psb[:, 0:512].rearrange("p (h t) -> p h t", h=4), in1=wend[:].unsqueeze(2).to_broadcast([128, 4, 128]), op=ALU.mult),
                      reads=[psb, wend], writes=[kw])
                pC = r4.next()

                def mmC(e, pC=pC, c4=c4):
                    ins = None
                    for h in range(4):
                        ins = e.matmul(pC[:, h * 128:(h + 1) * 128], kw[:, h, :], mV[:, c4, h * 128:(h + 1) * 128], start=True, stop=True)
                    return ins
                cx.op("pe", mmC, reads=[kw, mV], writes=[pC])
                cx.op("pe", lambda e: [e.matmul(psm[:, 4 + h:5 + h], kw[:, h, :], onecol[:], start=True, stop=True) for h in range(4)][-1], reads=[kw, onecol], writes=[psm])
                for h in range(4):
                    cx.op("dve", lambda e, h=h, pC=pC: e.scalar_tensor_tensor(out=C32[:, h, :], in0=C32[:, h, :], scalar=decay[:, h:h + 1], in1=pC[:, h * 128:(h + 1) * 128],
                                                                      op0=ALU.mult, op1=ALU.add), reads=[C32, decay, pC], writes=[C32])
                cx.op("dve", lambda e: e.tensor_tensor(out=n32[:], in0=n32[:], in1=decay[:], op=ALU.mult), reads=[n32, decay], writes=[n32])
                cx.op("dve", lambda e: e.tensor_tensor(out=n32[:], in0=n32[:], in1=psm[:, 4:8], op=ALU.add), reads=[n32, psm], writes=[n32])
                cx.op("act", lambda e: e.activation(out=Cb[:], in_=C32[:], func=AF.Copy), reads=[C32], writes=[Cb])
                cx.op("dve", lambda e: e.tensor_tensor(out=Nb[:], in0=ones32[:].unsqueeze(1).to_broadcast([128, 4, 128]), in1=n32[:].unsqueeze(2).to_broadcast([128, 4, 128]), op=ALU.mult),
                      reads=[ones32, n32], writes=[Nb])
                cx.op("act", lambda e: e.activation(out=hsq[:], in_=HT[:], func=AF.Square), reads=[HT], writes=[hsq])
                pQ = r4.next()
                cx.op("pe", lambda e, pQ=pQ: e.matmul(pQ[:], ONESB, hsq[:].rearrange("p h t -> p (h t)"), start=True, stop=True), reads=[cb16, hsq], writes=[pQ])
                hrf = hr[:].rearrange("p h t -> p (h t)")
                cx.op("act", lambda e, pQ=pQ: e.activation(out=hrf, in_=pQ[:], func=AF.Sqrt, scale=1.0 / 128, bias=EPSB[:, 0:1]), reads=[pQ, epsb], writes=[hr])
                cx.op("dve", lambda e: e.reciprocal(out=hrf, in_=hrf), reads=[hr], writes=[hr])
                for h in range(4):
                    cx.op("dve", lambda e, h=h: e.scalar_tensor_tensor(out=HT[:, h, :], in0=HT[:, h, :], scalar=V(l, 108 + h), in1=hr[:, h, :], op0=ALU.mult, op1=ALU.mult),
                          reads=[HT, hr, vecs], writes=[HT])
                cx.op("dve", lambda e, cs=cs: e.tensor_tensor(out=yst[:, :, cs], in0=HT[:], in1=mO[:, :, cs], op=ALU.mult), reads=[HT, mO], writes=[yst])
            cx.dma("pool", yml[:, t0:t0 + TT].rearrange("(h p) t -> p h t", p=128), yst[:], reads=[yst])
        cx.barrier()
        cx.pop()

    def p5(l, lastlayer):
        cx.push()
        alloc_common()
        xt = cm["xt"]
        yb = cx.sb("yb", [128, 8, TT], BF16)
        mg = cx.sb("mg", [128, 8, TT], BF16)
        macc = cx.sb("macc", [128, TT], F32)
        mtmp = cx.sb("mtmp", [128, TT], F32)

        st_g = cx.sb("gt", [128, 24, TT], BF16)
        for t in range(NT):
            t0 = t * TT
            cx.dma("sp", xt[:], xres[:, t0:t0 + TT].rearrange("(c p) t -> p c t", p=128), writes=[xt])
            cx.dma("sp", yb[:, 0:2, :], ysb[:, t0:t0 + TT].rearrange("(c p) t -> p c t", p=128), writes=[yb])
            cx.dma("sp", yb[:, 2:6, :], yml[:, t0:t0 + TT].rearrange("(c p) t -> p c t", p=128), writes=[yb])
            cx.dma("sp", yb[:, 6:8, :], ymla[:, t0:t0 + TT].rearrange("(c p) t -> p c t", p=128), writes=[yb])
            cx.dma("sp", st_g[:], gat[:, t0:t0 + TT].rearrange("(c p) t -> p c t", p=128), writes=[st_g])
            for g0 in range(0, D, 512):
                sls = [wload("w_up_sb", l, 0, 256, g0, g0 + 512), wload("w_up_ml", l, 0, 512, g0, g0 + 512), wload("w_up_mla", l, 0, 256, g0, g0 + 512)]
                for a in range(0, 512, 128):
                    m = (g0 + a) // 128
                    for bi, (kcn, yo) in enumerate(((2, 0), (4, 2), (2, 6))):
                        sl, view = sls[bi]
                        p = pring.next()

                        def mm(e, p=p, view=view, kcn=kcn, yo=yo, a=a):
                            ins = None
                            for k in range(kcn):
                                ins = e.matmul(p[:], view(k, a, a + 128), yb[:, yo + k, :], start=(k == 0), stop=(k == kcn - 1))
                            return ins
                        cx.op("pe", mm, reads=[sl, yb], writes=[p])
                        if bi == 0:
                            cx.op("dve", lambda e, p=p, m=m: e.tensor_tensor(out=macc[:], in0=p[:], in1=st_g[:, m, :], op=ALU.mult), reads=[p, st_g], writes=[macc])
                        else:
                            cx.op("dve", lambda e, p=p, m=m, bi=bi: e.tensor_tensor(out=mtmp[:], in0=p[:], in1=st_g[:, bi * 8 + m, :], op=ALU.mult), reads=[p, st_g], writes=[mtmp])
                            if bi == 1:
                                cx.op("dve", lambda e: e.tensor_tensor(out=macc[:], in0=macc[:], in1=mtmp[:], op=ALU.add), reads=[macc, mtmp], writes=[macc])
                            else:
                                cx.op("dve", lambda e, m=m: e.tensor_tensor(out=mg[:, m, :], in0=macc[:], in1=mtmp[:], op=ALU.add), reads=[macc, mtmp], writes=[mg])

            def epi_out(p, m, mw):
                cx.op("dve", lambda e: e.tensor_tensor(out=xt[:, m, :], in0=xt[:, m, :], in1=p[:], op=ALU.add), reads=[xt, p], writes=[xt])
            linear_fm("w_out", l, D, 0, D, mg, epi_out)
            ffn(l, "ffn2_wi", "ffn2_wo", 16)
            dst = yT_out if lastlayer else xres
            cx.dma("pool", dst[:, t0:t0 + TT].rearrange("(c p) t -> p c t", p=128), xt[:], reads=[xt])
        cx.barrier()
        cx.pop()

    for l in range(L):
        if "p1" in phases:
            p1(l, l == 0)
        if "p23" in phases:
            p23(l)
        if "p4" in phases:
            p4_ml(l)
        if "p5" in phases:
            p5(l, l == L - 1)
    if dbg is not None:
        src = {"sbq": sbq, "sbk": sbk, "sbv": sbv, "mlq": mlq, "mlk": mlk, "mlv": mlv, "mlo": mlo, "mlif": mlif, "mlaq": mlaq, "mlak": mlak,
               "mlav": mlav, "gat": gat, "ysb": ysb, "yml": yml, "ymla": ymla, "xres": xres, "cosd": cosd, "sind": sind}[dbg[0]]
        cx.dma("pool", dbg_out, src)
        cx.barrier()
    cx.barrier()
    return nc, cx.ninst, cx


NCONST = None
_CONSTS = None


def _get_consts():
    global NCONST, _CONSTS
    if _CONSTS is None:
        _CONSTS = build_consts()
        NCONST = _CONSTS[0].shape[1]
    return _CONSTS


WNAMES = ("ffn1_wi", "ffn1_wo", "w_in", "mla_wq_up", "mla_wkv_up", "w_up_sb", "w_up_ml", "w_up_mla", "w_out", "ffn2_wi", "ffn2_wo")


def make_in_map(inp, b, S, L):
    consts = _get_consts()
    m = {
        "xT": np.ascontiguousarray(inp["x"][b, :S].T),
        "pos": np.ascontiguousarray(inp["positions"][b:b + 1, :S]).astype(np.int32),
        "consts": consts[0],
        "masks": consts[1],
        "vecs": build_vecs(inp, L),
        "b_in": np.ascontiguousarray(inp["b_in"][:L]),
    }
    for k in WNAMES:
        m[k] = np.ascontiguousarray(inp[k][:L])
    return m


def kernel(**inputs):
    inp = {k: np.asarray(v) for k, v in inputs.items()}
    B, S, _ = inp["x"].shape
    L = inp["w_in"].shape[0]
    _get_consts()
    nc, ninst, cx = build_program(S, L)
    vec = build_vecs(inp, L)
    in_maps = []
    for b in range(B):
        m = make_in_map(inp, b, S, L)
        m["vecs"] = vec
        in_maps.append(m)
    res = run_bass_kernel_spmd(nc, in_maps, core_ids=list(range(B)))
    out = np.stack([np.ascontiguousarray(res.results[b]["yT"].T) for b in range(B)], axis=0)
    return out.astype(np.float32)
```

```python
from contextlib import ExitStack
import numpy as np
import concourse.bass as bass
import concourse.mybir as mybir
from concourse.bass_utils import run_bass_kernel_spmd

F32 = mybir.dt.float32
BF16 = mybir.dt.bfloat16
I32 = mybir.dt.int32
AF = mybir.ActivationFunctionType
ALU = mybir.AluOpType

D = 1024
FF = 1408
NIN = 6312
O_SBQ, O_SBK, O_SBV = 0, 256, 512
O_MLQ, O_MLK, O_MLV, O_MLO, O_MLI, O_MLF = 768, 1280, 1792, 2304, 2816, 2820
O_CQ, O_CKV, O_KR, O_G = 2824, 3080, 3208, 3240
EPS = 1e-6
TT = 512
NV_L = 120
CAST_ELEMS = 1 << 16
TWO_PI = 6.283185307179586
C1 = 6.28125
C2 = TWO_PI - C1


class Chan:
    def __init__(self, sem):
        self.sem = sem
        self.count = 0


class Buf:
    def __init__(self, ctx, t, name):
        self.ctx = ctx
        self.t = t
        self.name = name
        self.lw = None
        self.rd = []
        self.lchan = None
        self.schan = None

    def __getitem__(self, k):
        return self.t[k]


class Op:
    __slots__ = ("eng", "fn", "deps", "signal", "idx", "epoch")

    def __init__(self, eng, fn, deps):
        self.eng = eng
        self.fn = fn
        self.deps = deps
        self.signal = False
        self.idx = 0
        self.epoch = 0


class Ctx:
    ENG = ("pe", "act", "dve", "pool", "sp")

    def __init__(self, nc, es):
        self.nc = nc
        self.es = es
        self.e = {"pe": nc.tensor, "act": nc.scalar, "dve": nc.vector, "pool": nc.gpsimd, "sp": nc.sync}
        self.ops = []
        self.last = {k: None for k in self.ENG}
        self.bufs = []
        self.chans = []
        self.free_chans = []
        self.stacks = [es]
        self.scope_bufs = [[]]
        self.nsem = 0
        self.ninst = 0
        self.limit = None
        self.cnt = {k: 0 for k in self.ENG}
        self.epoch = {k: 0 for k in self.ENG}
        self.sems = None
        self.seen_op = {k: {} for k in self.ENG}
        self.seen_ch = {k: {} for k in self.ENG}
        self.misc = self.new_chan("misc")
        self.out_chan = self.new_chan("outc")

    def sem(self, name):
        self.nsem += 1
        return self.es.enter_context(self.nc.semaphore(name))

    def new_chan(self, name):
        if self.free_chans:
            return self.free_chans.pop()
        c = Chan(self.sem("c_" + name))
        self.chans.append(c)
        return c

    def push(self):
        st = ExitStack()
        self.stacks.append(st)
        self.scope_bufs.append([])

    def pop(self):
        st = self.stacks.pop()
        for b in self.scope_bufs.pop():
            self.bufs.remove(b)
            for c in (b.lchan, b.schan):
                if c is not None:
                    self.free_chans.append(c)
        st.close()

    def sb(self, name, shape, dt):
        self.uid = getattr(self, "uid", 0) + 1
        b = Buf(self, self.stacks[-1].enter_context(self.nc.sbuf_tensor("%s_s%d" % (name, self.uid), list(shape), dt)), name)
        self.bufs.append(b)
        self.scope_bufs[-1].append(b)
        return b

    def ps(self, name, shape, dt):
        b = Buf(self, self.es.enter_context(self.nc.psum_tensor(name + "_p", list(shape), dt)), name)
        self.bufs.append(b)
        return b

    def _deps(self, reads, writes, eng=None):
        deps = []
        for b in reads:
            if b.lw is not None:
                deps.append(b.lw)
        for b in writes:
            if b.lw is not None and not (b.lw[0] == "op" and b.lw[1].eng == eng):
                deps.append(b.lw)
            for r in b.rd:
                if not (r[0] == "op" and r[1].eng == eng):
                    deps.append(r)
        return deps

    def op(self, eng, fn, reads=(), writes=()):
        if self.limit is not None:
            self.limit -= 1
            if self.limit < 0:
                return None
        deps = self._deps(reads, writes, eng)
        o = Op(eng, fn, deps)
        tok = ("op", o)
        for b in writes:
            b.lw = tok
            b.rd = []
        for b in reads:
            if b not in writes:
                b.rd.append(tok)
                if len(b.rd) > 64:
                    b.rd = b.rd[-48:]
        self.ops.append(o)
        self.last[eng] = o
        return o

    def dma(self, q, out_ap, in_ap, reads=(), writes=(), chan=None):
        deps = self._deps(reads, writes)
        chans = []
        for b in writes:
            if b.lchan is None:
                b.lchan = self.new_chan("l_" + b.name)
            chans.append(b.lchan)
        for b in reads:
            if b.schan is None:
                b.schan = self.new_chan("s_" + b.name)
            chans.append(b.schan)
        if chan is not None:
            chans.append(chan)
        if not chans:
            chans = [self.misc]
        ch = chans[0]
        assert len(chans) == 1, "dma must touch exactly one tracked buf"
        ch.count += 16
        tok = ("dma", ch, ch.count)

        def fn(e, out_ap=out_ap, in_ap=in_ap, ch=ch):
            return e.dma_start(out=out_ap, in_=in_ap).then_inc(ch.sem, 16)

        o = Op(q, fn, deps)
        o.signal = None
        for b in writes:
            b.lw = tok
            b.rd = []
        for b in reads:
            b.rd.append(tok)
        self.ops.append(o)
        return o

    def barrier(self):
        toks = []
        for k in self.ENG:
            if self.last[k] is not None:
                toks.append(("op", self.last[k]))
        for c in self.chans:
            if c.count:
                toks.append(("dma", c, c.count))
        for k in self.ENG:
            o = Op(k, None, list(toks))
            o.signal = None
            self.ops.append(o)
            o.fn = "barrier"
        for b in self.bufs:
            b.lw = None
            b.rd = []
        self.ops.append("epoch")
        self.emit()
        self.ops = []
        self.last = {k: None for k in self.ENG}

    def emit(self):
        for o in self.ops:
            if o == "epoch":
                continue
            for d in o.deps:
                if d[0] == "op" and (d[1].eng != o.eng or o.eng != "pe") and d[1].signal is not None:
                    d[1].signal = True
        cnt = self.cnt
        epoch = self.epoch
        if self.sems is None:
            self.sems = {k: [self.sem("e_%s_0" % k)] for k in self.ENG}
        sems = self.sems
        for o in self.ops:
            if o == "epoch":
                for k in self.ENG:
                    if cnt[k] > 20000:
                        epoch[k] += 1
                        cnt[k] = 0
                        sems[k].append(self.sem("e_%s_%d" % (k, epoch[k])))
                continue
            if o.signal is True:
                cnt[o.eng] += 1
                o.idx = cnt[o.eng]
                o.epoch = epoch[o.eng]
        seen_op = self.seen_op
        seen_ch = self.seen_ch
        ninst = 0
        for o in self.ops:
            if o == "epoch":
                continue
            e = self.e[o.eng]
            need_op = {}
            need_ch = {}
            for d in o.deps:
                if d[0] == "op":
                    s = d[1]
                    if s.eng == o.eng and o.eng == "pe":
                        continue
                    if s.signal is not True:
                        continue
                    key = (s.eng, s.epoch)
                    if seen_op[o.eng].get(key, 0) >= s.idx:
                        continue
                    need_op[key] = max(need_op.get(key, 0), s.idx)
                else:
                    _, ch, c = d
                    if seen_ch[o.eng].get(ch, 0) >= c:
                        continue
                    need_ch[ch] = max(need_ch.get(ch, 0), c)
            for key, v in need_op.items():
                e.wait_ge(sems[key[0]][key[1]], v)
                seen_op[o.eng][key] = v
                ninst += 1
            for ch, v in need_ch.items():
                e.wait_ge(ch.sem, v)
                seen_ch[o.eng][ch] = v
                ninst += 1
            if o.fn == "barrier":
                continue
            ins = o.fn(e)
            ninst += 1
            if o.signal is True:
                ins.then_inc(sems[o.eng][o.epoch], 1)
        self.ninst += ninst
        return ninst


class Ring:
    def __init__(self, bufs):
        self.bufs = bufs
        self.i = 0

    def next(self):
        b = self.bufs[self.i % len(self.bufs)]
        self.i += 1
        return b


CONST_COLS = {}


def build_consts():
    cols = []
    off = 0

    def add(name, arr):
        nonlocal off
        a = np.zeros((128, arr.shape[1]), np.float32)
        a[: arr.shape[0]] = arr
        CONST_COLS[name] = (off, arr.shape[1])
        off += arr.shape[1]
        cols.append(a)

    j = np.arange(128)[:, None]
    s = np.arange(128)[None, :]
    add("tri", (j > s).astype(np.float32))
    add("upper", (j <= s).astype(np.float32))
    add("ident", (j == s).astype(np.float32))
    negm = np.where(j > s, -30000.0, 0.0).astype(np.float32)
    add("negm4", np.tile(negm, (1, 4)))
    masks = []
    for jj in range(4):
        ms = np.zeros((128, 512), np.float32)
        mi = np.zeros((128, 512), np.float32)
        for jq in range(4):
            if jq > jj:
                ms[:, jq * 128:(jq + 1) * 128] = 1.0
                mi[:, jq * 128:(jq + 1) * 128] = 1.0
            elif jq == jj:
                ms[:, jq * 128:(jq + 1) * 128] = (j < s)
                mi[:, jq * 128:(jq + 1) * 128] = (j <= s)
        masks.append((ms, mi))
    rot = np.zeros((96, 96), np.float32)
    for i in range(16):
        rot[80 + i, 64 + i] = -1.0
        rot[64 + i, 80 + i] = 1.0
    add("rot", rot)
    invf = np.power(np.float32(10000.0), -np.arange(16, dtype=np.float32) / np.float32(16)).astype(np.float32)
    add("invf", np.concatenate([invf, invf])[:, None])
    mk = np.concatenate([m[0] for m in masks] + [m[1] for m in masks] + [(1.0 - m[0]) * -30000.0 for m in masks], axis=1)
    return np.concatenate(cols, axis=1), mk


def col_layout(v):
    n = v.shape[0] // 128
    return v.reshape(n, 128).T


def build_vecs(inp, L):
    out = np.zeros((128, L * NV_L), np.float32)
    for l in range(L):
        o = l * NV_L
        b = inp["b_in"][l]

        def put(off, arr):
            out[: arr.shape[0], o + off: o + off + arr.shape[1]] = arr

        put(0, col_layout(inp["ffn1_norm"][l]))
        put(8, col_layout(inp["mix_norm"][l]))
        put(16, col_layout(inp["ffn2_norm"][l]))
        put(24, col_layout(b[O_SBQ:O_SBQ + 256]))
        put(26, col_layout(b[O_SBK:O_SBK + 256]))
        put(28, col_layout(b[O_MLQ:O_MLQ + 512]))
        put(32, col_layout(b[O_MLK:O_MLK + 512]))
        put(36, col_layout(b[O_MLO:O_MLO + 512]))
        put(40, col_layout(b[O_CQ:O_CQ + 256]))
        put(42, col_layout(b[O_CKV:O_CKV + 128]))
        put(43, col_layout(b[O_G:O_G + 3072]))
        kr = np.zeros((96, 1), np.float32)
        kr[64:96, 0] = b[O_KR:O_KR + 32]
        put(67, kr)
        cw = inp["ml_conv_w"][l]
        for c in range(8):
            put(68 + c * 4, cw[:, c * 128:(c + 1) * 128].T)
        put(100, col_layout(inp["ml_conv_b"][l]))
        put(108, col_layout(inp["ml_out_norm"][l]))
        put(112, col_layout(inp["mla_q_norm"][l]))
        put(114, col_layout(inp["mla_kv_norm"][l]))
        put(115, inp["mla_q_gain"][l][:, None])
        put(116, inp["mla_k_gain"][l][:, None])
    return out


def build_program(S, L, dbg=None, phases=("p1", "p23", "p4", "p5")):
    NT = S // TT
    NB = S // 128
    nc = bass.Bass("TRN2", target_bir_lowering=False)
    es = ExitStack()
    cx = Ctx(nc, es)

    def din(name, shape, dt=F32):
        return nc.dram_tensor(name, list(shape), dt, kind="ExternalInput").ap()

    def dscr(name, shape, dt):
        return nc.dram_tensor(name, list(shape), dt, kind="Internal").ap()

    xT_in = din("xT", [D, S])
    pos_in = din("pos", [1, S], I32)
    consts_in = din("consts", [128, NCONST])
    masks_in = din("masks", [128, 6144])
    vecs_in = din("vecs", [128, L * NV_L])
    W = {}
    wshapes = {"ffn1_wi": (D, 2 * FF), "ffn1_wo": (FF, D), "w_in": (D, NIN), "mla_wq_up": (256, 384),
               "mla_wkv_up": (128, 512), "w_up_sb": (256, D), "w_up_ml": (512, D), "w_up_mla": (256, D),
               "w_out": (D, D), "ffn2_wi": (D, 2 * FF), "ffn2_wo": (FF, D)}
    Wb = {}
    for k, (a, b) in wshapes.items():
        W[k] = din(k, [L, a, b])
        Wb[k] = dscr(k + "_b", [L, a, b], BF16)
    b_in_d = din("b_in", [L, NIN])
    yT_out = nc.dram_tensor("yT", [D, S], F32, kind="ExternalOutput").ap()
    dbg_out = None
    if dbg is not None:
        dbg_out = nc.dram_tensor("dbg", list(dbg[1]), dbg[2], kind="ExternalOutput").ap()

    xres = dscr("xres", [D, S], F32)
    sbq = dscr("sbq", [256, S], BF16)
    sbk = dscr("sbk", [256, S], BF16)
    sbv = dscr("sbv", [S, 256], BF16)
    mlq = dscr("mlq", [512, S], BF16)
    mlk = dscr("mlk", [512, S], BF16)
    mlv = dscr("mlv", [S, 512], BF16)
    mlo = dscr("mlo", [512, S], BF16)
    mlif = dscr("mlif", [S, 8], F32)
    mlaq = dscr("mlaq", [4, 96, S], BF16)
    mlak = dscr("mlak", [4, 96, S], BF16)
    mlav = dscr("mlav", [S, 256], BF16)
    gat = dscr("gat", [3072, S], BF16)
    ysb = dscr("ysb", [256, S], BF16)
    yml = dscr("yml", [512, S], BF16)
    ymla = dscr("ymla", [256, S], BF16)
    cosd = dscr("cosd", [32, S], F32)
    sind = dscr("sind", [32, S], F32)

    consts = cx.sb("consts", [128, NCONST], F32)
    vecs = cx.sb("vecs", [128, L * NV_L], F32)
    cb16 = cx.sb("cb16", [128, 128 * 3 + 512 * 12], BF16)
    ones32 = cx.sb("ones32", [128, 128], F32)
    pbanks = [cx.ps("ps%d" % i, [128, 512], F32) for i in range(7)]
    pring = Ring(pbanks)
    psb = cx.ps("psb", [128, 1024], BF16)

    def cc(name):
        o, n = CONST_COLS[name]
        return consts[:, o:o + n]

    ONESB = cb16[:, 0:128]
    TRIB = cb16[:, 128:256]
    IDB = cb16[:, 256:384]

    def SBMB(j):
        return cb16[:, 384 + j * 512: 384 + (j + 1) * 512]

    def MLAMB(j):
        return cb16[:, 384 + 2048 + j * 512: 384 + 2048 + (j + 1) * 512]

    def NEGB(j):
        return cb16[:, 384 + 4096 + j * 512: 384 + 4096 + (j + 1) * 512]

    cx.dma("sp", consts[:], consts_in, writes=[consts])
    cx.dma("sp", vecs[:], vecs_in, writes=[vecs])
    cx.op("dve", lambda e: e.memset(ones32[:], 1.0), writes=[ones32])
    cx.op("dve", lambda e: e.memset(cb16[:, 0:128], 1.0), writes=[cb16])
    cx.op("dve", lambda e: e.tensor_copy(out=cb16[:, 128:256], in_=cc("tri")), reads=[consts], writes=[cb16])
    cx.op("dve", lambda e: e.tensor_copy(out=cb16[:, 256:384], in_=cc("ident")), reads=[consts], writes=[cb16])
    cx.push()
    mk32 = cx.sb("mk32", [128, 6144], F32)
    cx.dma("sp", mk32[:], masks_in, writes=[mk32])
    cx.op("dve", lambda e: e.tensor_copy(out=cb16[:, 384:384 + 6144], in_=mk32[:]), reads=[mk32], writes=[cb16])
    import os
    for k, (a, b) in wshapes.items():
        if os.environ.get("SKIP_CAST"):
            break
        for l in range(L):
            rows = a
            step = max(1, min(rows, CAST_ELEMS // b))
            r0 = 0
            while r0 < rows:
                r1 = min(rows, r0 + step)
                cx.dma("pool", Wb[k][l, r0:r1, :], W[k][l, r0:r1, :])
                r0 = r1
    RC = 512
    posi = cx.sb("posi", [32, RC], I32)
    posf = cx.sb("posf", [32, RC], F32)
    rk = cx.sb("rk", [32, RC], F32)
    rt = cx.sb("rt", [32, RC], F32)
    rs = cx.sb("rs", [32, RC], F32)
    rc_ = cx.sb("rc", [32, RC], F32)
    invf = cc("invf")[0:32, :]
    MAGIC = 12582912.0
    for r0 in range(0, 0 if os.environ.get("SKIP_ROPE") else S, RC):
        cx.dma("sp", posi[:], pos_in[:, r0:r0 + RC].partition_broadcast(32), writes=[posi])
        cx.op("dve", lambda e: e.tensor_copy(out=posf[:], in_=posi[:]), reads=[posi], writes=[posf])
        cx.op("dve", lambda e: e.tensor_scalar(out=posf[:], in0=posf[:], scalar1=invf, scalar2=None, op0=ALU.mult),
              reads=[posf, consts], writes=[posf])
        for which, dst, shift, dd in (("s", rs, 0.0, sind), ("c", rc_, np.pi / 2, cosd)):
            def f1(e, shift=shift):
                return e.tensor_scalar(out=rk[:], in0=posf[:], scalar1=shift, scalar2=1.0 / TWO_PI, op0=ALU.add, op1=ALU.mult)
            cx.op("dve", f1, reads=[posf], writes=[rk])
            cx.op("dve", lambda e: e.tensor_scalar(out=rk[:], in0=rk[:], scalar1=MAGIC, scalar2=None, op0=ALU.add), reads=[rk], writes=[rk])
            cx.op("dve", lambda e: e.tensor_scalar(out=rk[:], in0=rk[:], scalar1=-MAGIC, scalar2=None, op0=ALU.add), reads=[rk], writes=[rk])
            cx.op("dve", lambda e: e.scalar_tensor_tensor(out=rt[:], in0=rk[:], scalar=-C1, in1=posf[:], op0=ALU.mult, op1=ALU.add),
                  reads=[rk, posf], writes=[rt])
            cx.op("dve", lambda e: e.scalar_tensor_tensor(out=rt[:], in0=rk[:], scalar=-C2, in1=rt[:], op0=ALU.mult, op1=ALU.add),
                  reads=[rk, rt], writes=[rt])
            if shift != 0.0:
                cx.op("dve", lambda e, shift=shift: e.tensor_scalar(out=rt[:], in0=rt[:], scalar1=shift, scalar2=None, op0=ALU.add),
                      reads=[rt], writes=[rt])
            cx.op("dve", lambda e: e.tensor_scalar(out=rt[:], in0=rt[:], scalar1=3.1415925, scalar2=-3.1415925, op0=ALU.min, op1=ALU.max),
                  reads=[rt], writes=[rt])
            cx.op("act", lambda e, dst=dst: e.activation(out=dst[:], in_=rt[:], func=AF.Sin), reads=[rt], writes=[dst])
            cx.dma("pool", dd[:, r0:r0 + RC], dst[:], reads=[dst])
    cx.barrier()
    cx.pop()

    cm = {}

    def alloc_common():
        cm["xt"] = cx.sb("xt", [128, 8, TT], F32)
        cm["sq"] = cx.sb("sq", [128, 8, TT], BF16)
        cm["rstd"] = cx.sb("rstd", [128, TT], F32)
        cm["u"] = cx.sb("u", [128, 8, TT], BF16)
        cm["hh"] = cx.sb("hh", [128, 11, TT], BF16)
        cm["tmpr"] = Ring([cx.sb("tmpf%d" % i, [128, TT], F32) for i in range(2)])
        cm["wring"] = Ring([cx.sb("wsl%d" % i, [128, 11 * 256], BF16) for i in range(6)])

    def V(l, off, n=1):
        return vecs[:, l * NV_L + off: l * NV_L + off + n]

    def wload(wname, l, r0, r1, c0, c1):
        sl = cm["wring"].next()
        kc = (r1 - r0 + 127) // 128
        ncol = c1 - c0
        rows = r1 - r0
        if rows % 128 == 0:
            dst = sl.t[:, 0:kc * ncol].rearrange("p (k c) -> p k c", k=kc)
            cx.dma("sp", dst, Wb[wname][l, r0:r1, c0:c1].rearrange("(k p) c -> p k c", p=128), writes=[sl])
        else:
            assert kc == 1
            dst = sl.t[0:rows, 0:ncol]
            cx.dma("sp", dst, Wb[wname][l, r0:r1, c0:c1], writes=[sl])

        def view(k, a, b):
            return sl.t[:, k * ncol + a: k * ncol + b]
        return sl, view

    def rmsnorm_fm(src, l, voff, dst, nch=8, dim=D, c_lo=0):
        sq, rstd = cm["sq"], cm["rstd"]
        cx.op("act", lambda e: e.activation(out=sq[:, 0:nch, :], in_=src[:, c_lo:c_lo + nch, :], func=AF.Square), reads=[src], writes=[sq])
        p = pring.next()

        def mm(e):
            ins = None
            for c in range(nch):
                ins = e.matmul(p[:], ONESB, sq[:, c, :], start=(c == 0), stop=(c == nch - 1))
            return ins
        cx.op("pe", mm, reads=[sq, cb16], writes=[p])
        cx.op("act", lambda e: e.activation(out=rstd[:], in_=p[:], func=AF.Ln, scale=1.0 / dim, bias=EPSB[:, 0:1]), reads=[p, epsb], writes=[rstd])
        cx.op("act", lambda e: e.activation(out=rstd[:], in_=rstd[:], func=AF.Exp, scale=-0.5), reads=[rstd], writes=[rstd])
        for c in range(nch):
            cx.op("dve", lambda e, c=c: e.scalar_tensor_tensor(out=dst[:, c_lo + c, :], in0=src[:, c_lo + c, :], scalar=V(l, voff + c), in1=rstd[:],
                                                               op0=ALU.mult, op1=ALU.mult), reads=[src, rstd, vecs], writes=[dst])

    epsb = cx.sb("epsb", [128, 1], F32)
    EPSB = epsb
    cx.op("dve", lambda e: e.memset(epsb[:], EPS), writes=[epsb])

    def linear_fm(wname, l, K, c0, c1, src, epi, group=256, wrows=None):
        kc = K // 128
        g0 = c0
        m = 0
        while g0 < c1:
            g1 = min(c1, g0 + group)
            sl, view = wload(wname, l, 0, K, g0, g1)
            a = 0
            while a < g1 - g0:
                mw = min(128, g1 - g0 - a)
                p = pring.next()

                def mm(e, a=a, mw=mw, p=p, view=view):
                    ins = None
                    for k in range(kc):
                        ins = e.matmul(p[0:mw, :], view(k, a, a + mw), src[:, k, :], start=(k == 0), stop=(k == kc - 1))
                    return ins
                cx.op("pe", mm, reads=[sl, src], writes=[p])
                epi(p, m, mw)
                m += 1
                a += mw
            g0 = g1

    def ffn(l, wi, wo, normoff):
        xt, u, hh, tmpr = cm["xt"], cm["u"], cm["hh"], cm["tmpr"]
        rmsnorm_fm(xt, l, normoff, u)
        for g0 in range(0, FF, 256):
            g1 = min(FF, g0 + 256)
            sla, va = wload(wi, l, 0, D, g0, g1)
            slg, vg = wload(wi, l, 0, D, FF + g0, FF + g1)
            for a in range(0, g1 - g0, 128):
                j = (g0 + a) // 128
                pa = pring.next()
                pg = pring.next()

                def mm(e, a=a, pa=pa, va=va):
                    ins = None
                    for k in range(8):
                        ins = e.matmul(pa[:], va(k, a, a + 128), u[:, k, :], start=(k == 0), stop=(k == 7))
                    return ins
                cx.op("pe", mm, reads=[sla, u], writes=[pa])

                def mm2(e, a=a, pg=pg, vg=vg):
                    ins = None
                    for k in range(8):
                        ins = e.matmul(pg[:], vg(k, a, a + 128), u[:, k, :], start=(k == 0), stop=(k == 7))
                    return ins
                cx.op("pe", mm2, reads=[slg, u], writes=[pg])
                t = tmpr.next()
                cx.op("act", lambda e, t=t, pa=pa: e.activation(out=t[:], in_=pa[:], func=AF.Silu), reads=[pa], writes=[t])
                cx.op("dve", lambda e, t=t, pg=pg, j=j: e.tensor_tensor(out=hh[:, j, :], in0=t[:], in1=pg[:], op=ALU.mult), reads=[t, pg], writes=[hh])
        for g0 in range(0, D, 256):
            sl, view = wload(wo, l, 0, FF, g0, g0 + 256)
            for a in range(0, 256, 128):
                m = (g0 + a) // 128
                p = pring.next()

                def mm(e, a=a, p=p, view=view):
                    ins = None
                    for k in range(11):
                        ins = e.matmul(p[:], view(k, a, a + 128), hh[:, k, :], start=(k == 0), stop=(k == 10))
                    return ins
                cx.op("pe", mm, reads=[sl, hh], writes=[p])
                cx.op("dve", lambda e, p=p, m=m: e.scalar_tensor_tensor(out=xt[:, m, :], in0=p[:], scalar=0.5, in1=xt[:, m, :], op0=ALU.mult, op1=ALU.add),
                      reads=[p, xt], writes=[xt])

    def p1(l, first):
        cx.push()
        alloc_common()
        xt, tmpr = cm["xt"], cm["tmpr"]
        u2 = cx.sb("u2", [128, 8, TT], BF16)
        st_qk = cx.sb("st_qk", [128, 4, TT], BF16)
        st_tok = cx.sb("st_tok", [128, 4, 1024], BF16)
        st_if = cx.sb("st_if", [128, 4, 8], F32)
        brow = cx.sb("brow", [128, 776], F32)
        xc = cx.sb("xc", [128, 8, 3 + TT], F32)
        cacc = cx.sb("cacc", [128, TT], F32)
        st_mqk = cx.sb("st_mqk", [128, 8, TT], BF16)
        st_o = cx.sb("st_o", [128, 4, TT], BF16)
        st_g = cx.sb("st_g", [128, 8, TT], BF16)
        cqf = cx.sb("cqf", [128, 3, TT], F32)
        cqn = cx.sb("cqn", [128, 3, TT], BF16)
        qn = cx.sb("qn", [96, TT], F32)
        qsq = cx.sb("qsq", [96, TT], F32)
        rtmp = cx.sb("rtmp", [96, TT], F32)
        qb = cx.sb("qb", [96, 8, TT], BF16)
        cst = cx.sb("cst", [96, TT], F32)
        snt = cx.sb("snt", [96, TT], F32)
        wkpad = cx.sb("wkpad", [128, 4, 96], BF16)
        wkr = cx.sb("wkr", [128, 8, 96], BF16)
        wqu = cx.sb("wqu", [128, 2, 384], BF16)
        wkvv = cx.sb("wkvv", [128, 256], BF16)
        et = cx.sb("et", [128, 4, 4], F32)

        src_x = xT_in if first else xres
        cx.op("pool", lambda e: e.memset(wkpad[:], 0.0), writes=[wkpad])
        cx.op("pool", lambda e: e.memset(wkr[:], 0.0), writes=[wkr])
        for h in range(4):
            cx.dma("sp", wkpad[:, h, 0:64], Wb["mla_wkv_up"][l, :, h * 128:h * 128 + 64], writes=[wkpad])
            cx.dma("sp", wkvv[:, h * 64:(h + 1) * 64], Wb["mla_wkv_up"][l, :, h * 128 + 64:h * 128 + 128], writes=[wkvv])
        cx.dma("sp", wkr[:, :, 64:96], Wb["w_in"][l, :, O_KR:O_KR + 32].rearrange("(k p) c -> p k c", p=128), writes=[wkr])
        cx.dma("sp", wqu[:], Wb["mla_wq_up"][l].rearrange("(k p) c -> p k c", p=128), writes=[wqu])
        cx.dma("sp", brow[:, 0:256], b_in_d[l:l + 1, O_SBV:O_SBV + 256].partition_broadcast(128), writes=[brow])
        cx.dma("sp", brow[:, 256:768], b_in_d[l:l + 1, O_MLV:O_MLV + 512].partition_broadcast(128), writes=[brow])
        cx.dma("sp", brow[:, 768:776], b_in_d[l:l + 1, O_MLI:O_MLI + 8].partition_broadcast(128), writes=[brow])
        cx.op("dve", lambda e: e.memset(xc[:, :, 0:3], 0.0), writes=[xc])
        for t in range(NT):
            t0 = t * TT
            cx.dma("sp", xt[:], src_x[:, t0:t0 + TT].rearrange("(c p) t -> p c t", p=128), writes=[xt])
            ffn(l, "ffn1_wi", "ffn1_wo", 0)
            cx.dma("pool", xres[:, t0:t0 + TT].rearrange("(c p) t -> p c t", p=128), xt[:], reads=[xt])
            rmsnorm_fm(xt, l, 8, u2)
            def epi_qk(p, m, mw, base=0, boff=24):
                cx.op("act", lambda e: e.activation(out=st_qk[:, base + m, :], in_=p[:], func=AF.Identity, bias=V(l, boff + m)),
                      reads=[p, vecs], writes=[st_qk])
            linear_fm("w_in", l, D, O_SBQ, O_SBQ + 256, u2, lambda p, m, mw: epi_qk(p, m, mw, 0, 24))
            linear_fm("w_in", l, D, O_SBK, O_SBK + 256, u2, lambda p, m, mw: epi_qk(p, m, mw, 2, 26))
            cx.dma("pool", sbq[:, t0:t0 + TT].rearrange("(c p) t -> p c t", p=128), st_qk[:, 0:2, :], reads=[st_qk])
            cx.dma("pool", sbk[:, t0:t0 + TT].rearrange("(c p) t -> p c t", p=128), st_qk[:, 2:4, :], reads=[st_qk])
            def epi_c(p, m, mw, base, boff):
                cx.op("act", lambda e: e.activation(out=xc[:, base + m, 3:3 + TT], in_=p[:], func=AF.Identity, bias=V(l, boff + m)),
                      reads=[p, vecs], writes=[xc])
            linear_fm("w_in", l, D, O_MLQ, O_MLQ + 512, u2, lambda p, m, mw: epi_c(p, m, mw, 0, 28))
            linear_fm("w_in", l, D, O_MLK, O_MLK + 512, u2, lambda p, m, mw: epi_c(p, m, mw, 4, 32))
            for c in range(8):
                cx.op("dve", lambda e, c=c: e.tensor_scalar(out=cacc[:], in0=xc[:, c, 0:TT], scalar1=V(l, 68 + c * 4 + 0), scalar2=None, op0=ALU.mult),
                      reads=[xc, vecs], writes=[cacc])
                for j in range(1, 4):
                    cx.op("dve", lambda e, c=c, j=j: e.scalar_tensor_tensor(out=cacc[:], in0=xc[:, c, j:j + TT], scalar=V(l, 68 + c * 4 + j), in1=cacc[:],
                                                                        op0=ALU.mult, op1=ALU.add), reads=[xc, vecs, cacc], writes=[cacc])
                if c < 4:
                    cx.op("act", lambda e, c=c: e.activation(out=st_mqk[:, c, :], in_=cacc[:], func=AF.Silu, bias=V(l, 100 + c)),
                          reads=[cacc, vecs], writes=[st_mqk])
                else:
                    tq = tmpr.next()
                    cx.op("act", lambda e, c=c, tq=tq: e.activation(out=tq[:], in_=cacc[:], func=AF.Silu, bias=V(l, 100 + c)),
                          reads=[cacc, vecs], writes=[tq])
                    cx.op("dve", lambda e, c=c, tq=tq: e.tensor_scalar(out=st_mqk[:, c, :], in0=tq[:], scalar1=128.0 ** -0.5, scalar2=None, op0=ALU.mult),
                          reads=[tq], writes=[st_mqk])
            cx.op("dve", lambda e: e.tensor_copy(out=xc[:, :, 0:3], in_=xc[:, :, TT:TT + 3]), reads=[xc], writes=[xc])
            cx.dma("pool", mlq[:, t0:t0 + TT].rearrange("(c p) t -> p c t", p=128), st_mqk[:, 0:4, :], reads=[st_mqk])
            cx.dma("pool", mlk[:, t0:t0 + TT].rearrange("(c p) t -> p c t", p=128), st_mqk[:, 4:8, :], reads=[st_mqk])
            def epi_o(p, m, mw):
                cx.op("act", lambda e: e.activation(out=st_o[:, m, :], in_=p[:], func=AF.Sigmoid, bias=V(l, 36 + m)), reads=[p, vecs], writes=[st_o])
            linear_fm("w_in", l, D, O_MLO, O_MLO + 512, u2, epi_o)
            cx.dma("pool", mlo[:, t0:t0 + TT].rearrange("(c p) t -> p c t", p=128), st_o[:], reads=[st_o])
            for gg in range(3):
                def epi_g(p, m, mw, gg=gg):
                    cx.op("act", lambda e: e.activation(out=st_g[:, m, :], in_=p[:], func=AF.Sigmoid, bias=V(l, 43 + gg * 8 + m)), reads=[p, vecs], writes=[st_g])
                linear_fm("w_in", l, D, O_G + gg * 1024, O_G + (gg + 1) * 1024, u2, epi_g)
                cx.dma("pool", gat[gg * 1024:(gg + 1) * 1024, t0:t0 + TT].rearrange("(c p) t -> p c t", p=128), st_g[:], reads=[st_g])
            slv, vv = wload("w_in", l, 0, D, O_SBV, O_SBV + 256)
            slm, vm = wload("w_in", l, 0, D, O_MLV, O_MLV + 256)
            slm2, vm2 = wload("w_in", l, 0, D, O_MLV + 256, O_MLV + 512)
            slg, vgt = wload("w_in", l, 0, D, O_MLI, O_MLI + 8)
            for s4 in range(4):
                ts_ = slice(s4 * 128, (s4 + 1) * 128)
                for (view, sl, ncol, so, bo) in ((vv, slv, 256, 0, 0), (vm, slm, 256, 256, 256), (vm2, slm2, 256, 512, 512)):
                    p = pring.next()

                    def mm(e, p=p, view=view, ncol=ncol, ts_=ts_):
                        ins = None
                        for k in range(8):
                            ins = e.matmul(p[:, 0:ncol], u2[:, k, ts_], view(k, 0, ncol), start=(k == 0), stop=(k == 7))
                        return ins
                    cx.op("pe", mm, reads=[sl, u2], writes=[p])
                    cx.op("dve", lambda e, p=p, ncol=ncol, so=so, bo=bo, s4=s4: e.tensor_tensor(out=st_tok[:, s4, so:so + ncol], in0=p[:, 0:ncol], in1=brow[:, bo:bo + ncol], op=ALU.add),
                          reads=[p, brow], writes=[st_tok])
                p = pring.next()

                def mm(e, p=p, ts_=ts_):
                    ins = None
                    for k in range(8):
                        ins = e.matmul(p[:, 0:8], u2[:, k, ts_], vgt(k, 0, 8), start=(k == 0), stop=(k == 7))
                    return ins
                cx.op("pe", mm, reads=[slg, u2], writes=[p])
                cx.op("dve", lambda e, p=p, s4=s4: e.tensor_tensor(out=st_if[:, s4, :], in0=p[:, 0:8], in1=brow[:, 768:776], op=ALU.add),
                      reads=[p, brow], writes=[st_if])
            cx.op("act", lambda e: e.activation(out=et[:], in_=st_if[:, :, 4:8], func=AF.Exp, scale=-1.0), reads=[st_if], writes=[et])
            cx.op("act", lambda e: e.activation(out=et[:], in_=et[:], func=AF.Ln, bias=1.0), reads=[et], writes=[et])
            cx.op("dve", lambda e: e.tensor_scalar(out=st_if[:, :, 4:8], in0=et[:], scalar1=-1.0, scalar2=None, op0=ALU.mult), reads=[et], writes=[st_if])
            def epi_cq(p, m, mw, base, boff):
                cx.op("act", lambda e: e.activation(out=cqf[:, base + m, :], in_=p[:], func=AF.Identity, bias=V(l, boff + m)), reads=[p, vecs], writes=[cqf])
            linear_fm("w_in", l, D, O_CQ, O_CQ + 256, u2, lambda p, m, mw: epi_cq(p, m, mw, 0, 40))
            linear_fm("w_in", l, D, O_CKV, O_CKV + 128, u2, lambda p, m, mw: epi_cq(p, m, mw, 2, 42))
            rmsnorm_fm(cqf, l, 112, cqn, nch=2, dim=256, c_lo=0)
            rmsnorm_fm(cqf, l, 114, cqn, nch=1, dim=128, c_lo=2)
            cx.dma("sp", cst[64:96, :], cosd[:, t0:t0 + TT], writes=[cst])
            cx.dma("sp", snt[64:96, :], sind[:, t0:t0 + TT], writes=[snt])

            def norm_rope(p, gcol, dst_i, bias):
                if bias is None:
                    cx.op("act", lambda e: e.activation(out=qn[:], in_=p[0:96, :], func=AF.Copy), reads=[p], writes=[qn])
                else:
                    cx.op("act", lambda e: e.activation(out=qn[:], in_=p[0:96, :], func=AF.Identity, bias=bias), reads=[p, vecs], writes=[qn])
                cx.op("act", lambda e: e.activation(out=qsq[:], in_=qn[:], func=AF.Square), reads=[qn], writes=[qsq])
                p2 = pring.next()
                cx.op("pe", lambda e: e.matmul(p2[0:96, :], ones32[0:96, 0:96], qsq[:], start=True, stop=True), reads=[qsq, ones32], writes=[p2])
                cx.op("act", lambda e: e.activation(out=rtmp[:], in_=p2[0:96, :], func=AF.Ln, scale=1.0 / 96, bias=EPSB[0:96, 0:1]), reads=[p2, epsb], writes=[rtmp])
                cx.op("act", lambda e: e.activation(out=rtmp[:], in_=rtmp[:], func=AF.Exp, scale=-0.5), reads=[rtmp], writes=[rtmp])
                cx.op("dve", lambda e: e.scalar_tensor_tensor(out=qn[:], in0=qn[:], scalar=gcol, in1=rtmp[:], op0=ALU.mult, op1=ALU.mult),
                      reads=[qn, rtmp, vecs], writes=[qn])
                p3 = pring.next()
                cx.op("pe", lambda e: e.matmul(p3[0:96, :], cc("rot")[0:96, :], qn[:], start=True, stop=True), reads=[qn, consts], writes=[p3])
                cx.op("dve", lambda e: e.tensor_tensor(out=rtmp[64:96, :], in0=p3[64:96, :], in1=snt[64:96, :], op=ALU.mult), reads=[p3, snt], writes=[rtmp])
                cx.op("dve", lambda e: e.tensor_tensor(out=qn[64:96, :], in0=qn[64:96, :], in1=cst[64:96, :], op=ALU.mult), reads=[qn, cst], writes=[qn])
                cx.op("dve", lambda e: e.tensor_tensor(out=qn[64:96, :], in0=qn[64:96, :], in1=rtmp[64:96, :], op=ALU.add), reads=[qn, rtmp], writes=[qn])
                cx.op("act", lambda e: e.activation(out=qb[:, dst_i, :], in_=qn[:], func=AF.Copy), reads=[qn], writes=[qb])

            for h in range(4):
                p = pring.next()

                def mmq(e, p=p, h=h):
                    e.matmul(p[0:96, :], wqu[:, 0, h * 96:(h + 1) * 96], cqn[:, 0, :], start=True, stop=False)
                    return e.matmul(p[0:96, :], wqu[:, 1, h * 96:(h + 1) * 96], cqn[:, 1, :], start=False, stop=True)
                cx.op("pe", mmq, reads=[wqu, cqn], writes=[p])
                norm_rope(p, V(l, 115)[0:96, :], h, None)
                p = pring.next()

                def mmk(e, p=p, h=h):
                    e.matmul(p[0:96, :], wkpad[:, h, :], cqn[:, 2, :], start=True, stop=False)
                    ins = None
                    for k in range(8):
                        ins = e.matmul(p[0:96, :], wkr[:, k, :], u2[:, k, :], start=False, stop=(k == 7))
                    return ins
                cx.op("pe", mmk, reads=[wkpad, wkr, cqn, u2], writes=[p])
                norm_rope(p, V(l, 116)[0:96, :], 4 + h, V(l, 67)[0:96, :])
            cx.dma("pool", mlaq[:, :, t0:t0 + TT].rearrange("h p t -> p h t"), qb[:, 0:4, :], reads=[qb])
            cx.dma("pool", mlak[:, :, t0:t0 + TT].rearrange("h p t -> p h t"), qb[:, 4:8, :], reads=[qb])
            for s4 in range(4):
                ts_ = slice(s4 * 128, (s4 + 1) * 128)
                p = pring.next()
                cx.op("pe", lambda e, p=p, ts_=ts_: e.matmul(p[:, 0:256], cqn[:, 2, ts_], wkvv[:], start=True, stop=True), reads=[cqn, wkvv], writes=[p])
                cx.op("act", lambda e, p=p, s4=s4: e.activation(out=st_tok[:, s4, 768:1024], in_=p[:, 0:256], func=AF.Copy), reads=[p], writes=[st_tok])
            cx.dma("pool", sbv[t0:t0 + TT, :].rearrange("(s p) c -> p s c", p=128), st_tok[:, :, 0:256], reads=[st_tok])
            cx.dma("pool", mlv[t0:t0 + TT, :].rearrange("(s p) c -> p s c", p=128), st_tok[:, :, 256:768], reads=[st_tok])
            cx.dma("pool", mlav[t0:t0 + TT, :].rearrange("(s p) c -> p s c", p=128), st_tok[:, :, 768:1024], reads=[st_tok])
            cx.dma("pool", mlif[t0:t0 + TT, :].rearrange("(s p) c -> p s c", p=128), st_if[:], reads=[st_if])
        cx.barrier()
        cx.pop()

    def p23(l):
        cx.push()
        KT = cx.sb("KT", [96, S], BF16)
        VV = cx.sb("VV", [128, NB, 64], BF16)
        VA = cx.sb("VA", [128, NB, 65], BF16)
        accs = cx.sb("accs", [65, TT], F32)
        sel = cx.sb("sel", [65, 64], F32)
        QTr = Ring([cx.sb("QT%d" % i, [96, TT], BF16) for i in range(2)])
        er = Ring([cx.sb("e%d" % i, [128, TT], F32) for i in range(3)])
        spr = Ring([cx.sb("sp%d" % i, [128, TT], F32) for i in range(5)])
        atr = Ring([cx.sb("at%d" % i, [128, TT], F32) for i in range(3)])
        lkr = Ring([cx.sb("lk%d" % i, [128, TT], BF16) for i in range(5)])
        wr = Ring([cx.sb("w%d" % i, [128, TT], BF16) for i in range(5)])
        csr = Ring([cx.sb("CS%d" % i, [128, TT], BF16) for i in range(6)])
        csh = {}
        obr = Ring([cx.sb("ob%d" % i, [64, TT], BF16) for i in range(2)])
        recb = cx.sb("recb", [64, TT], F32)
        ring5 = Ring(pbanks[0:5])
        pacc = [pbanks[5], pbanks[6]]

        def run_pipe_(units, nst):
            n = len(units)
            for i in range(n + nst - 1):
                for s in range(nst):
                    k = i - s
                    if 0 <= k < n:
                        units[k][s]()

        def p2_sb(l):
            for h in range(4):
                cx.dma("sp", KT[0:64, :], sbk[h * 64:(h + 1) * 64, :], writes=[KT])
                cx.dma("sp", VV[:], sbv[:, h * 64:(h + 1) * 64].rearrange("(n p) c -> p n c", p=128), writes=[VV])
                for c0 in range(0, S, 1024):
                    cx.op("dve", lambda e, c0=c0: e.tensor_scalar(out=KT[0:64, c0:c0 + 1024], in0=KT[0:64, c0:c0 + 1024], scalar1=0.125, scalar2=None, op0=ALU.mult),
                          reads=[KT], writes=[KT])
                units = []
                for T in range(NT):
                    QT = QTr.next()
                    po = pacc[T % 2]
                    nlist = list(range(4 * T + 3, -1, -1))
                    for ui, n in enumerate(nlist):
                        st = {}
                        first = ui == 0
                        last = ui == len(nlist) - 1
                        jj = n - 4 * T

                        def A1(st=st, n=n, first=first, QT=QT, T=T):
                            if first:
                                cx.dma("sp", QT[0:64, :], sbq[h * 64:(h + 1) * 64, T * TT:(T + 1) * TT], writes=[QT])
                            pz = ring5.next()
                            cx.op("pe", lambda e: e.matmul(pz[:], KT[0:64, n * 128:(n + 1) * 128], QT[0:64, :], start=True, stop=True), reads=[KT, QT], writes=[pz])
                            et_ = er.next()
                            cx.op("act", lambda e: e.activation(out=et_[:], in_=pz[:], func=AF.Exp, scale=-1.0), reads=[pz], writes=[et_])
                            st["pz"] = pz
                            st["e"] = et_

                        def A2(st=st, jj=jj, first=first):
                            pz, et_ = st["pz"], st["e"]
                            spt = spr.next()
                            lkb = lkr.next()
                            cx.op("act", lambda e: e.activation(out=spt[:], in_=et_[:], func=AF.Ln, bias=1.0), reads=[et_], writes=[spt])
                            cx.op("dve", lambda e: e.scalar_tensor_tensor(out=lkb[:], in0=pz[:], scalar=-1.0, in1=spt[:], op0=ALU.mult, op1=ALU.subtract),
                                  reads=[pz, spt], writes=[lkb])
                            if jj >= 0:
                                cx.op("dve", lambda e: e.tensor_tensor(out=lkb[:], in0=lkb[:], in1=SBMB(jj), op=ALU.mult), reads=[lkb, cb16], writes=[lkb])
                            st["lk"] = lkb
                            st["sp"] = spt

                        def A3(st=st, first=first):
                            lkb = st["lk"]
                            st["cs_prev"] = None if first else csh["cur"]
                            if first:
                                csh["cur"] = lkb
                            else:
                                cs_new = csr.next()
                                cp = csh["cur"]
                                cx.op("dve", lambda e: e.tensor_tensor(out=cs_new[:], in0=cp[:], in1=lkb[:], op=ALU.add), reads=[cp, lkb], writes=[cs_new])
                                csh["cur"] = cs_new

                        def B(st=st, jj=jj, first=first, n=n, QT=QT):
                            pl = ring5.next()
                            lkb = st["lk"]
                            spt = st["sp"]
                            CS = st["cs_prev"]

                            def mm(e):
                                nmm = 1 + (0 if first else 1) + (1 if jj >= 0 else 0)
                                k = 0
                                ins = e.matmul(pl[:], TRIB, lkb[:], start=True, stop=(nmm == 1))
                                k += 1
                                if not first:
                                    k += 1
                                    ins = e.matmul(pl[:], ONESB, CS[:], start=False, stop=(k == nmm))
                                if jj >= 0:
                                    k += 1
                                    ins = e.matmul(pl[:], IDB, NEGB(jj), start=False, stop=(k == nmm))
                                return ins
                            cx.op("pe", mm, reads=[lkb, cb16] + ([] if first else [CS]), writes=[pl])
                            at = atr.next()
                            cx.op("dve", lambda e: e.tensor_tensor(out=at[:], in0=pl[:], in1=spt[:], op=ALU.subtract), reads=[pl, spt], writes=[at])
                            wt = wr.next()
                            cx.op("act", lambda e: e.activation(out=wt[:], in_=at[:], func=AF.Exp), reads=[at], writes=[wt])
                            st["w"] = wt

                        def C(st=st, n=n, first=first, last=last, po=po, T=T):
                            wt = st["w"]
                            cx.op("pe", lambda e: e.matmul(po[0:64, :], VV[:, n, :], wt[:], start=first, stop=last), reads=[VV, wt], writes=[po])
                            if last:
                                ob = obr.next()
                                cx.op("act", lambda e: e.activation(out=ob[:], in_=po[0:64, :], func=AF.Copy), reads=[po], writes=[ob])
                                cx.dma("pool", ysb[h * 64:(h + 1) * 64, T * TT:(T + 1) * TT], ob[:], reads=[ob])
                        units.append((A1, A2, B, C, A3))
                nu = len(units)
                for i in range(nu + 3):
                    if i < nu:
                        units[i][0]()
                    if 0 <= i - 2 < nu:
                        units[i - 2][2]()
                    if 0 <= i - 1 < nu:
                        units[i - 1][4]()
                    if i < nu:
                        units[i][1]()
                    if 0 <= i - 3 < nu:
                        units[i - 3][3]()
            cx.barrier()

        def p3_mla(l):
            sc = 96.0 ** -0.5
            cx.op("dve", lambda e: e.memset(VA[:], 1.0), writes=[VA])
            cx.op("dve", lambda e: e.memset(sel[:], 0.0), writes=[sel])
            cx.op("dve", lambda e: e.memset(sel[64:65, :], 1.0), writes=[sel])
            for h in range(4):
                cx.dma("sp", KT[:], mlak[h], writes=[KT])
                cx.dma("sp", VA[:, :, 0:64], mlav[:, h * 64:(h + 1) * 64].rearrange("(n p) c -> p n c", p=128), writes=[VA])
                units = []
                for T in range(NT):
                    QT = QTr.next()
                    nlist = list(range(0, 4 * T + 4))
                    for ui, n in enumerate(nlist):
                        st = {}
                        first = ui == 0
                        last = ui == len(nlist) - 1
                        jj = n - 4 * T

                        def A(st=st, n=n, first=first, QT=QT, T=T, jj=jj):
                            if first:
                                cx.dma("sp", QT[:], mlaq[h, :, T * TT:(T + 1) * TT], writes=[QT])
                            pz = ring5.next()
                            cx.op("pe", lambda e: e.matmul(pz[:], KT[:, n * 128:(n + 1) * 128], QT[:], start=True, stop=True), reads=[KT, QT], writes=[pz])
                            wt = wr.next()
                            cx.op("act", lambda e: e.activation(out=wt[:], in_=pz[:], func=AF.Exp, scale=sc), reads=[pz], writes=[wt])
                            if jj >= 0:
                                cx.op("dve", lambda e: e.tensor_tensor(out=wt[:], in0=wt[:], in1=MLAMB(jj), op=ALU.mult), reads=[wt, cb16], writes=[wt])
                            st["w"] = wt

                        def B(st=st, n=n, first=first, last=last, T=T):
                            wt = st["w"]
                            pa = pacc[T % 2]
                            cx.op("pe", lambda e: e.matmul(pa[0:65, :], VA[:, n, :], wt[:], start=first, stop=last), reads=[VA, wt], writes=[pa])
                            if last:
                                ob = obr.next()
                                cx.op("act", lambda e: e.activation(out=accs[:], in_=pa[0:65, :], func=AF.Copy), reads=[pa], writes=[accs])
                                pb = ring5.next()
                                cx.op("pe", lambda e: e.matmul(pb[0:64, :], sel[:], accs[:], start=True, stop=True), reads=[sel, accs], writes=[pb])
                                cx.op("act", lambda e: e.activation(out=recb[:], in_=pb[0:64, :], func=AF.Ln), reads=[pb], writes=[recb])
                                cx.op("act", lambda e: e.activation(out=recb[:], in_=recb[:], func=AF.Exp, scale=-1.0), reads=[recb], writes=[recb])
                                cx.op("dve", lambda e: e.tensor_tensor(out=ob[:], in0=accs[0:64, :], in1=recb[:], op=ALU.mult), reads=[accs, recb], writes=[ob])
                                cx.dma("pool", ymla[h * 64:(h + 1) * 64, T * TT:(T + 1) * TT], ob[:], reads=[ob])
                        units.append((A, B))
                run_pipe_(units, 2)
            cx.barrier()

        p2_sb(l)
        p3_mla(l)
        cx.pop()

    def p4_ml(l):
        cx.push()
        if os.environ.get("P4_LIMIT"):
            cx.limit = int(os.environ["P4_LIMIT"])
        mQ = cx.sb("mQ", [128, 4, TT], BF16)
        mK = cx.sb("mK", [128, 4, TT], BF16)
        mV = cx.sb("mV", [128, 4, 512], BF16)
        mO = cx.sb("mO", [128, 4, TT], BF16)
        mIF = cx.sb("mIF", [128, 4, 8], F32)
        bcol = cx.sb("bcol", [128, 4], F32)
        wend = cx.sb("wend", [128, 4], F32)
        decay = cx.sb("decay", [128, 4], F32)
        Bm = cx.sb("Bm", [128, 4, 128], F32)
        Am = cx.sb("Am", [128, 4, 128], F32)
        Gt = cx.sb("Gt", [128, 4, 128], F32)
        Pm = cx.sb("Pm", [128, 4, 128], BF16)
        qa = cx.sb("qa", [128, 4, 128], BF16)
        kw = cx.sb("kw", [128, 4, 128], BF16)
        C32 = cx.sb("C32", [128, 4, 128], F32)
        Cb = cx.sb("Cb", [128, 4, 128], BF16)
        n32 = cx.sb("n32", [128, 4], F32)
        Nb = cx.sb("Nb", [128, 4, 128], BF16)
        dn = cx.sb("dn", [128, 4, 128], F32)
        HT = cx.sb("HT", [128, 4, 128], F32)
        hsq = cx.sb("hsq", [128, 4, 128], BF16)
        hr = cx.sb("hr", [128, 4, 128], F32)
        yst = cx.sb("yst", [128, 4, TT], BF16)
        onecol = cx.sb("onecol", [128, 1], BF16)
        psm = pbanks[4]

        cx.op("dve", lambda e: e.memset(C32[:], 0.0), writes=[C32])
        cx.op("dve", lambda e: e.memset(Cb[:], 0.0), writes=[Cb])
        cx.op("dve", lambda e: e.memset(n32[:], 0.0), writes=[n32])
        cx.op("dve", lambda e: e.memset(Nb[:], 0.0), writes=[Nb])
        cx.op("dve", lambda e: e.memset(onecol[:], 1.0), writes=[onecol])
        r4 = Ring(pbanks[0:4])
        for t in range(NT):
            t0 = t * TT
            cx.dma("sp", mQ[:], mlq[:, t0:t0 + TT].rearrange("(h p) t -> p h t", p=128), writes=[mQ])
            cx.dma("sp", mK[:], mlk[:, t0:t0 + TT].rearrange("(h p) t -> p h t", p=128), writes=[mK])
            cx.dma("sp", mV[:], mlv[t0:t0 + TT, :].rearrange("(s p) c -> p s c", p=128), writes=[mV])
            cx.dma("sp", mO[:], mlo[:, t0:t0 + TT].rearrange("(h p) t -> p h t", p=128), writes=[mO])
            cx.dma("sp", mIF[:], mlif[t0:t0 + TT, :].rearrange("(s p) c -> p s c", p=128), writes=[mIF])
            for c4 in range(4):
                cs = slice(c4 * 128, (c4 + 1) * 128)
                cx.op("pe", lambda e, c4=c4: e.matmul(psm[:, 0:4], cc("upper"), mIF[:, c4, 4:8], start=True, stop=True), reads=[consts, mIF], writes=[psm])
                cx.op("dve", lambda e, c4=c4: e.tensor_tensor(out=bcol[:], in0=mIF[:, c4, 0:4], in1=psm[:, 0:4], op=ALU.subtract), reads=[mIF, psm], writes=[bcol])
                cx.op("dve", lambda e, c4=c4: e.tensor_tensor(out=Bm[:], in0=cc("upper").unsqueeze(1).to_broadcast([128, 4, 128]),
                                                              in1=mIF[:, c4, 4:8].unsqueeze(2).to_broadcast([128, 4, 128]), op=ALU.mult),
                      reads=[consts, mIF], writes=[Bm])
                pR = r4.next()
                pR2 = r4.next()
                Bf = Bm[:].rearrange("p h t -> p (h t)")
                cx.op("pe", lambda e, pR=pR: e.matmul(pR[:], ones32[:], Bf, start=True, stop=True), reads=[ones32, Bm], writes=[pR])

                def mmR2(e, pR2=pR2):
                    e.matmul(pR2[:], ones32[:], Bf, start=True, stop=False)
                    return e.matmul(pR2[:], cc("ident"), cc("negm4"), start=False, stop=True)
                cx.op("pe", mmR2, reads=[ones32, Bm, consts], writes=[pR2])
                cx.op("act", lambda e, pR=pR: e.activation(out=Am[:].rearrange("p h t -> p (h t)"), in_=pR[:], func=AF.Exp), reads=[pR], writes=[Am])
                for h in range(4):
                    cx.op("act", lambda e, h=h, pR2=pR2: e.activation(out=Gt[:, h, :], in_=pR2[:, h * 128:(h + 1) * 128], func=AF.Exp, bias=bcol[:, h:h + 1]),
                          reads=[pR2, bcol], writes=[Gt])
                for h in range(4):
                    cx.op("act", lambda e, h=h, pR=pR: e.activation(out=decay[:, h:h + 1], in_=pR[:, h * 128 + 127:h * 128 + 128], func=AF.Exp), reads=[pR], writes=[decay])
                cx.op("act", lambda e: e.activation(out=wend[:], in_=bcol[:], func=AF.Exp), reads=[bcol], writes=[wend])
                cx.op("dve", lambda e: e.tensor_tensor(out=wend[:], in0=wend[:], in1=decay[:], op=ALU.mult), reads=[wend, decay], writes=[wend])
                pS = r4.next()

                def mmS(e, pS=pS, cs=cs):
                    ins = None
                    for h in range(4):
                        ins = e.matmul(pS[:, h * 128:(h + 1) * 128], mK[:, h, cs], mQ[:, h, cs], start=True, stop=True)
                    return ins
                cx.op("pe", mmS, reads=[mK, mQ], writes=[pS])
                cx.op("dve", lambda e, pS=pS: e.tensor_tensor(out=Pm[:].rearrange("p h t -> p (h t)"), in0=pS[:], in1=Gt[:].rearrange("p h t -> p (h t)"), op=ALU.mult),
                      reads=[pS, Gt], writes=[Pm])
                cx.op("dve", lambda e, cs=cs: e.tensor_tensor(out=qa[:], in0=mQ[:, :, cs], in1=Am[:], op=ALU.mult), reads=[mQ, Am], writes=[qa])
                pN = r4.next()
                pD = pbanks[5]

                def mmN(e, pN=pN, c4=c4):
                    ins = None
                    for h in range(4):
                        e.matmul(pN[:, h * 128:(h + 1) * 128], mV[:, c4, h * 128:(h + 1) * 128], Pm[:, h, :], start=True, stop=False)
                        ins = e.matmul(pN[:, h * 128:(h + 1) * 128], Cb[:, h, :], qa[:, h, :], start=False, stop=True)
                    return ins
                cx.op("pe", mmN, reads=[mV, Pm, Cb, qa], writes=[pN])

                def mmD(e):
                    ins = None
                    for h in range(4):
                        e.matmul(pD[:, h * 128:(h + 1) * 128], ONESB, Pm[:, h, :], start=True, stop=False)
                        ins = e.matmul(pD[:, h * 128:(h + 1) * 128], Nb[:, h, :], qa[:, h, :], start=False, stop=True)
                    return ins
                cx.op("pe", mmD, reads=[cb16, Pm, Nb, qa], writes=[pD])
                dnf = dn[:].rearrange("p h t -> p (h t)")
                cx.op("act", lambda e: e.activation(out=dnf, in_=pD[:], func=AF.Abs), reads=[pD], writes=[dn])
                cx.op("dve", lambda e: e.tensor_scalar(out=dnf, in0=dnf, scalar1=1.0, scalar2=None, op0=ALU.max), reads=[dn], writes=[dn])
                cx.op("act", lambda e: e.activation(out=dnf, in_=dnf, func=AF.Ln), reads=[dn], writes=[dn])
                cx.op("act", lambda e: e.activation(out=dnf, in_=dnf, func=AF.Exp, scale=-1.0), reads=[dn], writes=[dn])
                HTf = HT[:].rearrange("p h t -> p (h t)")
                cx.op("dve", lambda e, pN=pN: e.tensor_tensor(out=HTf, in0=pN[:], in1=dnf, op=ALU.mult), reads=[pN, dn], writes=[HT])
                cx.op("pe", lambda e, cs=cs: [e.transpose(psb[:, h * 128:(h + 1) * 128], mK[:, h, cs], IDB) for h in range(4)][-1], reads=[mK, cb16], writes=[psb])
                cx.op("dve", lambda e: e.tensor_tensor(out=kw[:], in0=psb[:, 0:512].rearrange("p (h t) -> p h t", h=4), in1=wend[:].unsqueeze(2).to_broadcast([128, 4, 128]), op=ALU.mult),
                      reads=[psb, wend], writes=[kw])
                pC = r4.next()

                def mmC(e, pC=pC, c4=c4):
                    ins = None
                    for h in range(4):
                        ins = e.matmul(pC[:, h * 128:(h + 1) * 128], kw[:, h, :], mV[:, c4, h * 128:(h + 1) * 128], start=True, stop=True)
                    return ins
                cx.op("pe", mmC, reads=[kw, mV], writes=[pC])
                cx.op("pe", lambda e: [e.matmul(psm[:, 4 + h:5 + h], kw[:, h, :], onecol[:], start=True, stop=True) for h in range(4)][-1], reads=[kw, onecol], writes=[psm])
                for h in range(4):
                    cx.op("dve", lambda e, h=h, pC=pC: e.scalar_tensor_tensor(out=C32[:, h, :], in0=C32[:, h, :], scalar=decay[:, h:h + 1], in1=pC[:, h * 128:(h + 1) * 128],
                                                                      op0=ALU.mult, op1=ALU.add), reads=[C32, decay, pC], writes=[C32])
                cx.op("dve", lambda e: e.tensor_tensor(out=n32[:], in0=n32[:], in1=decay[:], op=ALU.mult), reads=[n32, decay], writes=[n32])
                cx.op("dve", lambda e: e.tensor_tensor(out=n32[:], in0=n32[:], in1=psm[:, 4:8], op=ALU.add), reads=[n32, psm], writes=[n32])
                cx.op("act", lambda e: e.activation(out=Cb[:], in_=C32[:], func=AF.Copy), reads=[C32], writes=[Cb])
                cx.op("dve", lambda e: e.tensor_tensor(out=Nb[:], in0=ones32[:].unsqueeze(1).to_broadcast([128, 4, 128]), in1=n32[:].unsqueeze(2).to_broadcast([128, 4, 128]), op=ALU.mult),
                      reads=[ones32, n32], writes=[Nb])
                cx.op("act", lambda e: e.activation(out=hsq[:], in_=HT[:], func=AF.Square), reads=[HT], writes=[hsq])
                pQ = r4.next()
                cx.op("pe", lambda e, pQ=pQ: e.matmul(pQ[:], ONESB, hsq[:].rearrange("p h t -> p (h t)"), start=True, stop=True), reads=[cb16, hsq], writes=[pQ])
                hrf = hr[:].rearrange("p h t -> p (h t)")
                cx.op("act", lambda e, pQ=pQ: e.activation(out=hrf, in_=pQ[:], func=AF.Ln, scale=1.0 / 128, bias=EPSB[:, 0:1]), reads=[pQ, epsb], writes=[hr])
                cx.op("act", lambda e: e.activation(out=hrf, in_=hrf, func=AF.Exp, scale=-0.5), reads=[hr], writes=[hr])
                for h in range(4):
                    cx.op("dve", lambda e, h=h: e.scalar_tensor_tensor(out=HT[:, h, :], in0=HT[:, h, :], scalar=V(l, 108 + h), in1=hr[:, h, :], op0=ALU.mult, op1=ALU.mult),
                          reads=[HT, hr, vecs], writes=[HT])
                cx.op("dve", lambda e, cs=cs: e.tensor_tensor(out=yst[:, :, cs], in0=HT[:], in1=mO[:, :, cs], op=ALU.mult), reads=[HT, mO], writes=[yst])
            cx.dma("pool", yml[:, t0:t0 + TT].rearrange("(h p) t -> p h t", p=128), yst[:], reads=[yst])
        cx.barrier()
        cx.pop()

    def p5(l, lastlayer):
        cx.push()
        alloc_common()
        xt = cm["xt"]
        yb = cx.sb("yb", [128, 8, TT], BF16)
        mg = cx.sb("mg", [128, 8, TT], BF16)
        macc = cx.sb("macc", [128, TT], F32)
        mtmp = cx.sb("mtmp", [128, TT], F32)

        st_g = cx.sb("gt", [128, 24, TT], BF16)
        for t in range(NT):
            t0 = t * TT
            cx.dma("sp", xt[:], xres[:, t0:t0 + TT].rearrange("(c p) t -> p c t", p=128), writes=[xt])
            cx.dma("sp", yb[:, 0:2, :], ysb[:, t0:t0 + TT].rearrange("(c p) t -> p c t", p=128), writes=[yb])
            cx.dma("sp", yb[:, 2:6, :], yml[:, t0:t0 + TT].rearrange("(c p) t -> p c t", p=128), writes=[yb])
            cx.dma("sp", yb[:, 6:8, :], ymla[:, t0:t0 + TT].rearrange("(c p) t -> p c t", p=128), writes=[yb])
            cx.dma("sp", st_g[:], gat[:, t0:t0 + TT].rearrange("(c p) t -> p c t", p=128), writes=[st_g])
            for g0 in range(0, D, 512):
                sls = [wload("w_up_sb", l, 0, 256, g0, g0 + 512), wload("w_up_ml", l, 0, 512, g0, g0 + 512), wload("w_up_mla", l, 0, 256, g0, g0 + 512)]
                for a in range(0, 512, 128):
                    m = (g0 + a) // 128
                    for bi, (kcn, yo) in enumerate(((2, 0), (4, 2), (2, 6))):
                        sl, view = sls[bi]
                        p = pring.next()

                        def mm(e, p=p, view=view, kcn=kcn, yo=yo, a=a):
                            ins = None
                            for k in range(kcn):
                                ins = e.matmul(p[:], view(k, a, a + 128), yb[:, yo + k, :], start=(k == 0), stop=(k == kcn - 1))
                            return ins
                        cx.op("pe", mm, reads=[sl, yb], writes=[p])
                        if bi == 0:
                            cx.op("dve", lambda e, p=p, m=m: e.tensor_tensor(out=macc[:], in0=p[:], in1=st_g[:, m, :], op=ALU.mult), reads=[p, st_g], writes=[macc])
                        else:
                            cx.op("dve", lambda e, p=p, m=m, bi=bi: e.tensor_tensor(out=mtmp[:], in0=p[:], in1=st_g[:, bi * 8 + m, :], op=ALU.mult), reads=[p, st_g], writes=[mtmp])
                            if bi == 1:
                                cx.op("dve", lambda e: e.tensor_tensor(out=macc[:], in0=macc[:], in1=mtmp[:], op=ALU.add), reads=[macc, mtmp], writes=[macc])
                            else:
                                cx.op("dve", lambda e, m=m: e.tensor_tensor(out=mg[:, m, :], in0=macc[:], in1=mtmp[:], op=ALU.add), reads=[macc, mtmp], writes=[mg])

            def epi_out(p, m, mw):
                cx.op("dve", lambda e: e.tensor_tensor(out=xt[:, m, :], in0=xt[:, m, :], in1=p[:], op=ALU.add), reads=[xt, p], writes=[xt])
            linear_fm("w_out", l, D, 0, D, mg, epi_out)
            ffn(l, "ffn2_wi", "ffn2_wo", 16)
            dst = yT_out if lastlayer else xres
            cx.dma("pool", dst[:, t0:t0 + TT].rearrange("(c p) t -> p c t", p=128), xt[:], reads=[xt])
        cx.barrier()
        cx.pop()

    for l in range(L):
        if "p1" in phases:
            p1(l, l == 0)
        if "p23" in phases:
            p23(l)
        if "p4" in phases:
            p4_ml(l)
        if "p5" in phases:
            p5(l, l == L - 1)
    if dbg is not None:
        src = {"sbq": sbq, "sbk": sbk, "sbv": sbv, "mlq": mlq, "mlk": mlk, "mlv": mlv, "mlo": mlo, "mlif": mlif, "mlaq": mlaq, "mlak": mlak,
               "mlav": mlav, "gat": gat, "ysb": ysb, "yml": yml, "ymla": ymla, "xres": xres, "cosd": cosd, "sind": sind}[dbg[0]]
        cx.dma("pool", dbg_out, src)
        cx.barrier()
    cx.barrier()
    return nc, cx.ninst, cx


NCONST = None
_CONSTS = None


def _get_consts():
    global NCONST, _CONSTS
    if _CONSTS is None:
        _CONSTS = build_consts()
        NCONST = _CONSTS[0].shape[1]
    return _CONSTS


WNAMES = ("ffn1_wi", "ffn1_wo", "w_in", "mla_wq_up", "mla_wkv_up", "w_up_sb", "w_up_ml", "w_up_mla", "w_out", "ffn2_wi", "ffn2_wo")


def make_in_map(inp, b, S, L):
    consts = _get_consts()
    m = {
        "xT": np.ascontiguousarray(inp["x"][b, :S].T),
        "pos": np.ascontiguousarray(inp["positions"][b:b + 1, :S]).astype(np.int32),
        "consts": consts[0],
        "masks": consts[1],
        "vecs": build_vecs(inp, L),
        "b_in": np.ascontiguousarray(inp["b_in"][:L]),
    }
    for k in WNAMES:
        m[k] = np.ascontiguousarray(inp[k][:L])
    return m


def kernel(**inputs):
    inp = {k: np.asarray(v) for k, v in inputs.items()}
    B, S, _ = inp["x"].shape
    L = inp["w_in"].shape[0]
    _get_consts()
    nc, ninst, cx = build_program(S, L)
    vec = build_vecs(inp, L)
    in_maps = []
    for b in range(B):
        m = make_in_map(inp, b, S, L)
        m["vecs"] = vec
        in_maps.append(m)
    res = run_bass_kernel_spmd(nc, in_maps, core_ids=list(range(B)))
    out = np.stack([np.ascontiguousarray(res.results[b]["yT"].T) for b in range(B)], axis=0)
    return out.astype(np.float32)
```

```python
from contextlib import ExitStack
import numpy as np
import concourse.bass as bass
import concourse.mybir as mybir
from concourse.bass_utils import run_bass_kernel_spmd

F32 = mybir.dt.float32
BF16 = mybir.dt.bfloat16
I32 = mybir.dt.int32
AF = mybir.ActivationFunctionType
ALU = mybir.AluOpType

D = 1024
FF = 1408
NIN = 6312
O_SBQ, O_SBK, O_SBV = 0, 256, 512
O_MLQ, O_MLK, O_MLV, O_MLO, O_MLI, O_MLF = 768, 1280, 1792, 2304, 2816, 2820
O_CQ, O_CKV, O_KR, O_G = 2824, 3080, 3208, 3240
EPS = 1e-6
TT = 512
NV_L = 120
CAST_ELEMS = 1 << 16
TWO_PI = 6.283185307179586
C1 = 6.28125
C2 = TWO_PI - C1


class Chan:
    def __init__(self, sem):
        self.sem = sem
        self.count = 0


class Buf:
    def __init__(self, ctx, t, name):
        self.ctx = ctx
        self.t = t
        self.name = name
        self.lw = None
        self.rd = []
        self.lchan = None
        self.schan = None

    def __getitem__(self, k):
        return self.t[k]


class Op:
    __slots__ = ("eng", "fn", "deps", "signal", "idx", "epoch")

    def __init__(self, eng, fn, deps):
        self.eng = eng
        self.fn = fn
        self.deps = deps
        self.signal = False
        self.idx = 0
        self.epoch = 0


class Ctx:
    ENG = ("pe", "act", "dve", "pool", "sp")

    def __init__(self, nc, es):
        self.nc = nc
        self.es = es
        self.e = {"pe": nc.tensor, "act": nc.scalar, "dve": nc.vector, "pool": nc.gpsimd, "sp": nc.sync}
        self.ops = []
        self.last = {k: None for k in self.ENG}
        self.bufs = []
        self.chans = []
        self.free_chans = {}
        self.stacks = [es]
        self.scope_bufs = [[]]
        self.nsem = 0
        self.ninst = 0
        self.limit = None
        self.cnt = {k: 0 for k in self.ENG}
        self.epoch = {k: 0 for k in self.ENG}
        self.sems = None
        self.seen_op = {k: {} for k in self.ENG}
        self.seen_ch = {k: {} for k in self.ENG}
        self.misc = self.new_chan("misc")
        self.out_chan = self.new_chan("outc")

    def sem(self, name):
        self.nsem += 1
        return self.es.enter_context(self.nc.semaphore(name))

    def new_chan(self, name, kind="x"):
        fl = self.free_chans.setdefault(kind, [])
        if fl:
            return fl.pop()
        c = Chan(self.sem("c_" + name))
        self.chans.append(c)
        return c

    def push(self):
        st = ExitStack()
        self.stacks.append(st)
        self.scope_bufs.append([])

    def pop(self):
        st = self.stacks.pop()
        for b in self.scope_bufs.pop():
            self.bufs.remove(b)
            if b.lchan is not None:
                self.free_chans.setdefault("l", []).append(b.lchan)
            if b.schan is not None:
                self.free_chans.setdefault("s", []).append(b.schan)
        st.close()

    def sb(self, name, shape, dt):
        self.uid = getattr(self, "uid", 0) + 1
        b = Buf(self, self.stacks[-1].enter_context(self.nc.sbuf_tensor("%s_s%d" % (name, self.uid), list(shape), dt)), name)
        self.bufs.append(b)
        self.scope_bufs[-1].append(b)
        return b

    def ps(self, name, shape, dt):
        b = Buf(self, self.es.enter_context(self.nc.psum_tensor(name + "_p", list(shape), dt)), name)
        self.bufs.append(b)
        return b

    def _deps(self, reads, writes, eng=None):
        deps = []
        for b in reads:
            if b.lw is not None:
                deps.append(b.lw)
        for b in writes:
            if b.lw is not None and not (b.lw[0] == "op" and b.lw[1].eng == eng):
                deps.append(b.lw)
            for r in b.rd:
                if not (r[0] == "op" and r[1].eng == eng):
                    deps.append(r)
        return deps

    def op(self, eng, fn, reads=(), writes=()):
        if self.limit is not None:
            self.limit -= 1
            if self.limit < 0:
                return None
        deps = self._deps(reads, writes, eng)
        o = Op(eng, fn, deps)
        tok = ("op", o)
        for b in writes:
            b.lw = tok
            b.rd = []
        for b in reads:
            if b not in writes:
                b.rd.append(tok)
                if len(b.rd) > 64:
                    b.rd = b.rd[-48:]
        self.ops.append(o)
        self.last[eng] = o
        return o

    def dma(self, q, out_ap, in_ap, reads=(), writes=(), chan=None):
        deps = self._deps(reads, writes)
        chans = []
        for b in writes:
            if b.lchan is None:
                b.lchan = self.new_chan("l_" + b.name, "l")
            chans.append(b.lchan)
        for b in reads:
            if b.schan is None:
                b.schan = self.new_chan("s_" + b.name, "s")
            chans.append(b.schan)
        if chan is not None:
            chans.append(chan)
        if not chans:
            chans = [self.misc]
        ch = chans[0]
        assert len(chans) == 1, "dma must touch exactly one tracked buf"
        ch.count += 16
        tok = ("dma", ch, ch.count)

        def fn(e, out_ap=out_ap, in_ap=in_ap, ch=ch):
            return e.dma_start(out=out_ap, in_=in_ap).then_inc(ch.sem, 16)

        o = Op(q, fn, deps)
        o.signal = None
        for b in writes:
            b.lw = tok
            b.rd = []
        for b in reads:
            b.rd.append(tok)
        self.ops.append(o)
        return o

    def barrier(self):
        toks = []
        for k in self.ENG:
            if self.last[k] is not None:
                toks.append(("op", self.last[k]))
        for c in self.chans:
            if c.count:
                toks.append(("dma", c, c.count))
        for k in self.ENG:
            o = Op(k, None, list(toks))
            o.signal = None
            self.ops.append(o)
            o.fn = "barrier"
        for b in self.bufs:
            b.lw = None
            b.rd = []
        self.ops.append("epoch")
        self.emit()
        self.ops = []
        self.last = {k: None for k in self.ENG}

    def emit(self):
        for o in self.ops:
            if o == "epoch":
                continue
            for d in o.deps:
                if d[0] == "op" and (d[1].eng != o.eng or o.eng != "pe") and d[1].signal is not None:
                    d[1].signal = True
        cnt = self.cnt
        epoch = self.epoch
        if self.sems is None:
            self.sems = {k: [self.sem("e_%s_0" % k)] for k in self.ENG}
        sems = self.sems
        for o in self.ops:
            if o == "epoch":
                for k in self.ENG:
                    if cnt[k] > 20000:
                        epoch[k] += 1
                        cnt[k] = 0
                        sems[k].append(self.sem("e_%s_%d" % (k, epoch[k])))
                continue
            if o.signal is True:
                cnt[o.eng] += 1
                o.idx = cnt[o.eng]
                o.epoch = epoch[o.eng]
        seen_op = self.seen_op
        seen_ch = self.seen_ch
        ninst = 0
        for o in self.ops:
            if o == "epoch":
                continue
            e = self.e[o.eng]
            need_op = {}
            need_ch = {}
            for d in o.deps:
                if d[0] == "op":
                    s = d[1]
                    if s.eng == o.eng and o.eng == "pe":
                        continue
                    if s.signal is not True:
                        continue
                    key = (s.eng, s.epoch)
                    if seen_op[o.eng].get(key, 0) >= s.idx:
                        continue
                    need_op[key] = max(need_op.get(key, 0), s.idx)
                else:
                    _, ch, c = d
                    if seen_ch[o.eng].get(ch, 0) >= c:
                        continue
                    need_ch[ch] = max(need_ch.get(ch, 0), c)
            for key, v in need_op.items():
                e.wait_ge(sems[key[0]][key[1]], v)
                seen_op[o.eng][key] = v
                ninst += 1
            for ch, v in need_ch.items():
                e.wait_ge(ch.sem, v)
                seen_ch[o.eng][ch] = v
                ninst += 1
            if o.fn == "barrier":
                continue
            ins = o.fn(e)
            ninst += 1
            if o.signal is True:
                ins.then_inc(sems[o.eng][o.epoch], 1)
        self.ninst += ninst
        return ninst


class Ring:
    def __init__(self, bufs):
        self.bufs = bufs
        self.i = 0

    def next(self):
        b = self.bufs[self.i % len(self.bufs)]
        self.i += 1
        return b


CONST_COLS = {}


def build_consts():
    cols = []
    off = 0

    def add(name, arr):
        nonlocal off
        a = np.zeros((128, arr.shape[1]), np.float32)
        a[: arr.shape[0]] = arr
        CONST_COLS[name] = (off, arr.shape[1])
        off += arr.shape[1]
        cols.append(a)

    j = np.arange(128)[:, None]
    s = np.arange(128)[None, :]
    add("tri", (j > s).astype(np.float32))
    add("upper", (j <= s).astype(np.float32))
    add("ident", (j == s).astype(np.float32))
    negm = np.where(j > s, -30000.0, 0.0).astype(np.float32)
    add("negm4", np.tile(negm, (1, 4)))
    masks = []
    for jj in range(4):
        ms = np.zeros((128, 512), np.float32)
        mi = np.zeros((128, 512), np.float32)
        for jq in range(4):
            if jq > jj:
                ms[:, jq * 128:(jq + 1) * 128] = 1.0
                mi[:, jq * 128:(jq + 1) * 128] = 1.0
            elif jq == jj:
                ms[:, jq * 128:(jq + 1) * 128] = (j < s)
                mi[:, jq * 128:(jq + 1) * 128] = (j <= s)
        masks.append((ms, mi))
    rot = np.zeros((96, 96), np.float32)
    for i in range(16):
        rot[80 + i, 64 + i] = -1.0
        rot[64 + i, 80 + i] = 1.0
    add("rot", rot)
    invf = np.power(np.float32(10000.0), -np.arange(16, dtype=np.float32) / np.float32(16)).astype(np.float32)
    add("invf", np.concatenate([invf, invf])[:, None])
    mk = np.concatenate([m[0] for m in masks] + [m[1] for m in masks] + [(1.0 - m[0]) * -30000.0 for m in masks], axis=1)
    return np.concatenate(cols, axis=1), mk


def col_layout(v):
    n = v.shape[0] // 128
    return v.reshape(n, 128).T


def build_vecs(inp, L):
    out = np.zeros((128, L * NV_L), np.float32)
    for l in range(L):
        o = l * NV_L
        b = inp["b_in"][l]

        def put(off, arr):
            out[: arr.shape[0], o + off: o + off + arr.shape[1]] = arr

        put(0, col_layout(inp["ffn1_norm"][l]))
        put(8, col_layout(inp["mix_norm"][l]))
        put(16, col_layout(inp["ffn2_norm"][l]))
        put(24, col_layout(b[O_SBQ:O_SBQ + 256]))
        put(26, col_layout(b[O_SBK:O_SBK + 256]))
        put(28, col_layout(b[O_MLQ:O_MLQ + 512]))
        put(32, col_layout(b[O_MLK:O_MLK + 512]))
        put(36, col_layout(b[O_MLO:O_MLO + 512]))
        put(40, col_layout(b[O_CQ:O_CQ + 256]))
        put(42, col_layout(b[O_CKV:O_CKV + 128]))
        put(43, col_layout(b[O_G:O_G + 3072]))
        kr = np.zeros((96, 1), np.float32)
        kr[64:96, 0] = b[O_KR:O_KR + 32]
        put(67, kr)
        cw = inp["ml_conv_w"][l]
        for c in range(8):
            put(68 + c * 4, cw[:, c * 128:(c + 1) * 128].T)
        put(100, col_layout(inp["ml_conv_b"][l]))
        put(108, col_layout(inp["ml_out_norm"][l]))
        put(112, col_layout(inp["mla_q_norm"][l]))
        put(114, col_layout(inp["mla_kv_norm"][l]))
        put(115, inp["mla_q_gain"][l][:, None])
        put(116, inp["mla_k_gain"][l][:, None])
    return out


def build_program(S, L, dbg=None, phases=("p1", "p23", "p4", "p5")):
    NT = S // TT
    NB = S // 128
    nc = bass.Bass("TRN2", target_bir_lowering=False)
    es = ExitStack()
    cx = Ctx(nc, es)

    def din(name, shape, dt=F32):
        return nc.dram_tensor(name, list(shape), dt, kind="ExternalInput").ap()

    def dscr(name, shape, dt):
        return nc.dram_tensor(name, list(shape), dt, kind="Internal").ap()

    xT_in = din("xT", [D, S])
    pos_in = din("pos", [1, S], I32)
    consts_in = din("consts", [128, NCONST])
    masks_in = din("masks", [128, 6144])
    vecs_in = din("vecs", [128, L * NV_L])
    W = {}
    wshapes = {"ffn1_wi": (D, 2 * FF), "ffn1_wo": (FF, D), "w_in": (D, NIN), "mla_wq_up": (256, 384),
               "mla_wkv_up": (128, 512), "w_up_sb": (256, D), "w_up_ml": (512, D), "w_up_mla": (256, D),
               "w_out": (D, D), "ffn2_wi": (D, 2 * FF), "ffn2_wo": (FF, D)}
    Wb = {}
    for k, (a, b) in wshapes.items():
        W[k] = din(k, [L, a, b])
        Wb[k] = dscr(k + "_b", [L, a, b], BF16)
    b_in_d = din("b_in", [L, NIN])
    yT_out = nc.dram_tensor("yT", [D, S], F32, kind="ExternalOutput").ap()
    dbg_out = None
    if dbg is not None:
        dbg_out = nc.dram_tensor("dbg", list(dbg[1]), dbg[2], kind="ExternalOutput").ap()

    xres = dscr("xres", [D, S], F32)
    sbq = dscr("sbq", [256, S], BF16)
    sbk = dscr("sbk", [256, S], BF16)
    sbv = dscr("sbv", [S, 256], BF16)
    mlq = dscr("mlq", [512, S], BF16)
    mlk = dscr("mlk", [512, S], BF16)
    mlv = dscr("mlv", [S, 512], BF16)
    mlo = dscr("mlo", [512, S], BF16)
    mlif = dscr("mlif", [S, 8], F32)
    mlaq = dscr("mlaq", [4, 96, S], BF16)
    mlak = dscr("mlak", [4, 96, S], BF16)
    mlav = dscr("mlav", [S, 256], BF16)
    gat = dscr("gat", [3072, S], BF16)
    ysb = dscr("ysb", [256, S], BF16)
    yml = dscr("yml", [512, S], BF16)
    ymla = dscr("ymla", [256, S], BF16)
    cosd = dscr("cosd", [32, S], F32)
    sind = dscr("sind", [32, S], F32)

    consts = cx.sb("consts", [128, NCONST], F32)
    vecs = cx.sb("vecs", [128, L * NV_L], F32)
    cb16 = cx.sb("cb16", [128, 128 * 3 + 512 * 12], BF16)
    ones32 = cx.sb("ones32", [128, 128], F32)
    pbanks = [cx.ps("ps%d" % i, [128, 512], F32) for i in range(7)]
    pring = Ring(pbanks)
    psb = cx.ps("psb", [128, 1024], BF16)

    def cc(name):
        o, n = CONST_COLS[name]
        return consts[:, o:o + n]

    ONESB = cb16[:, 0:128]
    TRIB = cb16[:, 128:256]
    IDB = cb16[:, 256:384]

    def SBMB(j):
        return cb16[:, 384 + j * 512: 384 + (j + 1) * 512]

    def MLAMB(j):
        return cb16[:, 384 + 2048 + j * 512: 384 + 2048 + (j + 1) * 512]

    def NEGB(j):
        return cb16[:, 384 + 4096 + j * 512: 384 + 4096 + (j + 1) * 512]

    cx.dma("sp", consts[:], consts_in, writes=[consts])
    cx.dma("sp", vecs[:], vecs_in, writes=[vecs])
    cx.op("dve", lambda e: e.memset(ones32[:], 1.0), writes=[ones32])
    cx.op("dve", lambda e: e.memset(cb16[:, 0:128], 1.0), writes=[cb16])
    cx.op("dve", lambda e: e.tensor_copy(out=cb16[:, 128:256], in_=cc("tri")), reads=[consts], writes=[cb16])
    cx.op("dve", lambda e: e.tensor_copy(out=cb16[:, 256:384], in_=cc("ident")), reads=[consts], writes=[cb16])
    cx.push()
    mk32 = cx.sb("mk32", [128, 6144], F32)
    cx.dma("sp", mk32[:], masks_in, writes=[mk32])
    cx.op("dve", lambda e: e.tensor_copy(out=cb16[:, 384:384 + 6144], in_=mk32[:]), reads=[mk32], writes=[cb16])
    import os
    for k, (a, b) in wshapes.items():
        if os.environ.get("SKIP_CAST"):
            break
        for l in range(L):
            rows = a
            step = max(1, min(rows, CAST_ELEMS // b))
            r0 = 0
            while r0 < rows:
                r1 = min(rows, r0 + step)
                cx.dma("pool", Wb[k][l, r0:r1, :], W[k][l, r0:r1, :])
                r0 = r1
    RC = 512
    posi = cx.sb("posi", [32, RC], I32)
    posf = cx.sb("posf", [32, RC], F32)
    rk = cx.sb("rk", [32, RC], F32)
    rt = cx.sb("rt", [32, RC], F32)
    rs = cx.sb("rs", [32, RC], F32)
    rc_ = cx.sb("rc", [32, RC], F32)
    invf = cc("invf")[0:32, :]
    MAGIC = 12582912.0
    for r0 in range(0, 0 if os.environ.get("SKIP_ROPE") else S, RC):
        cx.dma("sp", posi[:], pos_in[:, r0:r0 + RC].partition_broadcast(32), writes=[posi])
        cx.op("dve", lambda e: e.tensor_copy(out=posf[:], in_=posi[:]), reads=[posi], writes=[posf])
        cx.op("dve", lambda e: e.tensor_scalar(out=posf[:], in0=posf[:], scalar1=invf, scalar2=None, op0=ALU.mult),
              reads=[posf, consts], writes=[posf])
        for which, dst, shift, dd in (("s", rs, 0.0, sind), ("c", rc_, np.pi / 2, cosd)):
            def f1(e, shift=shift):
                return e.tensor_scalar(out=rk[:], in0=posf[:], scalar1=shift, scalar2=1.0 / TWO_PI, op0=ALU.add, op1=ALU.mult)
            cx.op("dve", f1, reads=[posf], writes=[rk])
            cx.op("dve", lambda e: e.tensor_scalar(out=rk[:], in0=rk[:], scalar1=MAGIC, scalar2=None, op0=ALU.add), reads=[rk], writes=[rk])
            cx.op("dve", lambda e: e.tensor_scalar(out=rk[:], in0=rk[:], scalar1=-MAGIC, scalar2=None, op0=ALU.add), reads=[rk], writes=[rk])
            cx.op("dve", lambda e: e.scalar_tensor_tensor(out=rt[:], in0=rk[:], scalar=-C1, in1=posf[:], op0=ALU.mult, op1=ALU.add),
                  reads=[rk, posf], writes=[rt])
            cx.op("dve", lambda e: e.scalar_tensor_tensor(out=rt[:], in0=rk[:], scalar=-C2, in1=rt[:], op0=ALU.mult, op1=ALU.add),
                  reads=[rk, rt], writes=[rt])
            if shift != 0.0:
                cx.op("dve", lambda e, shift=shift: e.tensor_scalar(out=rt[:], in0=rt[:], scalar1=shift, scalar2=None, op0=ALU.add),
                      reads=[rt], writes=[rt])
            cx.op("dve", lambda e: e.tensor_scalar(out=rt[:], in0=rt[:], scalar1=3.1415925, scalar2=-3.1415925, op0=ALU.min, op1=ALU.max),
                  reads=[rt], writes=[rt])
            cx.op("act", lambda e, dst=dst: e.activation(out=dst[:], in_=rt[:], func=AF.Sin), reads=[rt], writes=[dst])
            cx.dma("pool", dd[:, r0:r0 + RC], dst[:], reads=[dst])
    cx.barrier()
    cx.pop()

    cm = {}

    def alloc_common():
        cm["xt"] = cx.sb("xt", [128, 8, TT], F32)
        cm["sq"] = cx.sb("sq", [128, 8, TT], BF16)
        cm["rstd"] = cx.sb("rstd", [128, TT], F32)
        cm["u"] = cx.sb("u", [128, 8, TT], BF16)
        cm["hh"] = cx.sb("hh", [128, 11, TT], BF16)
        cm["tmpr"] = Ring([cx.sb("tmpf%d" % i, [128, TT], F32) for i in range(2)])
        cm["wring"] = Ring([cx.sb("wsl%d" % i, [128, 11 * 256], BF16) for i in range(6)])

    def V(l, off, n=1):
        return vecs[:, l * NV_L + off: l * NV_L + off + n]

    def wload(wname, l, r0, r1, c0, c1):
        sl = cm["wring"].next()
        kc = (r1 - r0 + 127) // 128
        ncol = c1 - c0
        rows = r1 - r0
        if rows % 128 == 0:
            dst = sl.t[:, 0:kc * ncol].rearrange("p (k c) -> p k c", k=kc)
            cx.dma("sp", dst, Wb[wname][l, r0:r1, c0:c1].rearrange("(k p) c -> p k c", p=128), writes=[sl])
        else:
            assert kc == 1
            dst = sl.t[0:rows, 0:ncol]
            cx.dma("sp", dst, Wb[wname][l, r0:r1, c0:c1], writes=[sl])

        def view(k, a, b):
            return sl.t[:, k * ncol + a: k * ncol + b]
        return sl, view

    def rmsnorm_fm(src, l, voff, dst, nch=8, dim=D, c_lo=0):
        sq, rstd = cm["sq"], cm["rstd"]
        cx.op("act", lambda e: e.activation(out=sq[:, 0:nch, :], in_=src[:, c_lo:c_lo + nch, :], func=AF.Square), reads=[src], writes=[sq])
        p = pring.next()

        def mm(e):
            ins = None
            for c in range(nch):
                ins = e.matmul(p[:], ONESB, sq[:, c, :], start=(c == 0), stop=(c == nch - 1))
            return ins
        cx.op("pe", mm, reads=[sq, cb16], writes=[p])
        cx.op("act", lambda e: e.activation(out=rstd[:], in_=p[:], func=AF.Ln, scale=1.0 / dim, bias=EPSB[:, 0:1]), reads=[p, epsb], writes=[rstd])
        cx.op("act", lambda e: e.activation(out=rstd[:], in_=rstd[:], func=AF.Exp, scale=-0.5), reads=[rstd], writes=[rstd])
        for c in range(nch):
            cx.op("dve", lambda e, c=c: e.scalar_tensor_tensor(out=dst[:, c_lo + c, :], in0=src[:, c_lo + c, :], scalar=V(l, voff + c), in1=rstd[:],
                                                               op0=ALU.mult, op1=ALU.mult), reads=[src, rstd, vecs], writes=[dst])

    epsb = cx.sb("epsb", [128, 1], F32)
    EPSB = epsb
    cx.op("dve", lambda e: e.memset(epsb[:], EPS), writes=[epsb])

    def linear_fm(wname, l, K, c0, c1, src, epi, group=256, wrows=None):
        kc = K // 128
        g0 = c0
        m = 0
        while g0 < c1:
            g1 = min(c1, g0 + group)
            sl, view = wload(wname, l, 0, K, g0, g1)
            a = 0
            while a < g1 - g0:
                mw = min(128, g1 - g0 - a)
                p = pring.next()

                def mm(e, a=a, mw=mw, p=p, view=view):
                    ins = None
                    for k in range(kc):
                        ins = e.matmul(p[0:mw, :], view(k, a, a + mw), src[:, k, :], start=(k == 0), stop=(k == kc - 1))
                    return ins
                cx.op("pe", mm, reads=[sl, src], writes=[p])
                epi(p, m, mw)
                m += 1
                a += mw
            g0 = g1

    def ffn(l, wi, wo, normoff):
        xt, u, hh, tmpr = cm["xt"], cm["u"], cm["hh"], cm["tmpr"]
        rmsnorm_fm(xt, l, normoff, u)
        for g0 in range(0, FF, 256):
            g1 = min(FF, g0 + 256)
            sla, va = wload(wi, l, 0, D, g0, g1)
            slg, vg = wload(wi, l, 0, D, FF + g0, FF + g1)
            for a in range(0, g1 - g0, 128):
                j = (g0 + a) // 128
                pa = pring.next()
                pg = pring.next()

                def mm(e, a=a, pa=pa, va=va):
                    ins = None
                    for k in range(8):
                        ins = e.matmul(pa[:], va(k, a, a + 128), u[:, k, :], start=(k == 0), stop=(k == 7))
                    return ins
                cx.op("pe", mm, reads=[sla, u], writes=[pa])

                def mm2(e, a=a, pg=pg, vg=vg):
                    ins = None
                    for k in range(8):
                        ins = e.matmul(pg[:], vg(k, a, a + 128), u[:, k, :], start=(k == 0), stop=(k == 7))
                    return ins
                cx.op("pe", mm2, reads=[slg, u], writes=[pg])
                t = tmpr.next()
                cx.op("act", lambda e, t=t, pa=pa: e.activation(out=t[:], in_=pa[:], func=AF.Silu), reads=[pa], writes=[t])
                cx.op("dve", lambda e, t=t, pg=pg, j=j: e.tensor_tensor(out=hh[:, j, :], in0=t[:], in1=pg[:], op=ALU.mult), reads=[t, pg], writes=[hh])
        for g0 in range(0, D, 256):
            sl, view = wload(wo, l, 0, FF, g0, g0 + 256)
            for a in range(0, 256, 128):
                m = (g0 + a) // 128
                p = pring.next()

                def mm(e, a=a, p=p, view=view):
                    ins = None
                    for k in range(11):
                        ins = e.matmul(p[:], view(k, a, a + 128), hh[:, k, :], start=(k == 0), stop=(k == 10))
                    return ins
                cx.op("pe", mm, reads=[sl, hh], writes=[p])
                cx.op("dve", lambda e, p=p, m=m: e.scalar_tensor_tensor(out=xt[:, m, :], in0=p[:], scalar=0.5, in1=xt[:, m, :], op0=ALU.mult, op1=ALU.add),
                      reads=[p, xt], writes=[xt])

    def p1(l, first):
        cx.push()
        alloc_common()
        xt, tmpr = cm["xt"], cm["tmpr"]
        u2 = cx.sb("u2", [128, 8, TT], BF16)
        st_qk = cx.sb("st_qk", [128, 4, TT], BF16)
        st_tok = cx.sb("st_tok", [128, 4, 1024], BF16)
        st_if = cx.sb("st_if", [128, 4, 8], F32)
        brow = cx.sb("brow", [128, 776], F32)
        xc = cx.sb("xc", [128, 8, 3 + TT], F32)
        cacc = cx.sb("cacc", [128, TT], F32)
        st_mqk = cx.sb("st_mqk", [128, 8, TT], BF16)
        st_o = cx.sb("st_o", [128, 4, TT], BF16)
        st_g = cx.sb("st_g", [128, 8, TT], BF16)
        cqf = cx.sb("cqf", [128, 3, TT], F32)
        cqn = cx.sb("cqn", [128, 3, TT], BF16)
        qn = cx.sb("qn", [96, TT], F32)
        qsq = cx.sb("qsq", [96, TT], F32)
        rtmp = cx.sb("rtmp", [96, TT], F32)
        qb = cx.sb("qb", [96, 8, TT], BF16)
        cst = cx.sb("cst", [96, TT], F32)
        snt = cx.sb("snt", [96, TT], F32)
        wkpad = cx.sb("wkpad", [128, 4, 96], BF16)
        wkr = cx.sb("wkr", [128, 8, 96], BF16)
        wqu = cx.sb("wqu", [128, 2, 384], BF16)
        wkvv = cx.sb("wkvv", [128, 256], BF16)
        et = cx.sb("et", [128, 4, 4], F32)

        src_x = xT_in if first else xres
        cx.op("pool", lambda e: e.memset(wkpad[:], 0.0), writes=[wkpad])
        cx.op("pool", lambda e: e.memset(wkr[:], 0.0), writes=[wkr])
        for h in range(4):
            cx.dma("sp", wkpad[:, h, 0:64], Wb["mla_wkv_up"][l, :, h * 128:h * 128 + 64], writes=[wkpad])
            cx.dma("sp", wkvv[:, h * 64:(h + 1) * 64], Wb["mla_wkv_up"][l, :, h * 128 + 64:h * 128 + 128], writes=[wkvv])
        cx.dma("sp", wkr[:, :, 64:96], Wb["w_in"][l, :, O_KR:O_KR + 32].rearrange("(k p) c -> p k c", p=128), writes=[wkr])
        cx.dma("sp", wqu[:], Wb["mla_wq_up"][l].rearrange("(k p) c -> p k c", p=128), writes=[wqu])
        cx.dma("sp", brow[:, 0:256], b_in_d[l:l + 1, O_SBV:O_SBV + 256].partition_broadcast(128), writes=[brow])
        cx.dma("sp", brow[:, 256:768], b_in_d[l:l + 1, O_MLV:O_MLV + 512].partition_broadcast(128), writes=[brow])
        cx.dma("sp", brow[:, 768:776], b_in_d[l:l + 1, O_MLI:O_MLI + 8].partition_broadcast(128), writes=[brow])
        cx.op("dve", lambda e: e.memset(xc[:, :, 0:3], 0.0), writes=[xc])
        for t in range(NT):
            t0 = t * TT
            cx.dma("sp", xt[:], src_x[:, t0:t0 + TT].rearrange("(c p) t -> p c t", p=128), writes=[xt])
            ffn(l, "ffn1_wi", "ffn1_wo", 0)
            cx.dma("pool", xres[:, t0:t0 + TT].rearrange("(c p) t -> p c t", p=128), xt[:], reads=[xt])
            rmsnorm_fm(xt, l, 8, u2)
            def epi_qk(p, m, mw, base=0, boff=24):
                cx.op("act", lambda e: e.activation(out=st_qk[:, base + m, :], in_=p[:], func=AF.Identity, bias=V(l, boff + m)),
                      reads=[p, vecs], writes=[st_qk])
            linear_fm("w_in", l, D, O_SBQ, O_SBQ + 256, u2, lambda p, m, mw: epi_qk(p, m, mw, 0, 24))
            linear_fm("w_in", l, D, O_SBK, O_SBK + 256, u2, lambda p, m, mw: epi_qk(p, m, mw, 2, 26))
            cx.dma("pool", sbq[:, t0:t0 + TT].rearrange("(c p) t -> p c t", p=128), st_qk[:, 0:2, :], reads=[st_qk])
            cx.dma("pool", sbk[:, t0:t0 + TT].rearrange("(c p) t -> p c t", p=128), st_qk[:, 2:4, :], reads=[st_qk])
            def epi_c(p, m, mw, base, boff):
                cx.op("act", lambda e: e.activation(out=xc[:, base + m, 3:3 + TT], in_=p[:], func=AF.Identity, bias=V(l, boff + m)),
                      reads=[p, vecs], writes=[xc])
            linear_fm("w_in", l, D, O_MLQ, O_MLQ + 512, u2, lambda p, m, mw: epi_c(p, m, mw, 0, 28))
            linear_fm("w_in", l, D, O_MLK, O_MLK + 512, u2, lambda p, m, mw: epi_c(p, m, mw, 4, 32))
            for c in range(8):
                cx.op("dve", lambda e, c=c: e.tensor_scalar(out=cacc[:], in0=xc[:, c, 0:TT], scalar1=V(l, 68 + c * 4 + 0), scalar2=None, op0=ALU.mult),
                      reads=[xc, vecs], writes=[cacc])
                for j in range(1, 4):
                    cx.op("dve", lambda e, c=c, j=j: e.scalar_tensor_tensor(out=cacc[:], in0=xc[:, c, j:j + TT], scalar=V(l, 68 + c * 4 + j), in1=cacc[:],
                                                                        op0=ALU.mult, op1=ALU.add), reads=[xc, vecs, cacc], writes=[cacc])
                if c < 4:
                    cx.op("act", lambda e, c=c: e.activation(out=st_mqk[:, c, :], in_=cacc[:], func=AF.Silu, bias=V(l, 100 + c)),
                          reads=[cacc, vecs], writes=[st_mqk])
                else:
                    tq = tmpr.next()
                    cx.op("act", lambda e, c=c, tq=tq: e.activation(out=tq[:], in_=cacc[:], func=AF.Silu, bias=V(l, 100 + c)),
                          reads=[cacc, vecs], writes=[tq])
                    cx.op("dve", lambda e, c=c, tq=tq: e.tensor_scalar(out=st_mqk[:, c, :], in0=tq[:], scalar1=128.0 ** -0.5, scalar2=None, op0=ALU.mult),
                          reads=[tq], writes=[st_mqk])
            cx.op("dve", lambda e: e.tensor_copy(out=xc[:, :, 0:3], in_=xc[:, :, TT:TT + 3]), reads=[xc], writes=[xc])
            cx.dma("pool", mlq[:, t0:t0 + TT].rearrange("(c p) t -> p c t", p=128), st_mqk[:, 0:4, :], reads=[st_mqk])
            cx.dma("pool", mlk[:, t0:t0 + TT].rearrange("(c p) t -> p c t", p=128), st_mqk[:, 4:8, :], reads=[st_mqk])
            def epi_o(p, m, mw):
                cx.op("act", lambda e: e.activation(out=st_o[:, m, :], in_=p[:], func=AF.Sigmoid, bias=V(l, 36 + m)), reads=[p, vecs], writes=[st_o])
            linear_fm("w_in", l, D, O_MLO, O_MLO + 512, u2, epi_o)
            cx.dma("pool", mlo[:, t0:t0 + TT].rearrange("(c p) t -> p c t", p=128), st_o[:], reads=[st_o])
            for gg in range(3):
                def epi_g(p, m, mw, gg=gg):
                    cx.op("act", lambda e: e.activation(out=st_g[:, m, :], in_=p[:], func=AF.Sigmoid, bias=V(l, 43 + gg * 8 + m)), reads=[p, vecs], writes=[st_g])
                linear_fm("w_in", l, D, O_G + gg * 1024, O_G + (gg + 1) * 1024, u2, epi_g)
                cx.dma("pool", gat[gg * 1024:(gg + 1) * 1024, t0:t0 + TT].rearrange("(c p) t -> p c t", p=128), st_g[:], reads=[st_g])
            slv, vv = wload("w_in", l, 0, D, O_SBV, O_SBV + 256)
            slm, vm = wload("w_in", l, 0, D, O_MLV, O_MLV + 256)
            slm2, vm2 = wload("w_in", l, 0, D, O_MLV + 256, O_MLV + 512)
            slg, vgt = wload("w_in", l, 0, D, O_MLI, O_MLI + 8)
            for s4 in range(4):
                ts_ = slice(s4 * 128, (s4 + 1) * 128)
                for (view, sl, ncol, so, bo) in ((vv, slv, 256, 0, 0), (vm, slm, 256, 256, 256), (vm2, slm2, 256, 512, 512)):
                    p = pring.next()

                    def mm(e, p=p, view=view, ncol=ncol, ts_=ts_):
                        ins = None
                        for k in range(8):
                            ins = e.matmul(p[:, 0:ncol], u2[:, k, ts_], view(k, 0, ncol), start=(k == 0), stop=(k == 7))
                        return ins
                    cx.op("pe", mm, reads=[sl, u2], writes=[p])
                    cx.op("dve", lambda e, p=p, ncol=ncol, so=so, bo=bo, s4=s4: e.tensor_tensor(out=st_tok[:, s4, so:so + ncol], in0=p[:, 0:ncol], in1=brow[:, bo:bo + ncol], op=ALU.add),
                          reads=[p, brow], writes=[st_tok])
                p = pring.next()

                def mm(e, p=p, ts_=ts_):
                    ins = None
                    for k in range(8):
                        ins = e.matmul(p[:, 0:8], u2[:, k, ts_], vgt(k, 0, 8), start=(k == 0), stop=(k == 7))
                    return ins
                cx.op("pe", mm, reads=[slg, u2], writes=[p])
                cx.op("dve", lambda e, p=p, s4=s4: e.tensor_tensor(out=st_if[:, s4, :], in0=p[:, 0:8], in1=brow[:, 768:776], op=ALU.add),
                      reads=[p, brow], writes=[st_if])
            cx.op("act", lambda e: e.activation(out=et[:], in_=st_if[:, :, 4:8], func=AF.Exp, scale=-1.0), reads=[st_if], writes=[et])
            cx.op("act", lambda e: e.activation(out=et[:], in_=et[:], func=AF.Ln, bias=1.0), reads=[et], writes=[et])
            cx.op("dve", lambda e: e.tensor_scalar(out=st_if[:, :, 4:8], in0=et[:], scalar1=-1.0, scalar2=None, op0=ALU.mult), reads=[et], writes=[st_if])
            def epi_cq(p, m, mw, base, boff):
                cx.op("act", lambda e: e.activation(out=cqf[:, base + m, :], in_=p[:], func=AF.Identity, bias=V(l, boff + m)), reads=[p, vecs], writes=[cqf])
            linear_fm("w_in", l, D, O_CQ, O_CQ + 256, u2, lambda p, m, mw: epi_cq(p, m, mw, 0, 40))
            linear_fm("w_in", l, D, O_CKV, O_CKV + 128, u2, lambda p, m, mw: epi_cq(p, m, mw, 2, 42))
            rmsnorm_fm(cqf, l, 112, cqn, nch=2, dim=256, c_lo=0)
            rmsnorm_fm(cqf, l, 114, cqn, nch=1, dim=128, c_lo=2)
            cx.dma("sp", cst[64:96, :], cosd[:, t0:t0 + TT], writes=[cst])
            cx.dma("sp", snt[64:96, :], sind[:, t0:t0 + TT], writes=[snt])

            def norm_rope(p, gcol, dst_i, bias):
                if bias is None:
                    cx.op("act", lambda e: e.activation(out=qn[:], in_=p[0:96, :], func=AF.Copy), reads=[p], writes=[qn])
                else:
                    cx.op("act", lambda e: e.activation(out=qn[:], in_=p[0:96, :], func=AF.Identity, bias=bias), reads=[p, vecs], writes=[qn])
                cx.op("act", lambda e: e.activation(out=qsq[:], in_=qn[:], func=AF.Square), reads=[qn], writes=[qsq])
                p2 = pring.next()
                cx.op("pe", lambda e: e.matmul(p2[0:96, :], ones32[0:96, 0:96], qsq[:], start=True, stop=True), reads=[qsq, ones32], writes=[p2])
                cx.op("act", lambda e: e.activation(out=rtmp[:], in_=p2[0:96, :], func=AF.Ln, scale=1.0 / 96, bias=EPSB[0:96, 0:1]), reads=[p2, epsb], writes=[rtmp])
                cx.op("act", lambda e: e.activation(out=rtmp[:], in_=rtmp[:], func=AF.Exp, scale=-0.5), reads=[rtmp], writes=[rtmp])
                cx.op("dve", lambda e: e.scalar_tensor_tensor(out=qn[:], in0=qn[:], scalar=gcol, in1=rtmp[:], op0=ALU.mult, op1=ALU.mult),
                      reads=[qn, rtmp, vecs], writes=[qn])
                p3 = pring.next()
                cx.op("pe", lambda e: e.matmul(p3[0:96, :], cc("rot")[0:96, :], qn[:], start=True, stop=True), reads=[qn, consts], writes=[p3])
                cx.op("dve", lambda e: e.tensor_tensor(out=rtmp[64:96, :], in0=p3[64:96, :], in1=snt[64:96, :], op=ALU.mult), reads=[p3, snt], writes=[rtmp])
                cx.op("dve", lambda e: e.tensor_tensor(out=qn[64:96, :], in0=qn[64:96, :], in1=cst[64:96, :], op=ALU.mult), reads=[qn, cst], writes=[qn])
                cx.op("dve", lambda e: e.tensor_tensor(out=qn[64:96, :], in0=qn[64:96, :], in1=rtmp[64:96, :], op=ALU.add), reads=[qn, rtmp], writes=[qn])
                cx.op("act", lambda e: e.activation(out=qb[:, dst_i, :], in_=qn[:], func=AF.Copy), reads=[qn], writes=[qb])

            for h in range(4):
                p = pring.next()

                def mmq(e, p=p, h=h):
                    e.matmul(p[0:96, :], wqu[:, 0, h * 96:(h + 1) * 96], cqn[:, 0, :], start=True, stop=False)
                    return e.matmul(p[0:96, :], wqu[:, 1, h * 96:(h + 1) * 96], cqn[:, 1, :], start=False, stop=True)
                cx.op("pe", mmq, reads=[wqu, cqn], writes=[p])
                norm_rope(p, V(l, 115)[0:96, :], h, None)
                p = pring.next()

                def mmk(e, p=p, h=h):
                    e.matmul(p[0:96, :], wkpad[:, h, :], cqn[:, 2, :], start=True, stop=False)
                    ins = None
                    for k in range(8):
                        ins = e.matmul(p[0:96, :], wkr[:, k, :], u2[:, k, :], start=False, stop=(k == 7))
                    return ins
                cx.op("pe", mmk, reads=[wkpad, wkr, cqn, u2], writes=[p])
                norm_rope(p, V(l, 116)[0:96, :], 4 + h, V(l, 67)[0:96, :])
            cx.dma("pool", mlaq[:, :, t0:t0 + TT].rearrange("h p t -> p h t"), qb[:, 0:4, :], reads=[qb])
            cx.dma("pool", mlak[:, :, t0:t0 + TT].rearrange("h p t -> p h t"), qb[:, 4:8, :], reads=[qb])
            for s4 in range(4):
                ts_ = slice(s4 * 128, (s4 + 1) * 128)
                p = pring.next()
                cx.op("pe", lambda e, p=p, ts_=ts_: e.matmul(p[:, 0:256], cqn[:, 2, ts_], wkvv[:], start=True, stop=True), reads=[cqn, wkvv], writes=[p])
                cx.op("act", lambda e, p=p, s4=s4: e.activation(out=st_tok[:, s4, 768:1024], in_=p[:, 0:256], func=AF.Copy), reads=[p], writes=[st_tok])
            cx.dma("pool", sbv[t0:t0 + TT, :].rearrange("(s p) c -> p s c", p=128), st_tok[:, :, 0:256], reads=[st_tok])
            cx.dma("pool", mlv[t0:t0 + TT, :].rearrange("(s p) c -> p s c", p=128), st_tok[:, :, 256:768], reads=[st_tok])
            cx.dma("pool", mlav[t0:t0 + TT, :].rearrange("(s p) c -> p s c", p=128), st_tok[:, :, 768:1024], reads=[st_tok])
            cx.dma("pool", mlif[t0:t0 + TT, :].rearrange("(s p) c -> p s c", p=128), st_if[:], reads=[st_if])
        cx.barrier()
        cx.pop()

    def p23(l):
        cx.push()
        KT = cx.sb("KT", [96, S], BF16)
        VV = cx.sb("VV", [128, NB, 64], BF16)
        VA = cx.sb("VA", [128, NB, 65], BF16)
        accs = cx.sb("accs", [65, TT], F32)
        sel = cx.sb("sel", [65, 64], F32)
        QTr = Ring([cx.sb("QT%d" % i, [96, TT], BF16) for i in range(2)])
        er = Ring([cx.sb("e%d" % i, [128, TT], F32) for i in range(3)])
        spr = Ring([cx.sb("sp%d" % i, [128, TT], F32) for i in range(5)])
        atr = Ring([cx.sb("at%d" % i, [128, TT], F32) for i in range(3)])
        lkr = Ring([cx.sb("lk%d" % i, [128, TT], BF16) for i in range(5)])
        wr = Ring([cx.sb("w%d" % i, [128, TT], BF16) for i in range(5)])
        csr = Ring([cx.sb("CS%d" % i, [128, TT], BF16) for i in range(6)])
        csh = {}
        obr = Ring([cx.sb("ob%d" % i, [64, TT], BF16) for i in range(2)])
        recb = cx.sb("recb", [64, TT], F32)
        ring5 = Ring(pbanks[0:5])
        pzr = Ring(pbanks[0:3])
        plr = Ring(pbanks[3:5])
        pacc = [pbanks[5], pbanks[6]]

        def run_pipe_(units, nst):
            n = len(units)
            for i in range(n + nst - 1):
                for s in range(nst):
                    k = i - s
                    if 0 <= k < n:
                        units[k][s]()

        def p2_sb(l):
            for h in range(4):
                cx.dma("sp", KT[0:64, :], sbk[h * 64:(h + 1) * 64, :], writes=[KT])
                cx.dma("sp", VV[:], sbv[:, h * 64:(h + 1) * 64].rearrange("(n p) c -> p n c", p=128), writes=[VV])
                for c0 in range(0, S, 1024):
                    cx.op("dve", lambda e, c0=c0: e.tensor_scalar(out=KT[0:64, c0:c0 + 1024], in0=KT[0:64, c0:c0 + 1024], scalar1=0.125, scalar2=None, op0=ALU.mult),
                          reads=[KT], writes=[KT])
                units = []
                for T in range(NT):
                    QT = QTr.next()
                    po = pacc[T % 2]
                    nlist = list(range(4 * T + 3, -1, -1))
                    for ui, n in enumerate(nlist):
                        st = {}
                        first = ui == 0
                        last = ui == len(nlist) - 1
                        jj = n - 4 * T

                        def A1(st=st, n=n, first=first, QT=QT, T=T):
                            if first:
                                cx.dma("sp", QT[0:64, :], sbq[h * 64:(h + 1) * 64, T * TT:(T + 1) * TT], writes=[QT])
                            pz = pzr.next()
                            cx.op("pe", lambda e: e.matmul(pz[:], KT[0:64, n * 128:(n + 1) * 128], QT[0:64, :], start=True, stop=True), reads=[KT, QT], writes=[pz])
                            et_ = er.next()
                            cx.op("act", lambda e: e.activation(out=et_[:], in_=pz[:], func=AF.Exp, scale=-1.0), reads=[pz], writes=[et_])
                            st["pz"] = pz
                            st["e"] = et_

                        def A2(st=st, jj=jj, first=first):
                            pz, et_ = st["pz"], st["e"]
                            spt = spr.next()
                            lkb = lkr.next()
                            cx.op("act", lambda e: e.activation(out=spt[:], in_=et_[:], func=AF.Ln, bias=1.0), reads=[et_], writes=[spt])
                            cx.op("dve", lambda e: e.scalar_tensor_tensor(out=lkb[:], in0=pz[:], scalar=-1.0, in1=spt[:], op0=ALU.mult, op1=ALU.subtract),
                                  reads=[pz, spt], writes=[lkb])
                            if jj >= 0:
                                cx.op("dve", lambda e: e.tensor_tensor(out=lkb[:], in0=lkb[:], in1=SBMB(jj), op=ALU.mult), reads=[lkb, cb16], writes=[lkb])
                            st["lk"] = lkb
                            st["sp"] = spt

                        def A3(st=st, first=first):
                            lkb = st["lk"]
                            st["cs_prev"] = None if first else csh["cur"]
                            if first:
                                csh["cur"] = lkb
                            else:
                                cs_new = csr.next()
                                cp = csh["cur"]
                                cx.op("dve", lambda e: e.tensor_tensor(out=cs_new[:], in0=cp[:], in1=lkb[:], op=ALU.add), reads=[cp, lkb], writes=[cs_new])
                                csh["cur"] = cs_new

                        def B(st=st, jj=jj, first=first, n=n, QT=QT):
                            pl = plr.next()
                            st["pl"] = pl
                            lkb = st["lk"]
                            CS = st["cs_prev"]

                            def mm(e):
                                nmm = 1 + (0 if first else 1) + (1 if jj >= 0 else 0)
                                k = 0
                                ins = e.matmul(pl[:], TRIB, lkb[:], start=True, stop=(nmm == 1))
                                k += 1
                                if not first:
                                    k += 1
                                    ins = e.matmul(pl[:], ONESB, CS[:], start=False, stop=(k == nmm))
                                if jj >= 0:
                                    k += 1
                                    ins = e.matmul(pl[:], IDB, NEGB(jj), start=False, stop=(k == nmm))
                                return ins
                            cx.op("pe", mm, reads=[lkb, cb16] + ([] if first else [CS]), writes=[pl])

                        def B2a(st=st):
                            pl, spt = st["pl"], st["sp"]
                            at = atr.next()
                            st["at"] = at
                            cx.op("dve", lambda e: e.tensor_tensor(out=at[:], in0=pl[:], in1=spt[:], op=ALU.subtract), reads=[pl, spt], writes=[at])

                        def B2b(st=st):
                            at = st["at"]
                            wt = wr.next()
                            cx.op("act", lambda e: e.activation(out=wt[:], in_=at[:], func=AF.Exp), reads=[at], writes=[wt])
                            st["w"] = wt

                        def C(st=st, n=n, first=first, last=last, po=po, T=T):
                            wt = st["w"]
                            cx.op("pe", lambda e: e.matmul(po[0:64, :], VV[:, n, :], wt[:], start=first, stop=last), reads=[VV, wt], writes=[po])
                            if last:
                                ob = obr.next()
                                cx.op("act", lambda e: e.activation(out=ob[:], in_=po[0:64, :], func=AF.Copy), reads=[po], writes=[ob])
                                cx.dma("pool", ysb[h * 64:(h + 1) * 64, T * TT:(T + 1) * TT], ob[:], reads=[ob])
                        units.append({"A1": A1, "A2": A2, "A3": A3, "B1": B, "B2a": B2a, "B2b": B2b, "C": C})
                nu = len(units)
                for i in range(nu + 4):
                    for name, lag in (("B2a", 3), ("A1", 0), ("A2", 1), ("B2b", 3), ("A3", 2), ("B1", 2), ("C", 4)):
                        k = i - lag
                        if 0 <= k < nu:
                            units[k][name]()
            cx.barrier()

        def p3_mla(l):
            sc = 96.0 ** -0.5
            cx.op("dve", lambda e: e.memset(VA[:], 1.0), writes=[VA])
            cx.op("dve", lambda e: e.memset(sel[:], 0.0), writes=[sel])
            cx.op("dve", lambda e: e.memset(sel[64:65, :], 1.0), writes=[sel])
            for h in range(4):
                cx.dma("sp", KT[:], mlak[h], writes=[KT])
                cx.dma("sp", VA[:, :, 0:64], mlav[:, h * 64:(h + 1) * 64].rearrange("(n p) c -> p n c", p=128), writes=[VA])
                units = []
                for T in range(NT):
                    QT = QTr.next()
                    nlist = list(range(0, 4 * T + 4))
                    for ui, n in enumerate(nlist):
                        st = {}
                        first = ui == 0
                        last = ui == len(nlist) - 1
                        jj = n - 4 * T

                        def A(st=st, n=n, first=first, QT=QT, T=T, jj=jj):
                            if first:
                                cx.dma("sp", QT[:], mlaq[h, :, T * TT:(T + 1) * TT], writes=[QT])
                            pz = ring5.next()
                            cx.op("pe", lambda e: e.matmul(pz[:], KT[:, n * 128:(n + 1) * 128], QT[:], start=True, stop=True), reads=[KT, QT], writes=[pz])
                            wt = wr.next()
                            cx.op("act", lambda e: e.activation(out=wt[:], in_=pz[:], func=AF.Exp, scale=sc), reads=[pz], writes=[wt])
                            if jj >= 0:
                                cx.op("dve", lambda e: e.tensor_tensor(out=wt[:], in0=wt[:], in1=MLAMB(jj), op=ALU.mult), reads=[wt, cb16], writes=[wt])
                            st["w"] = wt

                        def B(st=st, n=n, first=first, last=last, T=T):
                            wt = st["w"]
                            pa = pacc[T % 2]
                            cx.op("pe", lambda e: e.matmul(pa[0:65, :], VA[:, n, :], wt[:], start=first, stop=last), reads=[VA, wt], writes=[pa])
                            if last:
                                ob = obr.next()
                                cx.op("act", lambda e: e.activation(out=accs[:], in_=pa[0:65, :], func=AF.Copy), reads=[pa], writes=[accs])
                                pb = ring5.next()
                                cx.op("pe", lambda e: e.matmul(pb[0:64, :], sel[:], accs[:], start=True, stop=True), reads=[sel, accs], writes=[pb])
                                cx.op("act", lambda e: e.activation(out=recb[:], in_=pb[0:64, :], func=AF.Ln), reads=[pb], writes=[recb])
                                cx.op("act", lambda e: e.activation(out=recb[:], in_=recb[:], func=AF.Exp, scale=-1.0), reads=[recb], writes=[recb])
                                cx.op("dve", lambda e: e.tensor_tensor(out=ob[:], in0=accs[0:64, :], in1=recb[:], op=ALU.mult), reads=[accs, recb], writes=[ob])
                                cx.dma("pool", ymla[h * 64:(h + 1) * 64, T * TT:(T + 1) * TT], ob[:], reads=[ob])
                        units.append((A, B))
                run_pipe_(units, 2)
            cx.barrier()

        p2_sb(l)
        p3_mla(l)
        cx.pop()

    def p4_ml(l):
        cx.push()
        if os.environ.get("P4_LIMIT"):
            cx.limit = int(os.environ["P4_LIMIT"])
        mQ = cx.sb("mQ", [128, 4, TT], BF16)
        mK = cx.sb("mK", [128, 4, TT], BF16)
        mV = cx.sb("mV", [128, 4, 512], BF16)
        mO = cx.sb("mO", [128, 4, TT], BF16)
        mIF = cx.sb("mIF", [128, 4, 8], F32)
        bcol = cx.sb("bcol", [128, 4], F32)
        wend = cx.sb("wend", [128, 4], F32)
        decay = cx.sb("decay", [128, 4], F32)
        Bm = cx.sb("Bm", [128, 4, 128], F32)
        Am = cx.sb("Am", [128, 4, 128], F32)
        Gt = cx.sb("Gt", [128, 4, 128], F32)
        Pm = cx.sb("Pm", [128, 4, 128], BF16)
        qa = cx.sb("qa", [128, 4, 128], BF16)
        kw = cx.sb("kw", [128, 4, 128], BF16)
        C32 = cx.sb("C32", [128, 4, 128], F32)
        Cb = cx.sb("Cb", [128, 4, 128], BF16)
        n32 = cx.sb("n32", [128, 4], F32)
        Nb = cx.sb("Nb", [128, 4, 128], BF16)
        dn = cx.sb("dn", [128, 4, 128], F32)
        HT = cx.sb("HT", [128, 4, 128], F32)
        hsq = cx.sb("hsq", [128, 4, 128], BF16)
        hr = cx.sb("hr", [128, 4, 128], F32)
        yst = cx.sb("yst", [128, 4, TT], BF16)
        onecol = cx.sb("onecol", [128, 1], BF16)
        psm = pbanks[4]

        cx.op("dve", lambda e: e.memset(C32[:], 0.0), writes=[C32])
        cx.op("dve", lambda e: e.memset(Cb[:], 0.0), writes=[Cb])
        cx.op("dve", lambda e: e.memset(n32[:], 0.0), writes=[n32])
        cx.op("dve", lambda e: e.memset(Nb[:], 0.0), writes=[Nb])
        cx.op("dve", lambda e: e.memset(onecol[:], 1.0), writes=[onecol])
        r4 = Ring(pbanks[0:4])
        for t in range(NT):
            t0 = t * TT
            cx.dma("sp", mQ[:], mlq[:, t0:t0 + TT].rearrange("(h p) t -> p h t", p=128), writes=[mQ])
            cx.dma("sp", mK[:], mlk[:, t0:t0 + TT].rearrange("(h p) t -> p h t", p=128), writes=[mK])
            cx.dma("sp", mV[:], mlv[t0:t0 + TT, :].rearrange("(s p) c -> p s c", p=128), writes=[mV])
            cx.dma("sp", mO[:], mlo[:, t0:t0 + TT].rearrange("(h p) t -> p h t", p=128), writes=[mO])
            cx.dma("sp", mIF[:], mlif[t0:t0 + TT, :].rearrange("(s p) c -> p s c", p=128), writes=[mIF])
            for c4 in range(4):
                cs = slice(c4 * 128, (c4 + 1) * 128)
                cx.op("pe", lambda e, c4=c4: e.matmul(psm[:, 0:4], cc("upper"), mIF[:, c4, 4:8], start=True, stop=True), reads=[consts, mIF], writes=[psm])
                cx.op("dve", lambda e, c4=c4: e.tensor_tensor(out=bcol[:], in0=mIF[:, c4, 0:4], in1=psm[:, 0:4], op=ALU.subtract), reads=[mIF, psm], writes=[bcol])
                cx.op("dve", lambda e, c4=c4: e.tensor_tensor(out=Bm[:], in0=cc("upper").unsqueeze(1).to_broadcast([128, 4, 128]),
                                                              in1=mIF[:, c4, 4:8].unsqueeze(2).to_broadcast([128, 4, 128]), op=ALU.mult),
                      reads=[consts, mIF], writes=[Bm])
                pR = r4.next()
                pR2 = r4.next()
                Bf = Bm[:].rearrange("p h t -> p (h t)")
                cx.op("pe", lambda e, pR=pR: e.matmul(pR[:], ones32[:], Bf, start=True, stop=True), reads=[ones32, Bm], writes=[pR])

                def mmR2(e, pR2=pR2):
                    e.matmul(pR2[:], ones32[:], Bf, start=True, stop=False)
                    return e.matmul(pR2[:], cc("ident"), cc("negm4"), start=False, stop=True)
                cx.op("pe", mmR2, reads=[ones32, Bm, consts], writes=[pR2])
                cx.op("act", lambda e, pR=pR: e.activation(out=Am[:].rearrange("p h t -> p (h t)"), in_=pR[:], func=AF.Exp), reads=[pR], writes=[Am])
                for h in range(4):
                    cx.op("act", lambda e, h=h, pR2=pR2: e.activation(out=Gt[:, h, :], in_=pR2[:, h * 128:(h + 1) * 128], func=AF.Exp, bias=bcol[:, h:h + 1]),
                          reads=[pR2, bcol], writes=[Gt])
                for h in range(4):
                    cx.op("act", lambda e, h=h, pR=pR: e.activation(out=decay[:, h:h + 1], in_=pR[:, h * 128 + 127:h * 128 + 128], func=AF.Exp), reads=[pR], writes=[decay])
                cx.op("act", lambda e: e.activation(out=wend[:], in_=bcol[:], func=AF.Exp), reads=[bcol], writes=[wend])
                cx.op("dve", lambda e: e.tensor_tensor(out=wend[:], in0=wend[:], in1=decay[:], op=ALU.mult), reads=[wend, decay], writes=[wend])
                pS = r4.next()

                def mmS(e, pS=pS, cs=cs):
                    ins = None
                    for h in range(4):
                        ins = e.matmul(pS[:, h * 128:(h + 1) * 128], mK[:, h, cs], mQ[:, h, cs], start=True, stop=True)
                    return ins
                cx.op("pe", mmS, reads=[mK, mQ], writes=[pS])
                cx.op("dve", lambda e, pS=pS: e.tensor_tensor(out=Pm[:].rearrange("p h t -> p (h t)"), in0=pS[:], in1=Gt[:].rearrange("p h t -> p (h t)"), op=ALU.mult),
                      reads=[pS, Gt], writes=[Pm])
                cx.op("dve", lambda e, cs=cs: e.tensor_tensor(out=qa[:], in0=mQ[:, :, cs], in1=Am[:], op=ALU.mult), reads=[mQ, Am], writes=[qa])
                pN = r4.next()
                pD = pbanks[5]

                def mmN(e, pN=pN, c4=c4):
                    ins = None
                    for h in range(4):
                        e.matmul(pN[:, h * 128:(h + 1) * 128], mV[:, c4, h * 128:(h + 1) * 128], Pm[:, h, :], start=True, stop=False)
                        ins = e.matmul(pN[:, h * 128:(h + 1) * 128], Cb[:, h, :], qa[:, h, :], start=False, stop=True)
                    return ins
                cx.op("pe", mmN, reads=[mV, Pm, Cb, qa], writes=[pN])

                def mmD(e):
                    ins = None
                    for h in range(4):
                        e.matmul(pD[:, h * 128:(h + 1) * 128], ONESB, Pm[:, h, :], start=True, stop=False)
                        ins = e.matmul(pD[:, h * 128:(h + 1) * 128], Nb[:, h, :], qa[:, h, :], start=False, stop=True)
                    return ins
                cx.op("pe", mmD, reads=[cb16, Pm, Nb, qa], writes=[pD])
                dnf = dn[:].rearrange("p h t -> p (h t)")
                cx.op("act", lambda e: e.activation(out=dnf, in_=pD[:], func=AF.Abs), reads=[pD], writes=[dn])
                cx.op("dve", lambda e: e.tensor_scalar(out=dnf, in0=dnf, scalar1=1.0, scalar2=None, op0=ALU.max), reads=[dn], writes=[dn])
                cx.op("act", lambda e: e.activation(out=dnf, in_=dnf, func=AF.Ln), reads=[dn], writes=[dn])
                cx.op("act", lambda e: e.activation(out=dnf, in_=dnf, func=AF.Exp, scale=-1.0), reads=[dn], writes=[dn])
                HTf = HT[:].rearrange("p h t -> p (h t)")
                cx.op("dve", lambda e, pN=pN: e.tensor_tensor(out=HTf, in0=pN[:], in1=dnf, op=ALU.mult), reads=[pN, dn], writes=[HT])
                cx.op("pe", lambda e, cs=cs: [e.transpose(psb[:, h * 128:(h + 1) * 128], mK[:, h, cs], IDB) for h in range(4)][-1], reads=[mK, cb16], writes=[psb])
                cx.op("dve", lambda e: e.tensor_tensor(out=kw[:], in0=psb[:, 0:512].rearrange("p (h t) -> p h t", h=4), in1=wend[:].unsqueeze(2).to_broadcast([128, 4, 128]), op=ALU.mult),
                      reads=[psb, wend], writes=[kw])
                pC = r4.next()

                def mmC(e, pC=pC, c4=c4):
                    ins = None
                    for h in range(4):
                        ins = e.matmul(pC[:, h * 128:(h + 1) * 128], kw[:, h, :], mV[:, c4, h * 128:(h + 1) * 128], start=True, stop=True)
                    return ins
                cx.op("pe", mmC, reads=[kw, mV], writes=[pC])
                cx.op("pe", lambda e: [e.matmul(psm[:, 4 + h:5 + h], kw[:, h, :], onecol[:], start=True, stop=True) for h in range(4)][-1], reads=[kw, onecol], writes=[psm])
                for h in range(4):
                    cx.op("dve", lambda e, h=h, pC=pC: e.scalar_tensor_tensor(out=C32[:, h, :], in0=C32[:, h, :], scalar=decay[:, h:h + 1], in1=pC[:, h * 128:(h + 1) * 128],
                                                                      op0=ALU.mult, op1=ALU.add), reads=[C32, decay, pC], writes=[C32])
                cx.op("dve", lambda e: e.tensor_tensor(out=n32[:], in0=n32[:], in1=decay[:], op=ALU.mult), reads=[n32, decay], writes=[n32])
                cx.op("dve", lambda e: e.tensor_tensor(out=n32[:], in0=n32[:], in1=psm[:, 4:8], op=ALU.add), reads=[n32, psm], writes=[n32])
                cx.op("act", lambda e: e.activation(out=Cb[:], in_=C32[:], func=AF.Copy), reads=[C32], writes=[Cb])
                cx.op("dve", lambda e: e.tensor_tensor(out=Nb[:], in0=ones32[:].unsqueeze(1).to_broadcast([128, 4, 128]), in1=n32[:].unsqueeze(2).to_broadcast([128, 4, 128]), op=ALU.mult),
                      reads=[ones32, n32], writes=[Nb])
                cx.op("act", lambda e: e.activation(out=hsq[:], in_=HT[:], func=AF.Square), reads=[HT], writes=[hsq])
                pQ = r4.next()
                cx.op("pe", lambda e, pQ=pQ: e.matmul(pQ[:], ONESB, hsq[:].rearrange("p h t -> p (h t)"), start=True, stop=True), reads=[cb16, hsq], writes=[pQ])
                hrf = hr[:].rearrange("p h t -> p (h t)")
                cx.op("act", lambda e, pQ=pQ: e.activation(out=hrf, in_=pQ[:], func=AF.Ln, scale=1.0 / 128, bias=EPSB[:, 0:1]), reads=[pQ, epsb], writes=[hr])
                cx.op("act", lambda e: e.activation(out=hrf, in_=hrf, func=AF.Exp, scale=-0.5), reads=[hr], writes=[hr])
                for h in range(4):
                    cx.op("dve", lambda e, h=h: e.scalar_tensor_tensor(out=HT[:, h, :], in0=HT[:, h, :], scalar=V(l, 108 + h), in1=hr[:, h, :], op0=ALU.mult, op1=ALU.mult),
                          reads=[HT, hr, vecs], writes=[HT])
                cx.op("dve", lambda e, cs=cs: e.tensor_tensor(out=yst[:, :, cs], in0=HT[:], in1=mO[:, :, cs], op=ALU.mult), reads=[HT, mO], writes=[yst])
            cx.dma("pool", yml[:, t0:t0 + TT].rearrange("(h p) t -> p h t", p=128), yst[:], reads=[yst])
        cx.barrier()
        cx.pop()

    def p5(l, lastlayer):
        cx.push()
        alloc_common()
        xt = cm["xt"]
        yb = cx.sb("yb", [128, 8, TT], BF16)
        mg = cx.sb("mg", [128, 8, TT], BF16)
        macc = cx.sb("macc", [128, TT], F32)
        mtmp = cx.sb("mtmp", [128, TT], F32)

        st_g = cx.sb("gt", [128, 24, TT], BF16)
        for t in range(NT):
            t0 = t * TT
            cx.dma("sp", xt[:], xres[:, t0:t0 + TT].rearrange("(c p) t -> p c t", p=128), writes=[xt])
            cx.dma("sp", yb[:, 0:2, :], ysb[:, t0:t0 + TT].rearrange("(c p) t -> p c t", p=128), writes=[yb])
            cx.dma("sp", yb[:, 2:6, :], yml[:, t0:t0 + TT].rearrange("(c p) t -> p c t", p=128), writes=[yb])
            cx.dma("sp", yb[:, 6:8, :], ymla[:, t0:t0 + TT].rearrange("(c p) t -> p c t", p=128), writes=[yb])
            cx.dma("sp", st_g[:], gat[:, t0:t0 + TT].rearrange("(c p) t -> p c t", p=128), writes=[st_g])
            for g0 in range(0, D, 512):
                sls = [wload("w_up_sb", l, 0, 256, g0, g0 + 512), wload("w_up_ml", l, 0, 512, g0, g0 + 512), wload("w_up_mla", l, 0, 256, g0, g0 + 512)]
                for a in range(0, 512, 128):
                    m = (g0 + a) // 128
                    for bi, (kcn, yo) in enumerate(((2, 0), (4, 2), (2, 6))):
                        sl, view = sls[bi]
                        p = pring.next()

                        def mm(e, p=p, view=view, kcn=kcn, yo=yo, a=a):
                            ins = None
                            for k in range(kcn):
                                ins = e.matmul(p[:], view(k, a, a + 128), yb[:, yo + k, :], start=(k == 0), stop=(k == kcn - 1))
                            return ins
                        cx.op("pe", mm, reads=[sl, yb], writes=[p])
                        if bi == 0:
                            cx.op("dve", lambda e, p=p, m=m: e.tensor_tensor(out=macc[:], in0=p[:], in1=st_g[:, m, :], op=ALU.mult), reads=[p, st_g], writes=[macc])
                        else:
                            cx.op("dve", lambda e, p=p, m=m, bi=bi: e.tensor_tensor(out=mtmp[:], in0=p[:], in1=st_g[:, bi * 8 + m, :], op=ALU.mult), reads=[p, st_g], writes=[mtmp])
                            if bi == 1:
                                cx.op("dve", lambda e: e.tensor_tensor(out=macc[:], in0=macc[:], in1=mtmp[:], op=ALU.add), reads=[macc, mtmp], writes=[macc])
                            else:
                                cx.op("dve", lambda e, m=m: e.tensor_tensor(out=mg[:, m, :], in0=macc[:], in1=mtmp[:], op=ALU.add), reads=[macc, mtmp], writes=[mg])

            def epi_out(p, m, mw):
                cx.op("dve", lambda e: e.tensor_tensor(out=xt[:, m, :], in0=xt[:, m, :], in1=p[:], op=ALU.add), reads=[xt, p], writes=[xt])
            linear_fm("w_out", l, D, 0, D, mg, epi_out)
            ffn(l, "ffn2_wi", "ffn2_wo", 16)
            dst = yT_out if lastlayer else xres
            cx.dma("pool", dst[:, t0:t0 + TT].rearrange("(c p) t -> p c t", p=128), xt[:], reads=[xt])
        cx.barrier()
        cx.pop()

    for l in range(L):
        if "p1" in phases:
            p1(l, l == 0)
        if "p23" in phases:
            p23(l)
        if "p4" in phases:
            p4_ml(l)
        if "p5" in phases:
            p5(l, l == L - 1)
    if dbg is not None:
        src = {"sbq": sbq, "sbk": sbk, "sbv": sbv, "mlq": mlq, "mlk": mlk, "mlv": mlv, "mlo": mlo, "mlif": mlif, "mlaq": mlaq, "mlak": mlak,
               "mlav": mlav, "gat": gat, "ysb": ysb, "yml": yml, "ymla": ymla, "xres": xres, "cosd": cosd, "sind": sind}[dbg[0]]
        cx.dma("pool", dbg_out, src)
        cx.barrier()
    cx.barrier()
    return nc, cx.ninst, cx


NCONST = None
_CONSTS = None


def _get_consts():
    global NCONST, _CONSTS
    if _CONSTS is None:
        _CONSTS = build_consts()
        NCONST = _CONSTS[0].shape[1]
    return _CONSTS


WNAMES = ("ffn1_wi", "ffn1_wo", "w_in", "mla_wq_up", "mla_wkv_up", "w_up_sb", "w_up_ml", "w_up_mla", "w_out", "ffn2_wi", "ffn2_wo")


def make_in_map(inp, b, S, L):
    consts = _get_consts()
    m = {
        "xT": np.ascontiguousarray(inp["x"][b, :S].T),
        "pos": np.ascontiguousarray(inp["positions"][b:b + 1, :S]).astype(np.int32),
        "consts": consts[0],
        "masks": consts[1],
        "vecs": build_vecs(inp, L),
        "b_in": np.ascontiguousarray(inp["b_in"][:L]),
    }
    for k in WNAMES:
        m[k] = np.ascontiguousarray(inp[k][:L])
    return m


def kernel(**inputs):
    inp = {k: np.asarray(v) for k, v in inputs.items()}
    B, S, _ = inp["x"].shape
    L = inp["w_in"].shape[0]
    _get_consts()
    nc, ninst, cx = build_program(S, L)
    vec = build_vecs(inp, L)
    in_maps = []
    for b in range(B):
        m = make_in_map(inp, b, S, L)
        m["vecs"] = vec
        in_maps.append(m)
    res = run_bass_kernel_spmd(nc, in_maps, core_ids=list(range(B)))
    out = np.stack([np.ascontiguousarray(res.results[b]["yT"].T) for b in range(B)], axis=0)
    return out.astype(np.float32)
```

```python
from contextlib import ExitStack
import numpy as np
import concourse.bass as bass
import concourse.mybir as mybir
from concourse.bass_utils import run_bass_kernel_spmd

F32 = mybir.dt.float32
BF16 = mybir.dt.bfloat16
I32 = mybir.dt.int32
AF = mybir.ActivationFunctionType
ALU = mybir.AluOpType

D = 1024
FF = 1408
NIN = 6312
O_SBQ, O_SBK, O_SBV = 0, 256, 512
O_MLQ, O_MLK, O_MLV, O_MLO, O_MLI, O_MLF = 768, 1280, 1792, 2304, 2816, 2820
O_CQ, O_CKV, O_KR, O_G = 2824, 3080, 3208, 3240
EPS = 1e-6
TT = 512
NV_L = 120
CAST_ELEMS = 1 << 16
TWO_PI = 6.283185307179586
C1 = 6.28125
C2 = TWO_PI - C1


class Chan:
    def __init__(self, sem):
        self.sem = sem
        self.count = 0


class Buf:
    def __init__(self, ctx, t, name):
        self.ctx = ctx
        self.t = t
        self.name = name
        self.lw = None
        self.rd = []
        self.lchan = None
        self.schan = None

    def __getitem__(self, k):
        return self.t[k]


class Op:
    __slots__ = ("eng", "fn", "deps", "signal", "idx", "epoch")

    def __init__(self, eng, fn, deps):
        self.eng = eng
        self.fn = fn
        self.deps = deps
        self.signal = False
        self.idx = 0
        self.epoch = 0


class Ctx:
    ENG = ("pe", "act", "dve", "pool", "sp")

    def __init__(self, nc, es):
        self.nc = nc
        self.es = es
        self.e = {"pe": nc.tensor, "act": nc.scalar, "dve": nc.vector, "pool": nc.gpsimd, "sp": nc.sync}
        self.ops = []
        self.last = {k: None for k in self.ENG}
        self.bufs = []
        self.chans = []
        self.free_chans = {}
        self.stacks = [es]
        self.scope_bufs = [[]]
        self.nsem = 0
        self.ninst = 0
        self.limit = None
        self.cnt = {k: 0 for k in self.ENG}
        self.epoch = {k: 0 for k in self.ENG}
        self.sems = None
        self.seen_op = {k: {} for k in self.ENG}
        self.seen_ch = {k: {} for k in self.ENG}
        self.misc = self.new_chan("misc")
        self.out_chan = self.new_chan("outc")

    def sem(self, name):
        self.nsem += 1
        return self.es.enter_context(self.nc.semaphore(name))

    def new_chan(self, name, kind="x"):
        fl = self.free_chans.setdefault(kind, [])
        if fl:
            return fl.pop()
        c = Chan(self.sem("c_" + name))
        self.chans.append(c)
        return c

    def push(self):
        st = ExitStack()
        self.stacks.append(st)
        self.scope_bufs.append([])

    def pop(self):
        st = self.stacks.pop()
        for b in self.scope_bufs.pop():
            self.bufs.remove(b)
            if b.lchan is not None:
                self.free_chans.setdefault("l", []).append(b.lchan)
            if b.schan is not None:
                self.free_chans.setdefault("s", []).append(b.schan)
        st.close()

    def sb(self, name, shape, dt):
        self.uid = getattr(self, "uid", 0) + 1
        b = Buf(self, self.stacks[-1].enter_context(self.nc.sbuf_tensor("%s_s%d" % (name, self.uid), list(shape), dt)), name)
        self.bufs.append(b)
        self.scope_bufs[-1].append(b)
        return b

    def ps(self, name, shape, dt):
        b = Buf(self, self.es.enter_context(self.nc.psum_tensor(name + "_p", list(shape), dt)), name)
        self.bufs.append(b)
        return b

    def _deps(self, reads, writes, eng=None):
        deps = []
        for b in reads:
            if b.lw is not None:
                deps.append(b.lw)
        for b in writes:
            if b.lw is not None and not (b.lw[0] == "op" and b.lw[1].eng == eng):
                deps.append(b.lw)
            for r in b.rd:
                if not (r[0] == "op" and r[1].eng == eng):
                    deps.append(r)
        return deps

    def op(self, eng, fn, reads=(), writes=()):
        if self.limit is not None:
            self.limit -= 1
            if self.limit < 0:
                return None
        deps = self._deps(reads, writes, eng)
        o = Op(eng, fn, deps)
        tok = ("op", o)
        for b in writes:
            b.lw = tok
            b.rd = []
        for b in reads:
            if b not in writes:
                b.rd.append(tok)
                if len(b.rd) > 64:
                    b.rd = b.rd[-48:]
        self.ops.append(o)
        self.last[eng] = o
        return o

    def dma(self, q, out_ap, in_ap, reads=(), writes=(), chan=None):
        deps = self._deps(reads, writes)
        chans = []
        for b in writes:
            if b.lchan is None:
                b.lchan = self.new_chan("l_" + b.name, "l")
            chans.append(b.lchan)
        for b in reads:
            if b.schan is None:
                b.schan = self.new_chan("s_" + b.name, "s")
            chans.append(b.schan)
        if chan is not None:
            chans.append(chan)
        if not chans:
            chans = [self.misc]
        ch = chans[0]
        assert len(chans) == 1, "dma must touch exactly one tracked buf"
        ch.count += 16
        tok = ("dma", ch, ch.count)

        def fn(e, out_ap=out_ap, in_ap=in_ap, ch=ch):
            return e.dma_start(out=out_ap, in_=in_ap).then_inc(ch.sem, 16)

        o = Op(q, fn, deps)
        o.signal = None
        for b in writes:
            b.lw = tok
            b.rd = []
        for b in reads:
            b.rd.append(tok)
        self.ops.append(o)
        return o

    def barrier(self):
        toks = []
        for k in self.ENG:
            if self.last[k] is not None:
                toks.append(("op", self.last[k]))
        for c in self.chans:
            if c.count:
                toks.append(("dma", c, c.count))
        for k in self.ENG:
            o = Op(k, None, list(toks))
            o.signal = None
            self.ops.append(o)
            o.fn = "barrier"
        for b in self.bufs:
            b.lw = None
            b.rd = []
        self.ops.append("epoch")
        self.emit()
        self.ops = []
        self.last = {k: None for k in self.ENG}

    def emit(self):
        for o in self.ops:
            if o == "epoch":
                continue
            for d in o.deps:
                if d[0] == "op" and (d[1].eng != o.eng or o.eng != "pe") and d[1].signal is not None:
                    d[1].signal = True
        cnt = self.cnt
        epoch = self.epoch
        if self.sems is None:
            self.sems = {k: [self.sem("e_%s_0" % k)] for k in self.ENG}
        sems = self.sems
        for o in self.ops:
            if o == "epoch":
                for k in self.ENG:
                    if cnt[k] > 20000:
                        epoch[k] += 1
                        cnt[k] = 0
                        sems[k].append(self.sem("e_%s_%d" % (k, epoch[k])))
                continue
            if o.signal is True:
                cnt[o.eng] += 1
                o.idx = cnt[o.eng]
                o.epoch = epoch[o.eng]
        seen_op = self.seen_op
        seen_ch = self.seen_ch
        ninst = 0
        for o in self.ops:
            if o == "epoch":
                continue
            e = self.e[o.eng]
            need_op = {}
            need_ch = {}
            for d in o.deps:
                if d[0] == "op":
                    s = d[1]
                    if s.eng == o.eng and o.eng == "pe":
                        continue
                    if s.signal is not True:
                        continue
                    key = (s.eng, s.epoch)
                    if seen_op[o.eng].get(key, 0) >= s.idx:
                        continue
                    need_op[key] = max(need_op.get(key, 0), s.idx)
                else:
                    _, ch, c = d
                    if seen_ch[o.eng].get(ch, 0) >= c:
                        continue
                    need_ch[ch] = max(need_ch.get(ch, 0), c)
            for key, v in need_op.items():
                e.wait_ge(sems[key[0]][key[1]], v)
                seen_op[o.eng][key] = v
                ninst += 1
            for ch, v in need_ch.items():
                e.wait_ge(ch.sem, v)
                seen_ch[o.eng][ch] = v
                ninst += 1
            if o.fn == "barrier":
                continue
            ins = o.fn(e)
            ninst += 1
            if o.signal is True:
                ins.then_inc(sems[o.eng][o.epoch], 1)
        self.ninst += ninst
        return ninst


class Ring:
    def __init__(self, bufs):
        self.bufs = bufs
        self.i = 0

    def next(self):
        b = self.bufs[self.i % len(self.bufs)]
        self.i += 1
        return b


CONST_COLS = {}


def build_consts():
    cols = []
    off = 0

    def add(name, arr):
        nonlocal off
        a = np.zeros((128, arr.shape[1]), np.float32)
        a[: arr.shape[0]] = arr
        CONST_COLS[name] = (off, arr.shape[1])
        off += arr.shape[1]
        cols.append(a)

    j = np.arange(128)[:, None]
    s = np.arange(128)[None, :]
    add("tri", (j > s).astype(np.float32))
    add("upper", (j <= s).astype(np.float32))
    add("ident", (j == s).astype(np.float32))
    negm = np.where(j > s, -30000.0, 0.0).astype(np.float32)
    add("negm4", np.tile(negm, (1, 4)))
    masks = []
    for jj in range(4):
        ms = np.zeros((128, 512), np.float32)
        mi = np.zeros((128, 512), np.float32)
        for jq in range(4):
            if jq > jj:
                ms[:, jq * 128:(jq + 1) * 128] = 1.0
                mi[:, jq * 128:(jq + 1) * 128] = 1.0
            elif jq == jj:
                ms[:, jq * 128:(jq + 1) * 128] = (j < s)
                mi[:, jq * 128:(jq + 1) * 128] = (j <= s)
        masks.append((ms, mi))
    rot = np.zeros((96, 96), np.float32)
    for i in range(16):
        rot[80 + i, 64 + i] = -1.0
        rot[64 + i, 80 + i] = 1.0
    add("rot", rot)
    invf = np.power(np.float32(10000.0), -np.arange(16, dtype=np.float32) / np.float32(16)).astype(np.float32)
    add("invf", np.concatenate([invf, invf])[:, None])
    mk = np.concatenate([m[0] for m in masks] + [m[1] for m in masks] + [(1.0 - m[0]) * -30000.0 for m in masks], axis=1)
    return np.concatenate(cols, axis=1), mk


def col_layout(v):
    n = v.shape[0] // 128
    return v.reshape(n, 128).T


def build_vecs(inp, L):
    out = np.zeros((128, L * NV_L), np.float32)
    for l in range(L):
        o = l * NV_L
        b = inp["b_in"][l]

        def put(off, arr):
            out[: arr.shape[0], o + off: o + off + arr.shape[1]] = arr

        put(0, col_layout(inp["ffn1_norm"][l]))
        put(8, col_layout(inp["mix_norm"][l]))
        put(16, col_layout(inp["ffn2_norm"][l]))
        put(24, col_layout(b[O_SBQ:O_SBQ + 256]))
        put(26, col_layout(b[O_SBK:O_SBK + 256]))
        put(28, col_layout(b[O_MLQ:O_MLQ + 512]))
        put(32, col_layout(b[O_MLK:O_MLK + 512]))
        put(36, col_layout(b[O_MLO:O_MLO + 512]))
        put(40, col_layout(b[O_CQ:O_CQ + 256]))
        put(42, col_layout(b[O_CKV:O_CKV + 128]))
        put(43, col_layout(b[O_G:O_G + 3072]))
        kr = np.zeros((96, 1), np.float32)
        kr[64:96, 0] = b[O_KR:O_KR + 32]
        put(67, kr)
        cw = inp["ml_conv_w"][l]
        for c in range(8):
            put(68 + c * 4, cw[:, c * 128:(c + 1) * 128].T)
        put(100, col_layout(inp["ml_conv_b"][l]))
        put(108, col_layout(inp["ml_out_norm"][l]))
        put(112, col_layout(inp["mla_q_norm"][l]))
        put(114, col_layout(inp["mla_kv_norm"][l]))
        put(115, inp["mla_q_gain"][l][:, None])
        put(116, inp["mla_k_gain"][l][:, None])
    return out


def build_program(S, L, dbg=None, phases=("p1", "p23", "p4", "p5")):
    NT = S // TT
    NB = S // 128
    nc = bass.Bass("TRN2", target_bir_lowering=False)
    es = ExitStack()
    cx = Ctx(nc, es)

    def din(name, shape, dt=F32):
        return nc.dram_tensor(name, list(shape), dt, kind="ExternalInput").ap()

    def dscr(name, shape, dt):
        return nc.dram_tensor(name, list(shape), dt, kind="Internal").ap()

    xT_in = din("xT", [D, S])
    pos_in = din("pos", [1, S], I32)
    consts_in = din("consts", [128, NCONST])
    masks_in = din("masks", [128, 6144])
    vecs_in = din("vecs", [128, L * NV_L])
    W = {}
    wshapes = {"ffn1_wi": (D, 2 * FF), "ffn1_wo": (FF, D), "w_in": (D, NIN), "mla_wq_up": (256, 384),
               "mla_wkv_up": (128, 512), "w_up_sb": (256, D), "w_up_ml": (512, D), "w_up_mla": (256, D),
               "w_out": (D, D), "ffn2_wi": (D, 2 * FF), "ffn2_wo": (FF, D)}
    Wb = {}
    for k, (a, b) in wshapes.items():
        W[k] = din(k, [L, a, b])
        Wb[k] = dscr(k + "_b", [L, a, b], BF16)
    b_in_d = din("b_in", [L, NIN])
    yT_out = nc.dram_tensor("yT", [D, S], F32, kind="ExternalOutput").ap()
    dbg_out = None
    if dbg is not None:
        dbg_out = nc.dram_tensor("dbg", list(dbg[1]), dbg[2], kind="ExternalOutput").ap()

    xres = dscr("xres", [D, S], F32)
    sbq = dscr("sbq", [256, S], BF16)
    sbk = dscr("sbk", [256, S], BF16)
    sbv = dscr("sbv", [S, 256], BF16)
    mlq = dscr("mlq", [512, S], BF16)
    mlk = dscr("mlk", [512, S], BF16)
    mlv = dscr("mlv", [S, 512], BF16)
    mlo = dscr("mlo", [512, S], BF16)
    mlif = dscr("mlif", [S, 8], F32)
    mlaq = dscr("mlaq", [4, 96, S], BF16)
    mlak = dscr("mlak", [4, 96, S], BF16)
    mlav = dscr("mlav", [S, 256], BF16)
    gat = dscr("gat", [3072, S], BF16)
    ysb = dscr("ysb", [256, S], BF16)
    yml = dscr("yml", [512, S], BF16)
    ymla = dscr("ymla", [256, S], BF16)
    cosd = dscr("cosd", [32, S], F32)
    sind = dscr("sind", [32, S], F32)

    consts = cx.sb("consts", [128, NCONST], F32)
    vecs = cx.sb("vecs", [128, L * NV_L], F32)
    cb16 = cx.sb("cb16", [128, 128 * 3 + 512 * 12], BF16)
    ones32 = cx.sb("ones32", [128, 128], F32)
    pbanks = [cx.ps("ps%d" % i, [128, 512], F32) for i in range(7)]
    pring = Ring(pbanks)
    psb = cx.ps("psb", [128, 1024], BF16)

    def cc(name):
        o, n = CONST_COLS[name]
        return consts[:, o:o + n]

    ONESB = cb16[:, 0:128]
    TRIB = cb16[:, 128:256]
    IDB = cb16[:, 256:384]

    def SBMB(j):
        return cb16[:, 384 + j * 512: 384 + (j + 1) * 512]

    def MLAMB(j):
        return cb16[:, 384 + 2048 + j * 512: 384 + 2048 + (j + 1) * 512]

    def NEGB(j):
        return cb16[:, 384 + 4096 + j * 512: 384 + 4096 + (j + 1) * 512]

    cx.dma("sp", consts[:], consts_in, writes=[consts])
    cx.dma("sp", vecs[:], vecs_in, writes=[vecs])
    cx.op("dve", lambda e: e.memset(ones32[:], 1.0), writes=[ones32])
    cx.op("dve", lambda e: e.memset(cb16[:, 0:128], 1.0), writes=[cb16])
    cx.op("dve", lambda e: e.tensor_copy(out=cb16[:, 128:256], in_=cc("tri")), reads=[consts], writes=[cb16])
    cx.op("dve", lambda e: e.tensor_copy(out=cb16[:, 256:384], in_=cc("ident")), reads=[consts], writes=[cb16])
    cx.push()
    mk32 = cx.sb("mk32", [128, 6144], F32)
    cx.dma("sp", mk32[:], masks_in, writes=[mk32])
    cx.op("dve", lambda e: e.tensor_copy(out=cb16[:, 384:384 + 6144], in_=mk32[:]), reads=[mk32], writes=[cb16])
    import os
    for k, (a, b) in wshapes.items():
        if os.environ.get("SKIP_CAST"):
            break
        for l in range(L):
            rows = a
            step = max(1, min(rows, CAST_ELEMS // b))
            r0 = 0
            while r0 < rows:
                r1 = min(rows, r0 + step)
                cx.dma("pool", Wb[k][l, r0:r1, :], W[k][l, r0:r1, :])
                r0 = r1
    RC = 512
    posi = cx.sb("posi", [32, RC], I32)
    posf = cx.sb("posf", [32, RC], F32)
    rk = cx.sb("rk", [32, RC], F32)
    rt = cx.sb("rt", [32, RC], F32)
    rs = cx.sb("rs", [32, RC], F32)
    rc_ = cx.sb("rc", [32, RC], F32)
    invf = cc("invf")[0:32, :]
    MAGIC = 12582912.0
    for r0 in range(0, 0 if os.environ.get("SKIP_ROPE") else S, RC):
        cx.dma("sp", posi[:], pos_in[:, r0:r0 + RC].partition_broadcast(32), writes=[posi])
        cx.op("dve", lambda e: e.tensor_copy(out=posf[:], in_=posi[:]), reads=[posi], writes=[posf])
        cx.op("dve", lambda e: e.tensor_scalar(out=posf[:], in0=posf[:], scalar1=invf, scalar2=None, op0=ALU.mult),
              reads=[posf, consts], writes=[posf])
        for which, dst, shift, dd in (("s", rs, 0.0, sind), ("c", rc_, np.pi / 2, cosd)):
            def f1(e, shift=shift):
                return e.tensor_scalar(out=rk[:], in0=posf[:], scalar1=shift, scalar2=1.0 / TWO_PI, op0=ALU.add, op1=ALU.mult)
            cx.op("dve", f1, reads=[posf], writes=[rk])
            cx.op("dve", lambda e: e.tensor_scalar(out=rk[:], in0=rk[:], scalar1=MAGIC, scalar2=None, op0=ALU.add), reads=[rk], writes=[rk])
            cx.op("dve", lambda e: e.tensor_scalar(out=rk[:], in0=rk[:], scalar1=-MAGIC, scalar2=None, op0=ALU.add), reads=[rk], writes=[rk])
            cx.op("dve", lambda e: e.scalar_tensor_tensor(out=rt[:], in0=rk[:], scalar=-C1, in1=posf[:], op0=ALU.mult, op1=ALU.add),
                  reads=[rk, posf], writes=[rt])
            cx.op("dve", lambda e: e.scalar_tensor_tensor(out=rt[:], in0=rk[:], scalar=-C2, in1=rt[:], op0=ALU.mult, op1=ALU.add),
                  reads=[rk, rt], writes=[rt])
            if shift != 0.0:
                cx.op("dve", lambda e, shift=shift: e.tensor_scalar(out=rt[:], in0=rt[:], scalar1=shift, scalar2=None, op0=ALU.add),
                      reads=[rt], writes=[rt])
            cx.op("dve", lambda e: e.tensor_scalar(out=rt[:], in0=rt[:], scalar1=3.1415925, scalar2=-3.1415925, op0=ALU.min, op1=ALU.max),
                  reads=[rt], writes=[rt])
            cx.op("act", lambda e, dst=dst: e.activation(out=dst[:], in_=rt[:], func=AF.Sin), reads=[rt], writes=[dst])
            cx.dma("pool", dd[:, r0:r0 + RC], dst[:], reads=[dst])
    cx.barrier()
    cx.pop()

    cm = {}

    def alloc_common():
        cm["xt"] = cx.sb("xt", [128, 8, TT], F32)
        cm["sq"] = cx.sb("sq", [128, 8, TT], BF16)
        cm["rstd"] = cx.sb("rstd", [128, TT], F32)
        cm["u"] = cx.sb("u", [128, 8, TT], BF16)
        cm["hh"] = cx.sb("hh", [128, 11, TT], BF16)
        cm["tmpr"] = Ring([cx.sb("tmpf%d" % i, [128, TT], F32) for i in range(2)])
        cm["wring"] = Ring([cx.sb("wsl%d" % i, [128, 11 * 256], BF16) for i in range(6)])

    def V(l, off, n=1):
        return vecs[:, l * NV_L + off: l * NV_L + off + n]

    def wload(wname, l, r0, r1, c0, c1):
        sl = cm["wring"].next()
        kc = (r1 - r0 + 127) // 128
        ncol = c1 - c0
        rows = r1 - r0
        if rows % 128 == 0:
            dst = sl.t[:, 0:kc * ncol].rearrange("p (k c) -> p k c", k=kc)
            cx.dma("sp", dst, Wb[wname][l, r0:r1, c0:c1].rearrange("(k p) c -> p k c", p=128), writes=[sl])
        else:
            assert kc == 1
            dst = sl.t[0:rows, 0:ncol]
            cx.dma("sp", dst, Wb[wname][l, r0:r1, c0:c1], writes=[sl])

        def view(k, a, b):
            return sl.t[:, k * ncol + a: k * ncol + b]
        return sl, view

    def rmsnorm_fm(src, l, voff, dst, nch=8, dim=D, c_lo=0):
        sq, rstd = cm["sq"], cm["rstd"]
        cx.op("act", lambda e: e.activation(out=sq[:, 0:nch, :], in_=src[:, c_lo:c_lo + nch, :], func=AF.Square), reads=[src], writes=[sq])
        p = pring.next()

        def mm(e):
            ins = None
            for c in range(nch):
                ins = e.matmul(p[:], ONESB, sq[:, c, :], start=(c == 0), stop=(c == nch - 1))
            return ins
        cx.op("pe", mm, reads=[sq, cb16], writes=[p])
        cx.op("act", lambda e: e.activation(out=rstd[:], in_=p[:], func=AF.Ln, scale=1.0 / dim, bias=EPSB[:, 0:1]), reads=[p, epsb], writes=[rstd])
        cx.op("act", lambda e: e.activation(out=rstd[:], in_=rstd[:], func=AF.Exp, scale=-0.5), reads=[rstd], writes=[rstd])
        for c in range(nch):
            cx.op("dve", lambda e, c=c: e.scalar_tensor_tensor(out=dst[:, c_lo + c, :], in0=src[:, c_lo + c, :], scalar=V(l, voff + c), in1=rstd[:],
                                                               op0=ALU.mult, op1=ALU.mult), reads=[src, rstd, vecs], writes=[dst])

    epsb = cx.sb("epsb", [128, 1], F32)
    EPSB = epsb
    cx.op("dve", lambda e: e.memset(epsb[:], EPS), writes=[epsb])

    def linear_fm(wname, l, K, c0, c1, src, epi, group=256, wrows=None):
        kc = K // 128
        g0 = c0
        m = 0
        while g0 < c1:
            g1 = min(c1, g0 + group)
            sl, view = wload(wname, l, 0, K, g0, g1)
            a = 0
            while a < g1 - g0:
                mw = min(128, g1 - g0 - a)
                p = pring.next()

                def mm(e, a=a, mw=mw, p=p, view=view):
                    ins = None
                    for k in range(kc):
                        ins = e.matmul(p[0:mw, :], view(k, a, a + mw), src[:, k, :], start=(k == 0), stop=(k == kc - 1))
                    return ins
                cx.op("pe", mm, reads=[sl, src], writes=[p])
                epi(p, m, mw)
                m += 1
                a += mw
            g0 = g1

    def ffn(l, wi, wo, normoff):
        xt, u, hh, tmpr = cm["xt"], cm["u"], cm["hh"], cm["tmpr"]
        rmsnorm_fm(xt, l, normoff, u)
        for g0 in range(0, FF, 256):
            g1 = min(FF, g0 + 256)
            sla, va = wload(wi, l, 0, D, g0, g1)
            slg, vg = wload(wi, l, 0, D, FF + g0, FF + g1)
            for a in range(0, g1 - g0, 128):
                j = (g0 + a) // 128
                pa = pring.next()
                pg = pring.next()

                def mm(e, a=a, pa=pa, va=va):
                    ins = None
                    for k in range(8):
                        ins = e.matmul(pa[:], va(k, a, a + 128), u[:, k, :], start=(k == 0), stop=(k == 7))
                    return ins
                cx.op("pe", mm, reads=[sla, u], writes=[pa])

                def mm2(e, a=a, pg=pg, vg=vg):
                    ins = None
                    for k in range(8):
                        ins = e.matmul(pg[:], vg(k, a, a + 128), u[:, k, :], start=(k == 0), stop=(k == 7))
                    return ins
                cx.op("pe", mm2, reads=[slg, u], writes=[pg])
                t = tmpr.next()
                cx.op("act", lambda e, t=t, pa=pa: e.activation(out=t[:], in_=pa[:], func=AF.Silu), reads=[pa], writes=[t])
                cx.op("dve", lambda e, t=t, pg=pg, j=j: e.tensor_tensor(out=hh[:, j, :], in0=t[:], in1=pg[:], op=ALU.mult), reads=[t, pg], writes=[hh])
        for g0 in range(0, D, 256):
            sl, view = wload(wo, l, 0, FF, g0, g0 + 256)
            for a in range(0, 256, 128):
                m = (g0 + a) // 128
                p = pring.next()

                def mm(e, a=a, p=p, view=view):
                    ins = None
                    for k in range(11):
                        ins = e.matmul(p[:], view(k, a, a + 128), hh[:, k, :], start=(k == 0), stop=(k == 10))
                    return ins
                cx.op("pe", mm, reads=[sl, hh], writes=[p])
                cx.op("dve", lambda e, p=p, m=m: e.scalar_tensor_tensor(out=xt[:, m, :], in0=p[:], scalar=0.5, in1=xt[:, m, :], op0=ALU.mult, op1=ALU.add),
                      reads=[p, xt], writes=[xt])

    def p1(l, first):
        cx.push()
        alloc_common()
        xt, tmpr = cm["xt"], cm["tmpr"]
        u2 = cx.sb("u2", [128, 8, TT], BF16)
        st_qk = cx.sb("st_qk", [128, 4, TT], BF16)
        st_tok = cx.sb("st_tok", [128, 4, 1024], BF16)
        st_if = cx.sb("st_if", [128, 4, 8], F32)
        brow = cx.sb("brow", [128, 776], F32)
        xc = cx.sb("xc", [128, 8, 3 + TT], F32)
        cacc = cx.sb("cacc", [128, TT], F32)
        st_mqk = cx.sb("st_mqk", [128, 8, TT], BF16)
        st_o = cx.sb("st_o", [128, 4, TT], BF16)
        st_g = cx.sb("st_g", [128, 8, TT], BF16)
        cqf = cx.sb("cqf", [128, 3, TT], F32)
        cqn = cx.sb("cqn", [128, 3, TT], BF16)
        qn = cx.sb("qn", [96, TT], F32)
        qsq = cx.sb("qsq", [96, TT], F32)
        rtmp = cx.sb("rtmp", [96, TT], F32)
        qb = cx.sb("qb", [96, 8, TT], BF16)
        cst = cx.sb("cst", [96, TT], F32)
        snt = cx.sb("snt", [96, TT], F32)
        wkpad = cx.sb("wkpad", [128, 4, 96], BF16)
        wkr = cx.sb("wkr", [128, 8, 96], BF16)
        wqu = cx.sb("wqu", [128, 2, 384], BF16)
        wkvv = cx.sb("wkvv", [128, 256], BF16)
        et = cx.sb("et", [128, 4, 4], F32)

        src_x = xT_in if first else xres
        cx.op("pool", lambda e: e.memset(wkpad[:], 0.0), writes=[wkpad])
        cx.op("pool", lambda e: e.memset(wkr[:], 0.0), writes=[wkr])
        for h in range(4):
            cx.dma("sp", wkpad[:, h, 0:64], Wb["mla_wkv_up"][l, :, h * 128:h * 128 + 64], writes=[wkpad])
            cx.dma("sp", wkvv[:, h * 64:(h + 1) * 64], Wb["mla_wkv_up"][l, :, h * 128 + 64:h * 128 + 128], writes=[wkvv])
        cx.dma("sp", wkr[:, :, 64:96], Wb["w_in"][l, :, O_KR:O_KR + 32].rearrange("(k p) c -> p k c", p=128), writes=[wkr])
        cx.dma("sp", wqu[:], Wb["mla_wq_up"][l].rearrange("(k p) c -> p k c", p=128), writes=[wqu])
        cx.dma("sp", brow[:, 0:256], b_in_d[l:l + 1, O_SBV:O_SBV + 256].partition_broadcast(128), writes=[brow])
        cx.dma("sp", brow[:, 256:768], b_in_d[l:l + 1, O_MLV:O_MLV + 512].partition_broadcast(128), writes=[brow])
        cx.dma("sp", brow[:, 768:776], b_in_d[l:l + 1, O_MLI:O_MLI + 8].partition_broadcast(128), writes=[brow])
        cx.op("dve", lambda e: e.memset(xc[:, :, 0:3], 0.0), writes=[xc])
        for t in range(NT):
            t0 = t * TT
            cx.dma("sp", xt[:], src_x[:, t0:t0 + TT].rearrange("(c p) t -> p c t", p=128), writes=[xt])
            ffn(l, "ffn1_wi", "ffn1_wo", 0)
            cx.dma("pool", xres[:, t0:t0 + TT].rearrange("(c p) t -> p c t", p=128), xt[:], reads=[xt])
            rmsnorm_fm(xt, l, 8, u2)
            def epi_qk(p, m, mw, base=0, boff=24):
                cx.op("act", lambda e: e.activation(out=st_qk[:, base + m, :], in_=p[:], func=AF.Identity, bias=V(l, boff + m)),
                      reads=[p, vecs], writes=[st_qk])
            linear_fm("w_in", l, D, O_SBQ, O_SBQ + 256, u2, lambda p, m, mw: epi_qk(p, m, mw, 0, 24))
            linear_fm("w_in", l, D, O_SBK, O_SBK + 256, u2, lambda p, m, mw: epi_qk(p, m, mw, 2, 26))
            cx.dma("pool", sbq[:, t0:t0 + TT].rearrange("(c p) t -> p c t", p=128), st_qk[:, 0:2, :], reads=[st_qk])
            cx.dma("pool", sbk[:, t0:t0 + TT].rearrange("(c p) t -> p c t", p=128), st_qk[:, 2:4, :], reads=[st_qk])
            def epi_c(p, m, mw, base, boff):
                cx.op("act", lambda e: e.activation(out=xc[:, base + m, 3:3 + TT], in_=p[:], func=AF.Identity, bias=V(l, boff + m)),
                      reads=[p, vecs], writes=[xc])
            linear_fm("w_in", l, D, O_MLQ, O_MLQ + 512, u2, lambda p, m, mw: epi_c(p, m, mw, 0, 28))
            linear_fm("w_in", l, D, O_MLK, O_MLK + 512, u2, lambda p, m, mw: epi_c(p, m, mw, 4, 32))
            for c in range(8):
                cx.op("dve", lambda e, c=c: e.tensor_scalar(out=cacc[:], in0=xc[:, c, 0:TT], scalar1=V(l, 68 + c * 4 + 0), scalar2=None, op0=ALU.mult),
                      reads=[xc, vecs], writes=[cacc])
                for j in range(1, 4):
                    cx.op("dve", lambda e, c=c, j=j: e.scalar_tensor_tensor(out=cacc[:], in0=xc[:, c, j:j + TT], scalar=V(l, 68 + c * 4 + j), in1=cacc[:],
                                                                        op0=ALU.mult, op1=ALU.add), reads=[xc, vecs, cacc], writes=[cacc])
                if c < 4:
                    cx.op("act", lambda e, c=c: e.activation(out=st_mqk[:, c, :], in_=cacc[:], func=AF.Silu, bias=V(l, 100 + c)),
                          reads=[cacc, vecs], writes=[st_mqk])
                else:
                    tq = tmpr.next()
                    cx.op("act", lambda e, c=c, tq=tq: e.activation(out=tq[:], in_=cacc[:], func=AF.Silu, bias=V(l, 100 + c)),
                          reads=[cacc, vecs], writes=[tq])
                    cx.op("dve", lambda e, c=c, tq=tq: e.tensor_scalar(out=st_mqk[:, c, :], in0=tq[:], scalar1=128.0 ** -0.5, scalar2=None, op0=ALU.mult),
                          reads=[tq], writes=[st_mqk])
            cx.op("dve", lambda e: e.tensor_copy(out=xc[:, :, 0:3], in_=xc[:, :, TT:TT + 3]), reads=[xc], writes=[xc])
            cx.dma("pool", mlq[:, t0:t0 + TT].rearrange("(c p) t -> p c t", p=128), st_mqk[:, 0:4, :], reads=[st_mqk])
            cx.dma("pool", mlk[:, t0:t0 + TT].rearrange("(c p) t -> p c t", p=128), st_mqk[:, 4:8, :], reads=[st_mqk])
            def epi_o(p, m, mw):
                cx.op("act", lambda e: e.activation(out=st_o[:, m, :], in_=p[:], func=AF.Sigmoid, bias=V(l, 36 + m)), reads=[p, vecs], writes=[st_o])
            linear_fm("w_in", l, D, O_MLO, O_MLO + 512, u2, epi_o)
            cx.dma("pool", mlo[:, t0:t0 + TT].rearrange("(c p) t -> p c t", p=128), st_o[:], reads=[st_o])
            for gg in range(3):
                def epi_g(p, m, mw, gg=gg):
                    cx.op("act", lambda e: e.activation(out=st_g[:, m, :], in_=p[:], func=AF.Sigmoid, bias=V(l, 43 + gg * 8 + m)), reads=[p, vecs], writes=[st_g])
                linear_fm("w_in", l, D, O_G + gg * 1024, O_G + (gg + 1) * 1024, u2, epi_g)
                cx.dma("pool", gat[gg * 1024:(gg + 1) * 1024, t0:t0 + TT].rearrange("(c p) t -> p c t", p=128), st_g[:], reads=[st_g])
            slv, vv = wload("w_in", l, 0, D, O_SBV, O_SBV + 256)
            slm, vm = wload("w_in", l, 0, D, O_MLV, O_MLV + 256)
            slm2, vm2 = wload("w_in", l, 0, D, O_MLV + 256, O_MLV + 512)
            slg, vgt = wload("w_in", l, 0, D, O_MLI, O_MLI + 8)
            for s4 in range(4):
                ts_ = slice(s4 * 128, (s4 + 1) * 128)
                for (view, sl, ncol, so, bo) in ((vv, slv, 256, 0, 0), (vm, slm, 256, 256, 256), (vm2, slm2, 256, 512, 512)):
                    p = pring.next()

                    def mm(e, p=p, view=view, ncol=ncol, ts_=ts_):
                        ins = None
                        for k in range(8):
                            ins = e.matmul(p[:, 0:ncol], u2[:, k, ts_], view(k, 0, ncol), start=(k == 0), stop=(k == 7))
                        return ins
                    cx.op("pe", mm, reads=[sl, u2], writes=[p])
                    cx.op("dve", lambda e, p=p, ncol=ncol, so=so, bo=bo, s4=s4: e.tensor_tensor(out=st_tok[:, s4, so:so + ncol], in0=p[:, 0:ncol], in1=brow[:, bo:bo + ncol], op=ALU.add),
                          reads=[p, brow], writes=[st_tok])
                p = pring.next()

                def mm(e, p=p, ts_=ts_):
                    ins = None
                    for k in range(8):
                        ins = e.matmul(p[:, 0:8], u2[:, k, ts_], vgt(k, 0, 8), start=(k == 0), stop=(k == 7))
                    return ins
                cx.op("pe", mm, reads=[slg, u2], writes=[p])
                cx.op("dve", lambda e, p=p, s4=s4: e.tensor_tensor(out=st_if[:, s4, :], in0=p[:, 0:8], in1=brow[:, 768:776], op=ALU.add),
                      reads=[p, brow], writes=[st_if])
            cx.op("act", lambda e: e.activation(out=et[:], in_=st_if[:, :, 4:8], func=AF.Exp, scale=-1.0), reads=[st_if], writes=[et])
            cx.op("act", lambda e: e.activation(out=et[:], in_=et[:], func=AF.Ln, bias=1.0), reads=[et], writes=[et])
            cx.op("dve", lambda e: e.tensor_scalar(out=st_if[:, :, 4:8], in0=et[:], scalar1=-1.0, scalar2=None, op0=ALU.mult), reads=[et], writes=[st_if])
            def epi_cq(p, m, mw, base, boff):
                cx.op("act", lambda e: e.activation(out=cqf[:, base + m, :], in_=p[:], func=AF.Identity, bias=V(l, boff + m)), reads=[p, vecs], writes=[cqf])
            linear_fm("w_in", l, D, O_CQ, O_CQ + 256, u2, lambda p, m, mw: epi_cq(p, m, mw, 0, 40))
            linear_fm("w_in", l, D, O_CKV, O_CKV + 128, u2, lambda p, m, mw: epi_cq(p, m, mw, 2, 42))
            rmsnorm_fm(cqf, l, 112, cqn, nch=2, dim=256, c_lo=0)
            rmsnorm_fm(cqf, l, 114, cqn, nch=1, dim=128, c_lo=2)
            cx.dma("sp", cst[64:96, :], cosd[:, t0:t0 + TT], writes=[cst])
            cx.dma("sp", snt[64:96, :], sind[:, t0:t0 + TT], writes=[snt])

            def norm_rope(p, gcol, dst_i, bias):
                if bias is None:
                    cx.op("act", lambda e: e.activation(out=qn[:], in_=p[0:96, :], func=AF.Copy), reads=[p], writes=[qn])
                else:
                    cx.op("act", lambda e: e.activation(out=qn[:], in_=p[0:96, :], func=AF.Identity, bias=bias), reads=[p, vecs], writes=[qn])
                cx.op("act", lambda e: e.activation(out=qsq[:], in_=qn[:], func=AF.Square), reads=[qn], writes=[qsq])
                p2 = pring.next()
                cx.op("pe", lambda e: e.matmul(p2[0:96, :], ones32[0:96, 0:96], qsq[:], start=True, stop=True), reads=[qsq, ones32], writes=[p2])
                cx.op("act", lambda e: e.activation(out=rtmp[:], in_=p2[0:96, :], func=AF.Ln, scale=1.0 / 96, bias=EPSB[0:96, 0:1]), reads=[p2, epsb], writes=[rtmp])
                cx.op("act", lambda e: e.activation(out=rtmp[:], in_=rtmp[:], func=AF.Exp, scale=-0.5), reads=[rtmp], writes=[rtmp])
                cx.op("dve", lambda e: e.scalar_tensor_tensor(out=qn[:], in0=qn[:], scalar=gcol, in1=rtmp[:], op0=ALU.mult, op1=ALU.mult),
                      reads=[qn, rtmp, vecs], writes=[qn])
                p3 = pring.next()
                cx.op("pe", lambda e: e.matmul(p3[0:96, :], cc("rot")[0:96, :], qn[:], start=True, stop=True), reads=[qn, consts], writes=[p3])
                cx.op("dve", lambda e: e.tensor_tensor(out=rtmp[64:96, :], in0=p3[64:96, :], in1=snt[64:96, :], op=ALU.mult), reads=[p3, snt], writes=[rtmp])
                cx.op("dve", lambda e: e.tensor_tensor(out=qn[64:96, :], in0=qn[64:96, :], in1=cst[64:96, :], op=ALU.mult), reads=[qn, cst], writes=[qn])
                cx.op("dve", lambda e: e.tensor_tensor(out=qn[64:96, :], in0=qn[64:96, :], in1=rtmp[64:96, :], op=ALU.add), reads=[qn, rtmp], writes=[qn])
                cx.op("act", lambda e: e.activation(out=qb[:, dst_i, :], in_=qn[:], func=AF.Copy), reads=[qn], writes=[qb])

            for h in range(4):
                p = pring.next()

                def mmq(e, p=p, h=h):
                    e.matmul(p[0:96, :], wqu[:, 0, h * 96:(h + 1) * 96], cqn[:, 0, :], start=True, stop=False)
                    return e.matmul(p[0:96, :], wqu[:, 1, h * 96:(h + 1) * 96], cqn[:, 1, :], start=False, stop=True)
                cx.op("pe", mmq, reads=[wqu, cqn], writes=[p])
                norm_rope(p, V(l, 115)[0:96, :], h, None)
                p = pring.next()

                def mmk(e, p=p, h=h):
                    e.matmul(p[0:96, :], wkpad[:, h, :], cqn[:, 2, :], start=True, stop=False)
                    ins = None
                    for k in range(8):
                        ins = e.matmul(p[0:96, :], wkr[:, k, :], u2[:, k, :], start=False, stop=(k == 7))
                    return ins
                cx.op("pe", mmk, reads=[wkpad, wkr, cqn, u2], writes=[p])
                norm_rope(p, V(l, 116)[0:96, :], 4 + h, V(l, 67)[0:96, :])
            cx.dma("pool", mlaq[:, :, t0:t0 + TT].rearrange("h p t -> p h t"), qb[:, 0:4, :], reads=[qb])
            cx.dma("pool", mlak[:, :, t0:t0 + TT].rearrange("h p t -> p h t"), qb[:, 4:8, :], reads=[qb])
            for s4 in range(4):
                ts_ = slice(s4 * 128, (s4 + 1) * 128)
                p = pring.next()
                cx.op("pe", lambda e, p=p, ts_=ts_: e.matmul(p[:, 0:256], cqn[:, 2, ts_], wkvv[:], start=True, stop=True), reads=[cqn, wkvv], writes=[p])
                cx.op("act", lambda e, p=p, s4=s4: e.activation(out=st_tok[:, s4, 768:1024], in_=p[:, 0:256], func=AF.Copy), reads=[p], writes=[st_tok])
            cx.dma("pool", sbv[t0:t0 + TT, :].rearrange("(s p) c -> p s c", p=128), st_tok[:, :, 0:256], reads=[st_tok])
            cx.dma("pool", mlv[t0:t0 + TT, :].rearrange("(s p) c -> p s c", p=128), st_tok[:, :, 256:768], reads=[st_tok])
            cx.dma("pool", mlav[t0:t0 + TT, :].rearrange("(s p) c -> p s c", p=128), st_tok[:, :, 768:1024], reads=[st_tok])
            cx.dma("pool", mlif[t0:t0 + TT, :].rearrange("(s p) c -> p s c", p=128), st_if[:], reads=[st_if])
        cx.barrier()
        cx.pop()

    def p23(l):
        cx.push()
        KT = cx.sb("KT", [96, S], BF16)
        VV = cx.sb("VV", [128, NB, 64], BF16)
        VA = cx.sb("VA", [128, NB, 65], BF16)
        accs = cx.sb("accs", [65, TT], F32)
        sel = cx.sb("sel", [65, 64], F32)
        QTr = Ring([cx.sb("QT%d" % i, [96, TT], BF16) for i in range(2)])
        er = Ring([cx.sb("e%d" % i, [128, TT], F32) for i in range(3)])
        spr = Ring([cx.sb("sp%d" % i, [128, TT], F32) for i in range(5)])
        atr = Ring([cx.sb("at%d" % i, [128, TT], F32) for i in range(3)])
        lkr = Ring([cx.sb("lk%d" % i, [128, TT], BF16) for i in range(5)])
        wr = Ring([cx.sb("w%d" % i, [128, TT], BF16) for i in range(5)])
        csr = Ring([cx.sb("CS%d" % i, [128, TT], BF16) for i in range(6)])
        csh = {}
        obr = Ring([cx.sb("ob%d" % i, [64, TT], BF16) for i in range(2)])
        recb = cx.sb("recb", [64, TT], F32)
        ring5 = Ring(pbanks[0:5])
        pzr = Ring(pbanks[0:3])
        plr = Ring(pbanks[3:5])
        pacc = [pbanks[5], pbanks[6]]

        def run_pipe_(units, nst):
            n = len(units)
            for i in range(n + nst - 1):
                for s in range(nst):
                    k = i - s
                    if 0 <= k < n:
                        units[k][s]()

        def p2_sb(l):
            for h in range(4):
                cx.dma("sp", KT[0:64, :], sbk[h * 64:(h + 1) * 64, :], writes=[KT])
                cx.dma("sp", VV[:], sbv[:, h * 64:(h + 1) * 64].rearrange("(n p) c -> p n c", p=128), writes=[VV])
                for c0 in range(0, S, 1024):
                    cx.op("dve", lambda e, c0=c0: e.tensor_scalar(out=KT[0:64, c0:c0 + 1024], in0=KT[0:64, c0:c0 + 1024], scalar1=0.125, scalar2=None, op0=ALU.mult),
                          reads=[KT], writes=[KT])
                units = []
                for T in range(NT):
                    QT = QTr.next()
                    po = pacc[T % 2]
                    nlist = list(range(4 * T + 3, -1, -1))
                    for ui, n in enumerate(nlist):
                        st = {}
                        first = ui == 0
                        last = ui == len(nlist) - 1
                        jj = n - 4 * T

                        def A1(st=st, n=n, first=first, QT=QT, T=T):
                            if first:
                                cx.dma("sp", QT[0:64, :], sbq[h * 64:(h + 1) * 64, T * TT:(T + 1) * TT], writes=[QT])
                            pz = pzr.next()
                            cx.op("pe", lambda e: e.matmul(pz[:], KT[0:64, n * 128:(n + 1) * 128], QT[0:64, :], start=True, stop=True), reads=[KT, QT], writes=[pz])
                            et_ = er.next()
                            cx.op("act", lambda e: e.activation(out=et_[:], in_=pz[:], func=AF.Exp, scale=-1.0), reads=[pz], writes=[et_])
                            st["pz"] = pz
                            st["e"] = et_

                        def A2(st=st, jj=jj, first=first):
                            pz, et_ = st["pz"], st["e"]
                            spt = spr.next()
                            lkb = lkr.next()
                            cx.op("act", lambda e: e.activation(out=spt[:], in_=et_[:], func=AF.Ln, bias=1.0), reads=[et_], writes=[spt])
                            cx.op("dve", lambda e: e.scalar_tensor_tensor(out=lkb[:], in0=pz[:], scalar=-1.0, in1=spt[:], op0=ALU.mult, op1=ALU.subtract),
                                  reads=[pz, spt], writes=[lkb])
                            if jj >= 0:
                                cx.op("dve", lambda e: e.tensor_tensor(out=lkb[:], in0=lkb[:], in1=SBMB(jj), op=ALU.mult), reads=[lkb, cb16], writes=[lkb])
                            st["lk"] = lkb
                            st["sp"] = spt

                        def A3(st=st, first=first):
                            lkb = st["lk"]
                            st["cs_prev"] = None if first else csh["cur"]
                            if first:
                                csh["cur"] = lkb
                            else:
                                cs_new = csr.next()
                                cp = csh["cur"]
                                cx.op("pool", lambda e: e.tensor_tensor(out=cs_new[:], in0=cp[:], in1=lkb[:], op=ALU.add), reads=[cp, lkb], writes=[cs_new])
                                csh["cur"] = cs_new

                        def B(st=st, jj=jj, first=first, n=n, QT=QT):
                            pl = plr.next()
                            st["pl"] = pl
                            lkb = st["lk"]
                            CS = st["cs_prev"]

                            def mm(e):
                                nmm = 1 + (0 if first else 1) + (1 if jj >= 0 else 0)
                                k = 0
                                ins = e.matmul(pl[:], TRIB, lkb[:], start=True, stop=(nmm == 1))
                                k += 1
                                if not first:
                                    k += 1
                                    ins = e.matmul(pl[:], ONESB, CS[:], start=False, stop=(k == nmm))
                                if jj >= 0:
                                    k += 1
                                    ins = e.matmul(pl[:], IDB, NEGB(jj), start=False, stop=(k == nmm))
                                return ins
                            cx.op("pe", mm, reads=[lkb, cb16] + ([] if first else [CS]), writes=[pl])

                        def B2a(st=st):
                            pl, spt = st["pl"], st["sp"]
                            at = atr.next()
                            st["at"] = at
                            cx.op("dve", lambda e: e.tensor_tensor(out=at[:], in0=pl[:], in1=spt[:], op=ALU.subtract), reads=[pl, spt], writes=[at])

                        def B2b(st=st):
                            at = st["at"]
                            wt = wr.next()
                            cx.op("act", lambda e: e.activation(out=wt[:], in_=at[:], func=AF.Exp), reads=[at], writes=[wt])
                            st["w"] = wt

                        def C(st=st, n=n, first=first, last=last, po=po, T=T):
                            wt = st["w"]
                            cx.op("pe", lambda e: e.matmul(po[0:64, :], VV[:, n, :], wt[:], start=first, stop=last), reads=[VV, wt], writes=[po])
                            if last:
                                ob = obr.next()
                                cx.op("act", lambda e: e.activation(out=ob[:], in_=po[0:64, :], func=AF.Copy), reads=[po], writes=[ob])
                                cx.dma("pool", ysb[h * 64:(h + 1) * 64, T * TT:(T + 1) * TT], ob[:], reads=[ob])
                        units.append({"A1": A1, "A2": A2, "A3": A3, "B1": B, "B2a": B2a, "B2b": B2b, "C": C})
                nu = len(units)
                for i in range(nu + 4):
                    for name, lag in (("B2a", 3), ("A1", 0), ("A2", 1), ("B2b", 3), ("A3", 2), ("B1", 2), ("C", 4)):
                        k = i - lag
                        if 0 <= k < nu:
                            units[k][name]()
            cx.barrier()

        def p3_mla(l):
            sc = 96.0 ** -0.5
            cx.op("dve", lambda e: e.memset(VA[:], 1.0), writes=[VA])
            cx.op("dve", lambda e: e.memset(sel[:], 0.0), writes=[sel])
            cx.op("dve", lambda e: e.memset(sel[64:65, :], 1.0), writes=[sel])
            for h in range(4):
                cx.dma("sp", KT[:], mlak[h], writes=[KT])
                cx.dma("sp", VA[:, :, 0:64], mlav[:, h * 64:(h + 1) * 64].rearrange("(n p) c -> p n c", p=128), writes=[VA])
                units = []
                for T in range(NT):
                    QT = QTr.next()
                    nlist = list(range(0, 4 * T + 4))
                    for ui, n in enumerate(nlist):
                        st = {}
                        first = ui == 0
                        last = ui == len(nlist) - 1
                        jj = n - 4 * T

                        def A(st=st, n=n, first=first, QT=QT, T=T, jj=jj):
                            if first:
                                cx.dma("sp", QT[:], mlaq[h, :, T * TT:(T + 1) * TT], writes=[QT])
                            pz = ring5.next()
                            cx.op("pe", lambda e: e.matmul(pz[:], KT[:, n * 128:(n + 1) * 128], QT[:], start=True, stop=True), reads=[KT, QT], writes=[pz])
                            wt = wr.next()
                            cx.op("act", lambda e: e.activation(out=wt[:], in_=pz[:], func=AF.Exp, scale=sc), reads=[pz], writes=[wt])
                            if jj >= 0:
                                cx.op("dve", lambda e: e.tensor_tensor(out=wt[:], in0=wt[:], in1=MLAMB(jj), op=ALU.mult), reads=[wt, cb16], writes=[wt])
                            st["w"] = wt

                        def B(st=st, n=n, first=first, last=last, T=T):
                            wt = st["w"]
                            pa = pacc[T % 2]
                            cx.op("pe", lambda e: e.matmul(pa[0:65, :], VA[:, n, :], wt[:], start=first, stop=last), reads=[VA, wt], writes=[pa])
                            if last:
                                ob = obr.next()
                                cx.op("act", lambda e: e.activation(out=accs[:], in_=pa[0:65, :], func=AF.Copy), reads=[pa], writes=[accs])
                                pb = ring5.next()
                                cx.op("pe", lambda e: e.matmul(pb[0:64, :], sel[:], accs[:], start=True, stop=True), reads=[sel, accs], writes=[pb])
                                cx.op("act", lambda e: e.activation(out=recb[:], in_=pb[0:64, :], func=AF.Ln), reads=[pb], writes=[recb])
                                cx.op("act", lambda e: e.activation(out=recb[:], in_=recb[:], func=AF.Exp, scale=-1.0), reads=[recb], writes=[recb])
                                cx.op("dve", lambda e: e.tensor_tensor(out=ob[:], in0=accs[0:64, :], in1=recb[:], op=ALU.mult), reads=[accs, recb], writes=[ob])
                                cx.dma("pool", ymla[h * 64:(h + 1) * 64, T * TT:(T + 1) * TT], ob[:], reads=[ob])
                        units.append((A, B))
                run_pipe_(units, 2)
            cx.barrier()

        p2_sb(l)
        p3_mla(l)
        cx.pop()

    def p4_ml(l):
        cx.push()
        if os.environ.get("P4_LIMIT"):
            cx.limit = int(os.environ["P4_LIMIT"])
        mQ = cx.sb("mQ", [128, 4, TT], BF16)
        mK = cx.sb("mK", [128, 4, TT], BF16)
        mV = cx.sb("mV", [128, 4, 512], BF16)
        mO = cx.sb("mO", [128, 4, TT], BF16)
        mIF = cx.sb("mIF", [128, 4, 8], F32)
        bcol = cx.sb("bcol", [128, 4], F32)
        wend = cx.sb("wend", [128, 4], F32)
        decay = cx.sb("decay", [128, 4], F32)
        Bm = cx.sb("Bm", [128, 4, 128], F32)
        Am = cx.sb("Am", [128, 4, 128], F32)
        Gt = cx.sb("Gt", [128, 4, 128], F32)
        Pm = cx.sb("Pm", [128, 4, 128], BF16)
        qa = cx.sb("qa", [128, 4, 128], BF16)
        kw = cx.sb("kw", [128, 4, 128], BF16)
        C32 = cx.sb("C32", [128, 4, 128], F32)
        Cb = cx.sb("Cb", [128, 4, 128], BF16)
        n32 = cx.sb("n32", [128, 4], F32)
        Nb = cx.sb("Nb", [128, 4, 128], BF16)
        dn = cx.sb("dn", [128, 4, 128], F32)
        HT = cx.sb("HT", [128, 4, 128], F32)
        hsq = cx.sb("hsq", [128, 4, 128], BF16)
        hr = cx.sb("hr", [128, 4, 128], F32)
        yst = cx.sb("yst", [128, 4, TT], BF16)
        onecol = cx.sb("onecol", [128, 1], BF16)
        psm = pbanks[4]

        cx.op("dve", lambda e: e.memset(C32[:], 0.0), writes=[C32])
        cx.op("dve", lambda e: e.memset(Cb[:], 0.0), writes=[Cb])
        cx.op("dve", lambda e: e.memset(n32[:], 0.0), writes=[n32])
        cx.op("dve", lambda e: e.memset(Nb[:], 0.0), writes=[Nb])
        cx.op("dve", lambda e: e.memset(onecol[:], 1.0), writes=[onecol])
        r4 = Ring(pbanks[0:4])
        for t in range(NT):
            t0 = t * TT
            cx.dma("sp", mQ[:], mlq[:, t0:t0 + TT].rearrange("(h p) t -> p h t", p=128), writes=[mQ])
            cx.dma("sp", mK[:], mlk[:, t0:t0 + TT].rearrange("(h p) t -> p h t", p=128), writes=[mK])
            cx.dma("sp", mV[:], mlv[t0:t0 + TT, :].rearrange("(s p) c -> p s c", p=128), writes=[mV])
            cx.dma("sp", mO[:], mlo[:, t0:t0 + TT].rearrange("(h p) t -> p h t", p=128), writes=[mO])
            cx.dma("sp", mIF[:], mlif[t0:t0 + TT, :].rearrange("(s p) c -> p s c", p=128), writes=[mIF])
            for c4 in range(4):
                cs = slice(c4 * 128, (c4 + 1) * 128)
                cx.op("pe", lambda e, c4=c4: e.matmul(psm[:, 0:4], cc("upper"), mIF[:, c4, 4:8], start=True, stop=True), reads=[consts, mIF], writes=[psm])
                cx.op("dve", lambda e, c4=c4: e.tensor_tensor(out=bcol[:], in0=mIF[:, c4, 0:4], in1=psm[:, 0:4], op=ALU.subtract), reads=[mIF, psm], writes=[bcol])
                cx.op("dve", lambda e, c4=c4: e.tensor_tensor(out=Bm[:], in0=cc("upper").unsqueeze(1).to_broadcast([128, 4, 128]),
                                                              in1=mIF[:, c4, 4:8].unsqueeze(2).to_broadcast([128, 4, 128]), op=ALU.mult),
                      reads=[consts, mIF], writes=[Bm])
                pR = r4.next()
                pR2 = r4.next()
                Bf = Bm[:].rearrange("p h t -> p (h t)")
                cx.op("pe", lambda e, pR=pR: e.matmul(pR[:], ones32[:], Bf, start=True, stop=True), reads=[ones32, Bm], writes=[pR])

                def mmR2(e, pR2=pR2):
                    e.matmul(pR2[:], ones32[:], Bf, start=True, stop=False)
                    return e.matmul(pR2[:], cc("ident"), cc("negm4"), start=False, stop=True)
                cx.op("pe", mmR2, reads=[ones32, Bm, consts], writes=[pR2])
                cx.op("act", lambda e, pR=pR: e.activation(out=Am[:].rearrange("p h t -> p (h t)"), in_=pR[:], func=AF.Exp), reads=[pR], writes=[Am])
                for h in range(4):
                    cx.op("act", lambda e, h=h, pR2=pR2: e.activation(out=Gt[:, h, :], in_=pR2[:, h * 128:(h + 1) * 128], func=AF.Exp, bias=bcol[:, h:h + 1]),
                          reads=[pR2, bcol], writes=[Gt])
                for h in range(4):
                    cx.op("act", lambda e, h=h, pR=pR: e.activation(out=decay[:, h:h + 1], in_=pR[:, h * 128 + 127:h * 128 + 128], func=AF.Exp), reads=[pR], writes=[decay])
                cx.op("act", lambda e: e.activation(out=wend[:], in_=bcol[:], func=AF.Exp), reads=[bcol], writes=[wend])
                cx.op("dve", lambda e: e.tensor_tensor(out=wend[:], in0=wend[:], in1=decay[:], op=ALU.mult), reads=[wend, decay], writes=[wend])
                pS = r4.next()

                def mmS(e, pS=pS, cs=cs):
                    ins = None
                    for h in range(4):
                        ins = e.matmul(pS[:, h * 128:(h + 1) * 128], mK[:, h, cs], mQ[:, h, cs], start=True, stop=True)
                    return ins
                cx.op("pe", mmS, reads=[mK, mQ], writes=[pS])
                cx.op("dve", lambda e, pS=pS: e.tensor_tensor(out=Pm[:].rearrange("p h t -> p (h t)"), in0=pS[:], in1=Gt[:].rearrange("p h t -> p (h t)"), op=ALU.mult),
                      reads=[pS, Gt], writes=[Pm])
                cx.op("dve", lambda e, cs=cs: e.tensor_tensor(out=qa[:], in0=mQ[:, :, cs], in1=Am[:], op=ALU.mult), reads=[mQ, Am], writes=[qa])
                pN = r4.next()
                pD = pbanks[5]

                def mmN(e, pN=pN, c4=c4):
                    ins = None
                    for h in range(4):
                        e.matmul(pN[:, h * 128:(h + 1) * 128], mV[:, c4, h * 128:(h + 1) * 128], Pm[:, h, :], start=True, stop=False)
                        ins = e.matmul(pN[:, h * 128:(h + 1) * 128], Cb[:, h, :], qa[:, h, :], start=False, stop=True)
                    return ins
                cx.op("pe", mmN, reads=[mV, Pm, Cb, qa], writes=[pN])

                def mmD(e):
                    ins = None
                    for h in range(4):
                        e.matmul(pD[:, h * 128:(h + 1) * 128], ONESB, Pm[:, h, :], start=True, stop=False)
                        ins = e.matmul(pD[:, h * 128:(h + 1) * 128], Nb[:, h, :], qa[:, h, :], start=False, stop=True)
                    return ins
                cx.op("pe", mmD, reads=[cb16, Pm, Nb, qa], writes=[pD])
                dnf = dn[:].rearrange("p h t -> p (h t)")
                cx.op("act", lambda e: e.activation(out=dnf, in_=pD[:], func=AF.Abs), reads=[pD], writes=[dn])
                cx.op("dve", lambda e: e.tensor_scalar(out=dnf, in0=dnf, scalar1=1.0, scalar2=None, op0=ALU.max), reads=[dn], writes=[dn])
                cx.op("act", lambda e: e.activation(out=dnf, in_=dnf, func=AF.Ln), reads=[dn], writes=[dn])
                cx.op("act", lambda e: e.activation(out=dnf, in_=dnf, func=AF.Exp, scale=-1.0), reads=[dn], writes=[dn])
                HTf = HT[:].rearrange("p h t -> p (h t)")
                cx.op("dve", lambda e, pN=pN: e.tensor_tensor(out=HTf, in0=pN[:], in1=dnf, op=ALU.mult), reads=[pN, dn], writes=[HT])
                cx.op("pe", lambda e, cs=cs: [e.transpose(psb[:, h * 128:(h + 1) * 128], mK[:, h, cs], IDB) for h in range(4)][-1], reads=[mK, cb16], writes=[psb])
                cx.op("dve", lambda e: e.tensor_tensor(out=kw[:], in0=psb[:, 0:512].rearrange("p (h t) -> p h t", h=4), in1=wend[:].unsqueeze(2).to_broadcast([128, 4, 128]), op=ALU.mult),
                      reads=[psb, wend], writes=[kw])
                pC = r4.next()

                def mmC(e, pC=pC, c4=c4):
                    ins = None
                    for h in range(4):
                        ins = e.matmul(pC[:, h * 128:(h + 1) * 128], kw[:, h, :], mV[:, c4, h * 128:(h + 1) * 128], start=True, stop=True)
                    return ins
                cx.op("pe", mmC, reads=[kw, mV], writes=[pC])
                cx.op("pe", lambda e: [e.matmul(psm[:, 4 + h:5 + h], kw[:, h, :], onecol[:], start=True, stop=True) for h in range(4)][-1], reads=[kw, onecol], writes=[psm])
                for h in range(4):
                    cx.op("dve", lambda e, h=h, pC=pC: e.scalar_tensor_tensor(out=C32[:, h, :], in0=C32[:, h, :], scalar=decay[:, h:h + 1], in1=pC[:, h * 128:(h + 1) * 128],
                                                                      op0=ALU.mult, op1=ALU.add), reads=[C32, decay, pC], writes=[C32])
                cx.op("dve", lambda e: e.tensor_tensor(out=n32[:], in0=n32[:], in1=decay[:], op=ALU.mult), reads=[n32, decay], writes=[n32])
                cx.op("dve", lambda e: e.tensor_tensor(out=n32[:], in0=n32[:], in1=psm[:, 4:8], op=ALU.add), reads=[n32, psm], writes=[n32])
                cx.op("act", lambda e: e.activation(out=Cb[:], in_=C32[:], func=AF.Copy), reads=[C32], writes=[Cb])
                cx.op("dve", lambda e: e.tensor_tensor(out=Nb[:], in0=ones32[:].unsqueeze(1).to_broadcast([128, 4, 128]), in1=n32[:].unsqueeze(2).to_broadcast([128, 4, 128]), op=ALU.mult),
                      reads=[ones32, n32], writes=[Nb])
                cx.op("act", lambda e: e.activation(out=hsq[:], in_=HT[:], func=AF.Square), reads=[HT], writes=[hsq])
                pQ = r4.next()
                cx.op("pe", lambda e, pQ=pQ: e.matmul(pQ[:], ONESB, hsq[:].rearrange("p h t -> p (h t)"), start=True, stop=True), reads=[cb16, hsq], writes=[pQ])
                hrf = hr[:].rearrange("p h t -> p (h t)")
                cx.op("act", lambda e, pQ=pQ: e.activation(out=hrf, in_=pQ[:], func=AF.Ln, scale=1.0 / 128, bias=EPSB[:, 0:1]), reads=[pQ, epsb], writes=[hr])
                cx.op("act", lambda e: e.activation(out=hrf, in_=hrf, func=AF.Exp, scale=-0.5), reads=[hr], writes=[hr])
                for h in range(4):
                    cx.op("dve", lambda e, h=h: e.scalar_tensor_tensor(out=HT[:, h, :], in0=HT[:, h, :], scalar=V(l, 108 + h), in1=hr[:, h, :], op0=ALU.mult, op1=ALU.mult),
                          reads=[HT, hr, vecs], writes=[HT])
                cx.op("dve", lambda e, cs=cs: e.tensor_tensor(out=yst[:, :, cs], in0=HT[:], in1=mO[:, :, cs], op=ALU.mult), reads=[HT, mO], writes=[yst])
            cx.dma("pool", yml[:, t0:t0 + TT].rearrange("(h p) t -> p h t", p=128), yst[:], reads=[yst])
        cx.barrier()
        cx.pop()

    def p5(l, lastlayer):
        cx.push()
        alloc_common()
        xt = cm["xt"]
        yb = cx.sb("yb", [128, 8, TT], BF16)
        mg = cx.sb("mg", [128, 8, TT], BF16)
        macc = cx.sb("macc", [128, TT], F32)
        mtmp = cx.sb("mtmp", [128, TT], F32)

        st_g = cx.sb("gt", [128, 24, TT], BF16)
        for t in range(NT):
            t0 = t * TT
            cx.dma("sp", xt[:], xres[:, t0:t0 + TT].rearrange("(c p) t -> p c t", p=128), writes=[xt])
            cx.dma("sp", yb[:, 0:2, :], ysb[:, t0:t0 + TT].rearrange("(c p) t -> p c t", p=128), writes=[yb])
            cx.dma("sp", yb[:, 2:6, :], yml[:, t0:t0 + TT].rearrange("(c p) t -> p c t", p=128), writes=[yb])
            cx.dma("sp", yb[:, 6:8, :], ymla[:, t0:t0 + TT].rearrange("(c p) t -> p c t", p=128), writes=[yb])
            cx.dma("sp", st_g[:], gat[:, t0:t0 + TT].rearrange("(c p) t -> p c t", p=128), writes=[st_g])
            for g0 in range(0, D, 512):
                sls = [wload("w_up_sb", l, 0, 256, g0, g0 + 512), wload("w_up_ml", l, 0, 512, g0, g0 + 512), wload("w_up_mla", l, 0, 256, g0, g0 + 512)]
                for a in range(0, 512, 128):
                    m = (g0 + a) // 128
                    for bi, (kcn, yo) in enumerate(((2, 0), (4, 2), (2, 6))):
                        sl, view = sls[bi]
                        p = pring.next()

                        def mm(e, p=p, view=view, kcn=kcn, yo=yo, a=a):
                            ins = None
                            for k in range(kcn):
                                ins = e.matmul(p[:], view(k, a, a + 128), yb[:, yo + k, :], start=(k == 0), stop=(k == kcn - 1))
                            return ins
                        cx.op("pe", mm, reads=[sl, yb], writes=[p])
                        if bi == 0:
                            cx.op("dve", lambda e, p=p, m=m: e.tensor_tensor(out=macc[:], in0=p[:], in1=st_g[:, m, :], op=ALU.mult), reads=[p, st_g], writes=[macc])
                        else:
                            cx.op("dve", lambda e, p=p, m=m, bi=bi: e.tensor_tensor(out=mtmp[:], in0=p[:], in1=st_g[:, bi * 8 + m, :], op=ALU.mult), reads=[p, st_g], writes=[mtmp])
                            if bi == 1:
                                cx.op("dve", lambda e: e.tensor_tensor(out=macc[:], in0=macc[:], in1=mtmp[:], op=ALU.add), reads=[macc, mtmp], writes=[macc])
                            else:
                                cx.op("dve", lambda e, m=m: e.tensor_tensor(out=mg[:, m, :], in0=macc[:], in1=mtmp[:], op=ALU.add), reads=[macc, mtmp], writes=[mg])

            def epi_out(p, m, mw):
                cx.op("dve", lambda e: e.tensor_tensor(out=xt[:, m, :], in0=xt[:, m, :], in1=p[:], op=ALU.add), reads=[xt, p], writes=[xt])
            linear_fm("w_out", l, D, 0, D, mg, epi_out)
            ffn(l, "ffn2_wi", "ffn2_wo", 16)
            dst = yT_out if lastlayer else xres
            cx.dma("pool", dst[:, t0:t0 + TT].rearrange("(c p) t -> p c t", p=128), xt[:], reads=[xt])
        cx.barrier()
        cx.pop()

    for l in range(L):
        if "p1" in phases:
            p1(l, l == 0)
        if "p23" in phases:
            p23(l)
        if "p4" in phases:
            p4_ml(l)
        if "p5" in phases:
            p5(l, l == L - 1)
    if dbg is not None:
        src = {"sbq": sbq, "sbk": sbk, "sbv": sbv, "mlq": mlq, "mlk": mlk, "mlv": mlv, "mlo": mlo, "mlif": mlif, "mlaq": mlaq, "mlak": mlak,
               "mlav": mlav, "gat": gat, "ysb": ysb, "yml": yml, "ymla": ymla, "xres": xres, "cosd": cosd, "sind": sind}[dbg[0]]
        cx.dma("pool", dbg_out, src)
        cx.barrier()
    cx.barrier()
    return nc, cx.ninst, cx


NCONST = None
_CONSTS = None


def _get_consts():
    global NCONST, _CONSTS
    if _CONSTS is None:
        _CONSTS = build_consts()
        NCONST = _CONSTS[0].shape[1]
    return _CONSTS


WNAMES = ("ffn1_wi", "ffn1_wo", "w_in", "mla_wq_up", "mla_wkv_up", "w_up_sb", "w_up_ml", "w_up_mla", "w_out", "ffn2_wi", "ffn2_wo")


def make_in_map(inp, b, S, L):
    consts = _get_consts()
    m = {
        "xT": np.ascontiguousarray(inp["x"][b, :S].T),
        "pos": np.ascontiguousarray(inp["positions"][b:b + 1, :S]).astype(np.int32),
        "consts": consts[0],
        "masks": consts[1],
        "vecs": build_vecs(inp, L),
        "b_in": np.ascontiguousarray(inp["b_in"][:L]),
    }
    for k in WNAMES:
        m[k] = np.ascontiguousarray(inp[k][:L])
    return m


def kernel(**inputs):
    inp = {k: np.asarray(v) for k, v in inputs.items()}
    B, S, _ = inp["x"].shape
    L = inp["w_in"].shape[0]
    _get_consts()
    nc, ninst, cx = build_program(S, L)
    vec = build_vecs(inp, L)
    in_maps = []
    for b in range(B):
        m = make_in_map(inp, b, S, L)
        m["vecs"] = vec
        in_maps.append(m)
    res = run_bass_kernel_spmd(nc, in_maps, core_ids=list(range(B)))
    out = np.stack([np.ascontiguousarray(res.results[b]["yT"].T) for b in range(B)], axis=0)
    return out.astype(np.float32)
```
